# Optimizing a Trainium2 kernel written in Bass

```python
import jax, jax.numpy as jnp
from jax import lax
import numpy as np

D_MODEL = 2048
BATCH = 16
SEQ = 2048
DEPTH = 2

HEAD_DIM = 128
A_GROUPS = ((128, 1), (512, 4), (2048, 16))
A_HEADS_PER_GROUP = 4
A_N_GROUPS = len(A_GROUPS)
A_HEADS = A_HEADS_PER_GROUP * A_N_GROUPS
A_WIDTH = A_HEADS * HEAD_DIM
A_OUT = A_HEADS_PER_GROUP * HEAD_DIM
ALIBI_MAX_BIAS = 8.0

B_HEADS = 8
B_NOPE = 128
B_ROPE = 64
B_V = 128
B_QK = B_NOPE + B_ROPE
Q_LORA = 512
KV_LORA = 512
B_OUT = B_HEADS * B_V
ROPE_THETA = 10000.0

N_BRANCH = 2
IN_COLS = 3 * A_WIDTH + Q_LORA + KV_LORA + B_ROPE + N_BRANCH * D_MODEL
QBLOCK = 128

N_GROUPS = 8
EXPERTS_PER_GROUP = 8
N_EXPERTS = N_GROUPS * EXPERTS_PER_GROUP
TOP_K = 2
D_EXPERT = 512
EXPERT_BLOCK = 128

N_MOD = 6
EPS = 1e-6

kernel_name = "hybrid_dilated_mla_hmoe_encoder"


def rms_norm(x, g):
    xf = x.astype(jnp.float32)
    y = xf * lax.rsqrt(jnp.mean(xf * xf, axis=-1, keepdims=True) + EPS)
    return (y * g.astype(jnp.float32)).astype(x.dtype)


def rope(x, pos):
    half = x.shape[-1] // 2
    inv = ROPE_THETA ** (-jnp.arange(half, dtype=jnp.float32) / half)
    ang = pos.astype(jnp.float32)[..., None] * inv
    ang = ang.reshape(ang.shape[:2] + (1,) * (x.ndim - 3) + (half,))
    cos, sin = jnp.cos(ang), jnp.sin(ang)
    xf = x.astype(jnp.float32)
    x1, x2 = xf[..., :half], xf[..., half:]
    return jnp.concatenate([x1 * cos - x2 * sin, x1 * sin + x2 * cos], axis=-1).astype(x.dtype)


def alibi_slopes(n):
    return 2.0 ** (-ALIBI_MAX_BIAS * jnp.arange(1, n + 1, dtype=jnp.float32) / n)


def dilated_group_attention(q, k, v, pos, slopes, dilation, radius):
    bsz, seq, nh, dh = q.shape
    offsets = dilation * jnp.arange(-radius, radius + 1)
    scale = dh ** -0.5
    vf = v.astype(jnp.float32)

    def block(ib):
        t0 = ib * QBLOCK
        t = t0 + jnp.arange(QBLOCK)
        idx = t[:, None] + offsets[None, :]
        valid = (idx >= 0) & (idx < seq)
        idx_c = jnp.clip(idx, 0, seq - 1)
        qb = lax.dynamic_slice_in_dim(q, t0, QBLOCK, axis=1)
        kb = jnp.take(k, idx_c, axis=1)
        vb = jnp.take(vf, idx_c, axis=1)
        s = jnp.einsum('bqhd,bqjhd->bhqj', qb, kb).astype(jnp.float32) * scale
        pq = lax.dynamic_slice_in_dim(pos, t0, QBLOCK, axis=1)
        pk = jnp.take(pos, idx_c, axis=1)
        dist = jnp.abs(pq[:, :, None] - pk).astype(jnp.float32)
        s = s - slopes[None, :, None, None] * dist[:, None]
        s = jnp.where(valid[None, None], s, -1e30)
        mx = jnp.max(s, axis=-1, keepdims=True)
        p = jnp.exp(s - mx)
        den = jnp.sum(p, axis=-1)
        o = jnp.einsum('bhqj,bqjhd->bqhd', p, vb) / jnp.transpose(den, (0, 2, 1))[..., None]
        lse = jnp.transpose(mx[..., 0] + jnp.log(den), (0, 2, 1))
        return o, lse

    o, lse = lax.map(block, jnp.arange(seq // QBLOCK))
    o = jnp.transpose(o, (1, 0, 2, 3, 4)).reshape(bsz, seq, nh, dh)
    lse = jnp.transpose(lse, (1, 0, 2, 3)).reshape(bsz, seq, nh)
    return o, lse


def dense_block_attention(q, k, v, scale):
    bsz, seq, nh, _ = q.shape
    vf = v.astype(jnp.float32)

    def block(ib):
        qb = lax.dynamic_slice_in_dim(q, ib * QBLOCK, QBLOCK, axis=1)
        s = jnp.einsum('bqhd,bkhd->bhqk', qb, k).astype(jnp.float32) * scale
        p = jax.nn.softmax(s, axis=-1)
        return jnp.einsum('bhqk,bkhd->bqhd', p, vf)

    o = lax.map(block, jnp.arange(seq // QBLOCK))
    return jnp.transpose(o, (1, 0, 2, 3, 4)).reshape(bsz, seq, nh, -1)


def hierarchical_route(h, w_grp, b_grp, w_exp, b_exp):
    n = h.shape[0]
    grp_logits = (h @ w_grp + b_grp).astype(jnp.float32)
    p_grp = jax.nn.softmax(grp_logits, axis=-1)
    p_top, g_idx = lax.top_k(p_grp, 1)
    exp_logits = (h @ w_exp + b_exp).astype(jnp.float32).reshape(n, N_GROUPS, EXPERTS_PER_GROUP)
    sel = jnp.take_along_axis(exp_logits, g_idx[:, :, None], axis=1)[:, 0]
    p_in = jax.nn.softmax(sel, axis=-1)
    vals, e_idx = lax.top_k(p_in, TOP_K)
    weights = p_top * vals / jnp.sum(vals, axis=-1, keepdims=True)
    ids = (g_idx * EXPERTS_PER_GROUP + e_idx).astype(jnp.int32)
    return ids, weights


def routed_experts(h, expert_ids, gate_w, w_gu, w_down):
    n_tok, d = h.shape
    m = n_tok * TOP_K
    flat_e = expert_ids.reshape(-1)
    flat_tok = jnp.repeat(jnp.arange(n_tok, dtype=jnp.int32), TOP_K)
    flat_w = gate_w.reshape(-1)
    order = jnp.argsort(flat_e)
    sorted_e = flat_e[order]
    counts = jnp.bincount(flat_e, length=N_EXPERTS)
    starts = jnp.cumsum(counts) - counts
    padded = (counts + EXPERT_BLOCK - 1) // EXPERT_BLOCK * EXPERT_BLOCK
    pends = jnp.cumsum(padded)
    pstarts = pends - padded
    dest = pstarts[sorted_e] + (jnp.arange(m) - starts[sorted_e])
    n_blocks = -(-m // EXPERT_BLOCK) + N_EXPERTS
    n_slots = n_blocks * EXPERT_BLOCK
    slot_tok = jnp.zeros((n_slots,), jnp.int32).at[dest].set(flat_tok[order])
    slot_w = jnp.zeros((n_slots,), h.dtype).at[dest].set(flat_w[order].astype(h.dtype))
    block_e = jnp.minimum(
        jnp.searchsorted(pends, jnp.arange(n_blocks) * EXPERT_BLOCK, side='right'), N_EXPERTS - 1)
    xs = h[slot_tok].reshape(n_blocks, EXPERT_BLOCK, d)

    def expert_block(args):
        xb, e = args
        g, u = jnp.split(xb @ w_gu[e], 2, axis=-1)
        return (jax.nn.silu(g) * u) @ w_down[e]

    ys = lax.map(expert_block, (xs, block_e)).reshape(n_slots, d)
    return jnp.zeros_like(h).at[slot_tok].add(ys * slot_w[:, None])


def setup_inputs(seed: int = 0) -> dict:
    key = jax.random.key(seed)
    ks = jax.random.split(key, 24)
    f32 = jnp.float32
    L, D = DEPTH, D_MODEL

    def nrm(k, shape, fan_in, mult=1.0):
        return (jax.random.normal(k, shape, f32) * (mult * fan_in ** -0.5)).astype(f32)

    x = jax.random.normal(ks[0], (BATCH, SEQ, D), f32)
    c = jax.random.normal(ks[1], (BATCH, D), f32)
    offs = jax.random.randint(ks[2], (BATCH, 1), 0, 1024)
    positions = (jnp.arange(SEQ)[None, :] + offs).astype(jnp.int32)
    return {
        "x": x,
        "c": c,
        "positions": positions,
        "ln1_g": 1.0 + 0.05 * jax.random.normal(ks[3], (L, D), f32),
        "ln2_g": 1.0 + 0.05 * jax.random.normal(ks[4], (L, D), f32),
        "w_ada": nrm(ks[5], (L, D, N_MOD * D), D, 0.1),
        "b_ada": 0.01 * jax.random.normal(ks[6], (L, N_MOD * D), f32),
        "w_in": nrm(ks[7], (L, D, IN_COLS), D),
        "q_norm_g": 1.0 + 0.05 * jax.random.normal(ks[8], (L, Q_LORA), f32),
        "w_uq": nrm(ks[9], (L, Q_LORA, B_HEADS * B_QK), Q_LORA),
        "kv_norm_g": 1.0 + 0.05 * jax.random.normal(ks[10], (L, KV_LORA), f32),
        "w_ukv": nrm(ks[11], (L, KV_LORA, B_HEADS * (B_NOPE + B_V)), KV_LORA),
        "w_a_up": nrm(ks[12], (L, A_OUT, D), A_OUT),
        "w_b_up": nrm(ks[13], (L, B_OUT, D), B_OUT),
        "w_o": nrm(ks[14], (L, D, D), D),
        "w_grp": nrm(ks[15], (L, D, N_GROUPS), D),
        "b_grp": 0.01 * jax.random.normal(ks[16], (L, N_GROUPS), f32),
        "w_exp": nrm(ks[17], (L, D, N_EXPERTS), D),
        "b_exp": 0.01 * jax.random.normal(ks[18], (L, N_EXPERTS), f32),
        "w_gu": nrm(ks[19], (L, N_EXPERTS, D, 2 * D_EXPERT), D),
        "w_down": nrm(ks[20], (L, N_EXPERTS, D_EXPERT, D), D_EXPERT),
        "final_g": 1.0 + 0.05 * jax.random.normal(ks[21], (D,), f32),
    }


def reference(x, c, positions, ln1_g, ln2_g, w_ada, b_ada, w_in, q_norm_g, w_uq,
              kv_norm_g, w_ukv, w_a_up, w_b_up, w_o, w_grp, b_grp, w_exp, b_exp,
              w_gu, w_down, final_g):
    bsz, seq, d = x.shape
    slopes = alibi_slopes(A_HEADS).reshape(A_N_GROUPS, A_HEADS_PER_GROUP)
    split_at = [A_WIDTH, 2 * A_WIDTH, 3 * A_WIDTH, 3 * A_WIDTH + Q_LORA,
                3 * A_WIDTH + Q_LORA + KV_LORA, 3 * A_WIDTH + Q_LORA + KV_LORA + B_ROPE]
    cs = jax.nn.silu(c)
    for l in range(DEPTH):
        mod = (cs @ w_ada[l] + b_ada[l])[:, None, :]
        sh1, sc1, gt1, sh2, sc2, gt2 = jnp.split(mod, N_MOD, axis=-1)

        h = rms_norm(x, ln1_g[l]) * (1.0 + sc1) + sh1
        proj = h @ w_in[l]
        qa, ka, va, cq, ckv, kr, gates = jnp.split(proj, split_at, axis=-1)

        qa = qa.reshape(bsz, seq, A_N_GROUPS, A_HEADS_PER_GROUP, HEAD_DIM)
        ka = ka.reshape(bsz, seq, A_N_GROUPS, A_HEADS_PER_GROUP, HEAD_DIM)
        va = va.reshape(bsz, seq, A_N_GROUPS, A_HEADS_PER_GROUP, HEAD_DIM)
        outs, lses = [], []
        for gi, (win, dil) in enumerate(A_GROUPS):
            o_g, lse_g = dilated_group_attention(qa[:, :, gi], ka[:, :, gi], va[:, :, gi],
                                                 positions, slopes[gi], dil, win // (2 * dil))
            outs.append(o_g)
            lses.append(lse_g)
        w_den = jax.nn.softmax(jnp.stack(lses, axis=0), axis=0)
        o_a = jnp.sum(w_den[..., None] * jnp.stack(outs, axis=0), axis=0)
        ya = o_a.reshape(bsz, seq, A_OUT).astype(x.dtype)

        q = (rms_norm(cq, q_norm_g[l]) @ w_uq[l]).reshape(bsz, seq, B_HEADS, B_QK)
        q = jnp.concatenate([q[..., :B_NOPE], rope(q[..., B_NOPE:], positions)], axis=-1)
        kv = (rms_norm(ckv, kv_norm_g[l]) @ w_ukv[l]).reshape(bsz, seq, B_HEADS, B_NOPE + B_V)
        k_rope = jnp.broadcast_to(rope(kr, positions)[:, :, None, :], (bsz, seq, B_HEADS, B_ROPE))
        k = jnp.concatenate([kv[..., :B_NOPE], k_rope], axis=-1)
        yb = dense_block_attention(q, k, kv[..., B_NOPE:], B_QK ** -0.5)
        yb = yb.reshape(bsz, seq, B_OUT).astype(x.dtype)

        g_a, g_b = jnp.split(jax.nn.sigmoid(gates.astype(jnp.float32)).astype(x.dtype), N_BRANCH, axis=-1)
        merged = g_a * (ya @ w_a_up[l]) + g_b * (yb @ w_b_up[l])
        x = x + (1.0 + gt1) * (merged @ w_o[l])

        h2 = rms_norm(x, ln2_g[l]) * (1.0 + sc2) + sh2
        hf = h2.reshape(-1, d)
        ids, wts = hierarchical_route(hf, w_grp[l], b_grp[l], w_exp[l], b_exp[l])
        y = routed_experts(hf, ids, wts, w_gu[l], w_down[l]).reshape(bsz, seq, d)
        x = x + (1.0 + gt2) * y
    return rms_norm(x, final_g)
```

```python
import math
from contextlib import ExitStack, contextmanager

import numpy as np
import concourse.bass as bass
import concourse.mybir as mybir
from concourse.bass_utils import run_bass_kernel_spmd

F32 = mybir.dt.float32
BF16 = mybir.dt.bfloat16
I32 = mybir.dt.int32
ALU = mybir.AluOpType
AF = mybir.ActivationFunctionType
AX = mybir.AxisListType

S = 2048
D = 2048
NE_FULL = 64
C_CAP = 256
TR = 4096
EPS = 1e-6
BIG = 1.0e9
IN_COLS = 9792
N_CORES = 8


class Res:
    def __init__(self, k, name, dma=False, multi=False):
        self.name = name
        self.w = None
        self.r = {}
        self.multi = multi
        self.sem = None
        self.cnt = 0
        if dma:
            self.sem, self.cnt = k.take_sem()
            k.live.append(self)


class Tl:
    def __init__(self, t, res):
        self.t = t
        self.res = res

    def __getitem__(self, i):
        return self.t[i]


class Eng:
    def __init__(self, k, name, h):
        self.name = name
        self.h = h
        self.sem = k.new_sem("e_" + name)
        self.cnt = 0
        self.waited = {}

    def wait(self, tok):
        if tok is None:
            return
        sem, val = tok
        if self.name == "pe" and sem is self.sem:
            return
        if self.waited.get(id(sem), 0) >= val:
            return
        self.waited[id(sem)] = val
        self.h.wait_ge(sem, val)


class Scope:
    def __init__(self, k):
        self.k = k
        self.stack = ExitStack()
        self.res = []

    def sb(self, name, shape, dt, dma=False):
        self.k.uid += 1
        t = self.stack.enter_context(self.k.nc.sbuf_tensor(f"{name}_{self.k.uid}", shape, dt))
        r = Res(self.k, name, dma)
        self.res.append(r)
        return Tl(t, r)

    def ps(self, name, shape, dt=F32):
        self.k.uid += 1
        t = self.stack.enter_context(self.k.nc.psum_tensor(f"{name}_{self.k.uid}", shape, dt))
        r = Res(self.k, name, False)
        self.res.append(r)
        return Tl(t, r)


class K:
    def __init__(self, nc, stack):
        self.nc = nc
        self.stack = stack
        self.free = []
        self.live = []
        self.uid = 0
        self.eng = {
            "pe": Eng(self, "pe", nc.tensor), "act": Eng(self, "act", nc.scalar),
            "dve": Eng(self, "dve", nc.vector), "pool": Eng(self, "pool", nc.gpsimd),
            "sp": Eng(self, "sp", nc.sync),
        }

    def new_sem(self, name):
        self.uid += 1
        self.nsem = getattr(self, "nsem", 0) + 1
        return self.stack.enter_context(self.nc.semaphore(f"{name}_{self.uid}"))

    def take_sem(self):
        while self.free:
            sem, cnt = self.free.pop()
            if cnt < 24000:
                return (sem, cnt)
        return (self.new_sem("d"), 0)

    def res(self, name, dma=True, multi=True):
        return Res(self, name, dma, multi)

    def barrier(self):
        toks = [(e.sem, e.cnt) for e in self.eng.values() if e.cnt]
        toks += [(r.sem, r.cnt) for r in self.live if r.cnt]
        for e in self.eng.values():
            for t in toks:
                e.wait(t)
        for e in self.eng.values():
            if e.cnt > 24000:
                e.sem = self.new_sem("e_" + e.name)
                e.cnt = 0

    @contextmanager
    def scope(self):
        sc = Scope(self)
        try:
            yield sc
        except BaseException:
            import traceback
            if not getattr(self, "_tb_done", False):
                traceback.print_exc()
                self._tb_done = True
            raise
        else:
            self.barrier()
            for r in sc.res:
                if r.sem is not None:
                    self.live.remove(r)
                    self.free.append((r.sem, r.cnt))
            sc.stack.close()

    @staticmethod
    def _r(x):
        return x if isinstance(x, Res) else x.res

    def _deps(self, e, reads, writes):
        for x in reads:
            e.wait(self._r(x).w)
        for x in writes:
            x = self._r(x)
            if not x.multi:
                e.wait(x.w)
            for tok in list(x.r.values()):
                e.wait(tok)

    def op(self, en, fn, reads=(), writes=()):
        e = self.eng[en]
        self._deps(e, reads, writes)
        e.cnt += 1
        tok = (e.sem, e.cnt)
        fn(e.h).then_inc(e.sem, 1)
        for x in reads:
            self._r(x).r[id(e.sem)] = tok
        for x in writes:
            x = self._r(x)
            x.w = tok
            x.r = {}
        return tok

    def dma(self, en, fns, reads=(), dst=None, inc=16):
        e = self.eng[en]
        d = self._r(dst)
        self._deps(e, reads, [d])
        if not isinstance(fns, (list, tuple)):
            fns = [fns]
        for fn in fns:
            d.cnt += inc
            fn(e.h).then_inc(d.sem, inc)
        tok = (d.sem, d.cnt)
        for x in reads:
            self._r(x).r[id(d.sem)] = tok
        d.w = tok
        if not d.multi:
            d.r = {}
        return tok


def build(NB, NL=2, NG=8, stop=None, ext=()):
    T = NB * S
    NT = T // 128
    NE = NG * 8
    NR = T // TR
    nc = bass.Bass("TRN2", target_bir_lowering=False)

    def din(name, shape, dt=F32):
        return nc.dram_tensor(name, shape, dt, kind="ExternalInput").ap()

    def dscr(name, shape, dt=F32):
        kind = "ExternalOutput" if name in ext else "Internal"
        return nc.dram_tensor(name, shape, dt, kind=kind).ap()

    run_attn = stop != "mod"
    run_moe = stop not in ("mod", "attn")
    nl_attn = NL if stop is None else (1 if run_attn else 0)
    nl_moe = NL if stop is None else (1 if run_moe else 0)

    x_d = din("x", [T, D])
    c_d = din("c", [NB, D])
    pos_d = din("positions", [NB, S], I32)
    W = {}
    for l in range(NL if stop is None else 1):
        W[l] = dict(
            w_ada=din(f"w_ada_{l}", [D, 6 * D]), b_ada=din(f"b_ada_{l}", [1, 6 * D]))
        if run_attn:
            W[l].update(
                ln1_g=din(f"ln1_g_{l}", [1, D]),
                w_in=din(f"w_in_{l}", [D, IN_COLS]),
                q_norm_g=din(f"q_norm_g_{l}", [512]), w_uq=din(f"w_uq_{l}", [512, 1536]),
                kv_norm_g=din(f"kv_norm_g_{l}", [512]), w_ukv=din(f"w_ukv_{l}", [512, 2048]),
                w_a_up=din(f"w_a_up_{l}", [512, D]), w_b_up=din(f"w_b_up_{l}", [1024, D]),
                w_o=din(f"w_o_{l}", [D, D]))
        if run_moe:
            W[l].update(
                ln2_g=din(f"ln2_g_{l}", [1, D]),
                w_grp=din(f"w_grp_{l}", [D, 8]), b_grp=din(f"b_grp_{l}", [1, 8]),
                w_exp=din(f"w_exp_{l}", [D, 64]), b_exp=din(f"b_exp_{l}", [1, 64]),
                w_gu=[din(f"w_gu_{l}_{g}", [8, D, 1024]) for g in range(NG)],
                w_down=[din(f"w_down_{l}_{g}", [8, 512, D]) for g in range(NG)])
    fin_g = din("final_g", [1, D]) if stop is None else None
    out_d = nc.dram_tensor("out", [T, D], F32, kind="ExternalOutput").ap()

    modD = dscr("modD", [2, NB, 6 * D])
    xaD = dscr("xaD", [T, D])
    xbD = dscr("xbD", [T, D])
    oD = [dscr(f"oD{g}", [T, 512]) for g in range(3)]
    lseD = [dscr(f"lseD{g}", [T, 4]) for g in range(3)]
    ybD = dscr("ybD", [T, 1024], BF16)
    gD = dscr("gD", [T, 4096], BF16)
    NSLOT = NE * C_CAP
    XsD = dscr("XsD", [NSLOT + 128, D], BF16)
    YsD = dscr("YsD", [NSLOT + 128, D])
    slotD = dscr("slotD", [T, 4])

    slopes = [2.0 ** (-8.0 * (n + 1) / 12.0) for n in range(12)]

    with ExitStack() as stack:
        k = K(nc, stack)
        r_mod = k.res("modD")
        r_xa = k.res("xaD")
        r_xb = k.res("xbD")
        r_o = [k.res(f"oD{g}") for g in range(3)]
        r_lse = [k.res(f"lseD{g}") for g in range(3)]
        r_yb = k.res("ybD")
        r_g = k.res("gD")
        r_xs = k.res("XsD")
        r_ys = k.res("YsD")
        r_out = k.res("out")
        r_slot = k.res("slotD")

        with k.scope() as g0:
            identf = g0.sb("identf", [128, 128], F32)
            ident = g0.sb("ident", [128, 128], BF16)
            onesf = g0.sb("onesf", [128, 128], F32)
            onesb = g0.sb("onesb", [128, 128], BF16)
            LTf = g0.sb("LTf", [128, 128], F32)
            LT = g0.sb("LT", [128, 128], BF16)
            mband = g0.sb("mband", [128, 384], F32)
            eCi = g0.sb("eCi", [128, 64], I32)
            eC = g0.sb("eC", [128, 64], F32)
            invi = g0.sb("invi", [64, 1], I32)
            inv = g0.sb("inv", [64, 1], F32)
            sgn = g0.sb("sgn", [64, 1], F32)

            k.op("pool", lambda e: e.memset(onesf[:], 1.0), writes=[onesf])
            k.op("pool", lambda e: e.memset(identf[:], 1.0), writes=[identf])
            k.op("pool", lambda e: e.affine_select(identf[:], identf[:], [[-1, 128]], ALU.is_equal, 0.0,
                                                    base=0, channel_multiplier=1), reads=[identf], writes=[identf])
            k.op("dve", lambda e: e.tensor_copy(ident[:], identf[:]), reads=[identf], writes=[ident])
            k.op("dve", lambda e: e.tensor_copy(onesb[:], onesf[:]), reads=[onesf], writes=[onesb])
            k.op("pool", lambda e: e.affine_select(LTf[:], onesf[:], [[1, 128]], ALU.is_ge, 0.0,
                                                    base=-1, channel_multiplier=-1), reads=[onesf], writes=[LTf])
            k.op("dve", lambda e: e.tensor_copy(LT[:], LTf[:]), reads=[LTf], writes=[LT])
            k.op("pool", lambda e: e.memset(mband[:], 0.0), writes=[mband])
            k.op("pool", lambda e: e.affine_select(mband[:], mband[:], [[1, 384]], ALU.is_ge, BIG,
                                                    base=-64, channel_multiplier=-1), reads=[mband], writes=[mband])
            k.op("pool", lambda e: e.affine_select(mband[:], mband[:], [[-1, 384]], ALU.is_ge, BIG,
                                                    base=192, channel_multiplier=1), reads=[mband], writes=[mband])
            k.op("pool", lambda e: e.iota(eCi[:], [[C_CAP, 64]], base=0, channel_multiplier=0), writes=[eCi])
            k.op("dve", lambda e: e.tensor_copy(eC[:], eCi[:]), reads=[eCi], writes=[eC])
            k.op("pool", lambda e: e.iota(invi[0:32, :], [[0, 1]], base=0, channel_multiplier=1), writes=[invi])
            k.op("pool", lambda e: e.iota(invi[32:64, :], [[0, 1]], base=0, channel_multiplier=1), writes=[invi])
            k.op("dve", lambda e: e.tensor_copy(inv[:], invi[:]), reads=[invi], writes=[inv])
            k.op("act", lambda e: e.activation(inv[:], inv[:], AF.Exp, scale=-math.log(10000.0) / 32.0),
                 reads=[inv], writes=[inv])
            k.op("pool", lambda e: e.memset(sgn[0:32, :], -1.0), writes=[sgn])
            k.op("pool", lambda e: e.memset(sgn[32:64, :], 1.0), writes=[sgn])
            epsD = g0.sb("epsD", [128, 1], F32)
            k.op("pool", lambda e: e.memset(epsD[:], EPS), writes=[epsD])
            cst = {"eps": epsD}
            if run_moe:
                with k.scope() as sz:
                    zeros = sz.sb("zeros", [128, D], F32)
                    k.op("pool", lambda e: e.memset(zeros[:], 0.0), writes=[zeros])
                    k.dma("sp", lambda e: e.dma_start(out=YsD[NSLOT:NSLOT + 128, :], in_=zeros[:]), reads=[zeros], dst=r_ys)

            def bcast(sc, name, row_ap, n, reads=()):
                t = sc.sb(name, [128, n], F32, dma=True)
                k.dma("sp", lambda e: e.dma_start(out=t[:], in_=row_ap.broadcast_to([128, n])), reads=reads, dst=t)
                return t

            rr = [0]

            def evac(out_ap, in_ap, reads, writes, scale=None):
                rr[0] += 1
                if rr[0] % 2:
                    if scale is None:
                        k.op("act", lambda e: e.copy(out_ap, in_ap), reads=reads, writes=writes)
                    else:
                        k.op("act", lambda e: e.mul(out_ap, in_ap, scale), reads=reads, writes=writes)
                else:
                    if scale is None:
                        k.op("dve", lambda e: e.tensor_copy(out_ap, in_ap), reads=reads, writes=writes)
                    else:
                        k.op("dve", lambda e: e.tensor_scalar(out_ap, in_ap, scale, None, ALU.mult),
                             reads=reads, writes=writes)

            def wload(t, src_ap, ncol, nsplit=1):
                v = src_ap.rearrange("(c p) n -> p c n", p=128)
                step = ncol // nsplit
                k.dma("pool", [lambda e, i=i: e.dma_start(out=t[:, :, i * step:(i + 1) * step],
                                                          in_=v[:, :, i * step:(i + 1) * step])
                               for i in range(nsplit)], dst=t)

            with k.scope() as sm:
                cT = sm.sb("cT", [128, 16, NB], F32, dma=True)
                csT = sm.sb("csT", [128, 16, NB], F32)
                k.dma("sp", [lambda e, b=b: e.dma_start(out=cT[:, :, b], in_=c_d[b].rearrange("(c p) -> p c", p=128),
                                                        allow_slow_non_contiguous=True) for b in range(NB)], dst=cT)
                k.op("act", lambda e: e.activation(csT[:], cT[:], AF.Silu), reads=[cT], writes=[csT])
                wb = [sm.sb(f"wada{i}", [128, 16, 512], F32, dma=True) for i in range(2)]
                mps = [sm.ps(f"mps{i}", [NB, 512]) for i in range(2)]
                msb = [sm.sb(f"msb{i}", [NB, 512], F32) for i in range(2)]
                for l in W:
                    bsb = sm.sb(f"bsb{l}", [NB, 6 * D], F32, dma=True)
                    k.dma("sp", lambda e, l=l: e.dma_start(out=bsb[:], in_=W[l]["b_ada"].broadcast_to([NB, 6 * D])), dst=bsb)
                    wv = W[l]["w_ada"].rearrange("(c p) n -> p c n", p=128)
                    for n in range(24):
                        i = n % 2
                        k.dma("sp", [lambda e, n=n, i=i, h=h: e.dma_start(out=wb[i][:, h * 8:(h + 1) * 8, :],
                                                                          in_=wv[:, h * 8:(h + 1) * 8, n * 512:(n + 1) * 512])
                                     for h in range(2)], dst=wb[i])
                        for c in range(16):
                            k.op("pe", lambda e, c=c, i=i: e.matmul(mps[i][:], csT[:, c, :], wb[i][:, c, :],
                                                                    start=(c == 0), stop=(c == 15)),
                                 reads=[csT, wb[i]], writes=[mps[i]])
                        k.op("dve", lambda e, n=n, i=i: e.tensor_tensor(msb[i][:], mps[i][:], bsb[:, n * 512:(n + 1) * 512], ALU.add),
                             reads=[mps[i], bsb], writes=[msb[i]])
                        k.dma("sp", lambda e, n=n, i=i, l=l: e.dma_start(out=modD[l, :, n * 512:(n + 1) * 512], in_=msb[i][:]),
                              reads=[msb[i]], dst=r_mod)

            def mod_row(l, b, j):
                return modD[l, b:b + 1, j * D:(j + 1) * D]

            def ln_stats(sc_tiles, xt, junk, st):
                k.op("act", lambda e: e.activation(junk[:], xt[:], AF.Square, accum_out=st[:, 0:1]),
                     reads=[xt], writes=[junk, st])
                k.op("act", lambda e: e.activation(st[:, 1:2], st[:, 0:1], AF.Sqrt, bias=sc_tiles["eps"][:, 0:1], scale=1.0 / D),
                     reads=[st, sc_tiles["eps"]], writes=[st])
                k.op("dve", lambda e: e.reciprocal(st[:, 1:2], st[:, 1:2]), reads=[st], writes=[st])

            def rope_tables(sc, b, cos_t, sin_t, scale):
                pki = sc.sb("pki", [64, S], I32, dma=True)
                ang = sc.sb("ang", [64, S], F32)
                kk = sc.sb("kk", [64, S], F32)
                kki = sc.sb("kki", [64, S], I32)
                k.dma("sp", lambda e: e.dma_start(out=pki[:], in_=pos_d[b:b + 1, :].broadcast_to([64, S])), dst=pki)
                k.op("dve", lambda e: e.tensor_copy(ang[:], pki[:]), reads=[pki], writes=[ang])
                k.op("dve", lambda e: e.tensor_scalar(ang[:], ang[:], inv[:, 0:1], None, ALU.mult), reads=[ang, inv], writes=[ang])
                TWO_PI = 2.0 * math.pi

                def reduce_sin(dst, shift):
                    k.op("dve", lambda e: e.tensor_scalar(kk[:], ang[:], shift, 1.0 / TWO_PI, ALU.add, ALU.mult), reads=[ang], writes=[kk])
                    k.op("dve", lambda e: e.tensor_copy(kki[:], kk[:]), reads=[kk], writes=[kki])
                    k.op("dve", lambda e: e.tensor_copy(kk[:], kki[:]), reads=[kki], writes=[kk])
                    k.op("dve", lambda e: e.scalar_tensor_tensor(kk[:], kk[:], -TWO_PI, ang[:], ALU.mult, ALU.add), reads=[kk, ang], writes=[kk])
                    if shift:
                        k.op("dve", lambda e: e.tensor_scalar(kk[:], kk[:], shift, None, ALU.add), reads=[kk], writes=[kk])
                    k.op("dve", lambda e: e.tensor_scalar(dst[:], kk[:], math.pi, -TWO_PI, ALU.is_gt, ALU.mult), reads=[kk], writes=[dst])
                    k.op("dve", lambda e: e.tensor_tensor(kk[:], kk[:], dst[:], ALU.add), reads=[kk, dst], writes=[kk])
                    k.op("dve", lambda e: e.tensor_scalar(dst[:], kk[:], -math.pi, TWO_PI, ALU.is_lt, ALU.mult), reads=[kk], writes=[dst])
                    k.op("dve", lambda e: e.tensor_tensor(kk[:], kk[:], dst[:], ALU.add), reads=[kk, dst], writes=[kk])
                    k.op("dve", lambda e: e.tensor_scalar(kk[:], kk[:], math.pi, -math.pi, ALU.min, ALU.max), reads=[kk], writes=[kk])
                    k.op("act", lambda e: e.activation(dst[:], kk[:], AF.Sin), reads=[kk], writes=[dst])

                reduce_sin(sin_t, 0.0)
                reduce_sin(cos_t, math.pi / 2.0)
                k.op("dve", lambda e: e.tensor_scalar(sin_t[:], sin_t[:], sgn[:, 0:1], scale, ALU.mult, ALU.mult), reads=[sin_t, sgn], writes=[sin_t])
                if scale != 1.0:
                    k.op("dve", lambda e: e.tensor_scalar(cos_t[:], cos_t[:], scale, None, ALU.mult), reads=[cos_t], writes=[cos_t])

            def attn_seq(l, b, xin, r_xin):
                Wl = W[l]
                win = Wl["w_in"]
                with k.scope() as so:
                    cqn = so.sb("cqn", [128, 4, S], BF16)
                    ckvn = so.sb("ckvn", [128, 4, S], BF16)
                    krT = so.sb("krT", [64, S], BF16)
                    with k.scope() as sh:
                        hT = sh.sb("hT", [128, 16, S], BF16)
                        with k.scope() as s1:
                            A1 = bcast(s1, "A1", mod_row(l, b, 1), D, reads=[r_mod])
                            B1 = bcast(s1, "B1", mod_row(l, b, 0), D, reads=[r_mod])
                            G1 = bcast(s1, "G1", Wl["ln1_g"], D)
                            k.op("dve", lambda e: e.scalar_tensor_tensor(A1[:], A1[:], 1.0, G1[:], ALU.add, ALU.mult),
                                 reads=[A1, G1], writes=[A1])
                            xt = [s1.sb(f"xt{i}", [128, D], F32, dma=True) for i in range(2)]
                            junk = s1.sb("junk", [128, D], F32)
                            hb = [s1.sb(f"hb{i}", [128, D], BF16) for i in range(2)]
                            st = [s1.sb(f"st{i}", [128, 2], F32) for i in range(2)]
                            pT = [s1.ps(f"pT{i}", [128, 8, 128], BF16) for i in range(2)]
                            for tt in range(16):
                                i = tt % 2
                                r0 = b * S + tt * 128
                                k.dma("sp", lambda e, i=i, r0=r0: e.dma_start(out=xt[i][:], in_=xin[r0:r0 + 128, :]),
                                      reads=[r_xin] if r_xin else [], dst=xt[i])
                                ln_stats(cst, xt[i], junk, st[i])
                                k.op("dve", lambda e, i=i: e.scalar_tensor_tensor(junk[:], xt[i][:], st[i][:, 1:2], A1[:], ALU.mult, ALU.mult),
                                     reads=[xt[i], st[i], A1], writes=[junk])
                                k.op("dve", lambda e, i=i: e.tensor_tensor(hb[i][:], junk[:], B1[:], ALU.add),
                                     reads=[junk, B1], writes=[hb[i]])
                                for hf in range(2):
                                    for j in range(8):
                                        c = hf * 8 + j
                                        k.op("pe", lambda e, i=i, hf=hf, j=j, c=c: e.transpose(pT[hf][:, j, :], hb[i][:, c * 128:(c + 1) * 128], ident[:]),
                                             reads=[hb[i], ident], writes=[pT[hf]])
                                    evac(hT[:, hf * 8:(hf + 1) * 8, tt * 128:(tt + 1) * 128], pT[hf][:], [pT[hf]], [hT])

                        for g, dil in enumerate((1, 4, 16)):
                            Lc = S // dil
                            tpc = Lc // 128

                            def hblk(c, tb_):
                                if dil == 1:
                                    return hT[:, c, tb_ * 512:(tb_ + 1) * 512]
                                v = hT[:, c, :].rearrange("p (i d) -> p d i", d=dil)
                                if dil == 4:
                                    return v[:, tb_, :]
                                return v[:, 4 * tb_:4 * tb_ + 4, :]

                            def htile(c, tt):
                                if dil == 1:
                                    return hT[:, c, tt * 128:(tt + 1) * 128]
                                v = hT[:, c, :].rearrange("p (i d) -> p d i", d=dil)
                                if dil == 4:
                                    return v[:, tt // 4, (tt % 4) * 128:(tt % 4 + 1) * 128]
                                return v[:, tt, :]

                            def pso(ps):
                                return ps[:].rearrange("p (r i) -> p r i", r=4) if dil == 16 else ps[:]

                            with k.scope() as sg:
                                qT = sg.sb("qT", [128, 4, S], BF16)
                                kT = sg.sb("kT", [128, 4, S], BF16)
                                V = sg.sb("V", [128, 16, 512], BF16)
                                with k.scope() as sw:
                                    wq = sw.sb("wq", [128, 16, 512], BF16, dma=True)
                                    wk = sw.sb("wk", [128, 16, 512], BF16, dma=True)
                                    wv = sw.sb("wv", [128, 16, 512], BF16, dma=True)
                                    wload(wq, win[:, g * 512:(g + 1) * 512], 512)
                                    wload(wk, win[:, 1536 + g * 512:1536 + (g + 1) * 512], 512)
                                    wload(wv, win[:, 3072 + g * 512:3072 + (g + 1) * 512], 512)
                                    pp = [sw.ps(f"pp{i}", [128, 512]) for i in range(2)]
                                    n = 0
                                    for (w_, dst, scl) in ((wq, qT, 128.0 ** -0.5), (wk, kT, None)):
                                        for h in range(4):
                                            for tb_ in range(4):
                                                ps = pp[n % 2]
                                                n += 1
                                                for c in range(16):
                                                    k.op("pe", lambda e, ps=ps, w_=w_, h=h, c=c, tb_=tb_: e.matmul(
                                                        pso(ps), w_[:, c, h * 128:(h + 1) * 128], hblk(c, tb_), start=(c == 0), stop=(c == 15)),
                                                        reads=[w_, hT], writes=[ps])
                                                evac(dst[:, h, tb_ * 512:(tb_ + 1) * 512], ps[:], [ps], [dst], scale=scl)
                                    for tt in range(16):
                                        ps = pp[n % 2]
                                        n += 1
                                        for c in range(16):
                                            k.op("pe", lambda e, ps=ps, c=c, tt=tt: e.matmul(ps[:], htile(c, tt), wv[:, c, :], start=(c == 0), stop=(c == 15)),
                                                 reads=[wv, hT], writes=[ps])
                                        evac(V[:, tt, :], ps[:], [ps], [V])
                                pqi = sg.sb("pqi", [128, 16], I32, dma=True)
                                pq = sg.sb("pq", [128, 16], F32)
                                pki = sg.sb("pki", [128, S], I32, dma=True)
                                pk = sg.sb("pk", [128, S], F32)
                                pb = pos_d[b]
                                if dil == 1:
                                    k.dma("sp", lambda e: e.dma_start(out=pqi[:], in_=pb.rearrange("(t a) -> a t", a=128), allow_slow_non_contiguous=True), dst=pqi)
                                elif dil == 4:
                                    k.dma("sp", lambda e: e.dma_start(out=pqi[:].rearrange("a (r q) -> a r q", r=4),
                                                                      in_=pb.rearrange("(q a r) -> a r q", q=4, a=128, r=4), allow_slow_non_contiguous=True), dst=pqi)
                                else:
                                    k.dma("sp", lambda e: e.dma_start(out=pqi[:], in_=pb.rearrange("(a r) -> a r", r=16)), dst=pqi)
                                k.dma("sp", lambda e: e.dma_start(out=pki[:], in_=pos_d[b:b + 1, :].broadcast_to([128, S])), dst=pki)
                                k.op("dve", lambda e: e.tensor_copy(pq[:], pqi[:]), reads=[pqi], writes=[pq])
                                k.op("dve", lambda e: e.tensor_scalar(pq[:], pq[:], -1.0, None, ALU.mult), reads=[pq], writes=[pq])
                                k.op("dve", lambda e: e.tensor_copy(pk[:], pki[:]), reads=[pki], writes=[pk])
                                dist = [sg.sb(f"dist{i}", [128, 384], F32) for i in range(2)]
                                ssb = [sg.sb(f"ssb{i}", [128, 384], F32) for i in range(2)]
                                pb16 = [sg.sb(f"pb{i}", [128, 384], BF16) for i in range(2)]
                                pts = [sg.sb(f"pts{i}", [128, 3, 128], BF16) for i in range(2)]
                                og = [sg.sb(f"og{i}", [128, 512], F32) for i in range(2)]
                                lse = [sg.sb(f"lse{i}", [128, 4], F32) for i in range(2)]
                                sm_ = [[sg.sb(f"sm{i}_{j}", [128, 1], F32) for j in range(5)] for i in range(2)]
                                sps = [sg.ps(f"sps{i}", [128, 512]) for i in range(2)]
                                ptp = [sg.ps(f"ptp{i}", [128, 4, 128], BF16) for i in range(2)]
                                ops = [sg.ps(f"ops{i}", [128, 512]) for i in range(2)]
                                for tt in range(16):
                                    it = tt % 2
                                    cls, ti = tt // tpc, tt % tpc
                                    i0 = ti * 128
                                    pbase = cls * Lc
                                    lo, hi = max(0, i0 - 128), min(Lc, i0 + 256)
                                    w = hi - lo
                                    c0 = 128 - (i0 - lo)
                                    nch = w // 128
                                    if dil == 1:
                                        pkv = pk[:, lo:hi]
                                    else:
                                        pkv = pk[:, :].rearrange("p (i d) -> p d i", d=dil)[:, cls, lo:hi]
                                    k.op("act", lambda e, it=it, pkv=pkv, tt=tt, w=w: e.activation(dist[it][:, :w], pkv, AF.Abs, bias=pq[:, tt:tt + 1], scale=1.0),
                                         reads=[pk, pq], writes=[dist[it]])
                                    k.op("dve", lambda e, it=it, w=w, c0=c0: e.tensor_tensor(dist[it][:, :w], dist[it][:, :w], mband[:, c0:c0 + w], ALU.add),
                                         reads=[dist[it], mband], writes=[dist[it]])
                                    for h in range(4):
                                        ih = h % 2
                                        mx, nmx, ll, rl, lnl = sm_[ih]
                                        slope = slopes[g * 4 + h]
                                        q0 = pbase + i0
                                        k.op("pe", lambda e, ih=ih, h=h, q0=q0, w=w, lo=lo, pbase=pbase: e.matmul(
                                            sps[ih][:, :w], qT[:, h, q0:q0 + 128], kT[:, h, pbase + lo:pbase + lo + w], start=True, stop=True),
                                            reads=[qT, kT], writes=[sps[ih]])
                                        k.op("dve", lambda e, ih=ih, it=it, w=w, slope=slope: e.scalar_tensor_tensor(
                                            ssb[ih][:, :w], dist[it][:, :w], -slope, sps[ih][:, :w], ALU.mult, ALU.add),
                                            reads=[dist[it], sps[ih]], writes=[ssb[ih]])
                                        k.op("dve", lambda e, ih=ih, w=w, mx=mx: e.reduce_max(mx[:], ssb[ih][:, :w], AX.X), reads=[ssb[ih]], writes=[mx])
                                        k.op("dve", lambda e, mx=mx, nmx=nmx: e.tensor_scalar(nmx[:], mx[:], -1.0, None, ALU.mult), reads=[mx], writes=[nmx])
                                        k.op("act", lambda e, ih=ih, w=w, nmx=nmx, ll=ll: e.activation(pb16[ih][:, :w], ssb[ih][:, :w], AF.Exp, bias=nmx[:, 0:1], scale=1.0, accum_out=ll[:, 0:1]),
                                             reads=[ssb[ih], nmx], writes=[pb16[ih], ll])
                                        for j in range(nch):
                                            k.op("pe", lambda e, ih=ih, j=j: e.transpose(ptp[ih][:, j, :], pb16[ih][:, j * 128:(j + 1) * 128], ident[:]),
                                                 reads=[pb16[ih], ident], writes=[ptp[ih]])
                                        evac(pts[ih][:, :nch, :], ptp[ih][:, :nch, :], [ptp[ih]], [pts[ih]])
                                        vt0 = (pbase + lo) // 128
                                        for j in range(nch):
                                            k.op("pe", lambda e, it=it, ih=ih, j=j, h=h, vt0=vt0, nch=nch: e.matmul(
                                                ops[it][:, h * 128:(h + 1) * 128], pts[ih][:, j, :], V[:, vt0 + j, h * 128:(h + 1) * 128],
                                                start=(j == 0), stop=(j == nch - 1)), reads=[pts[ih], V], writes=[ops[it]])
                                        k.op("dve", lambda e, ll=ll, rl=rl: e.reciprocal(rl[:], ll[:]), reads=[ll], writes=[rl])
                                        k.op("dve", lambda e, it=it, h=h, rl=rl: e.tensor_scalar(og[it][:, h * 128:(h + 1) * 128], ops[it][:, h * 128:(h + 1) * 128], rl[:, 0:1], None, ALU.mult),
                                             reads=[ops[it], rl], writes=[og[it]])
                                        k.op("act", lambda e, ll=ll, lnl=lnl: e.activation(lnl[:], ll[:], AF.Ln), reads=[ll], writes=[lnl])
                                        k.op("dve", lambda e, it=it, h=h, mx=mx, lnl=lnl: e.tensor_tensor(lse[it][:, h:h + 1], mx[:], lnl[:], ALU.add),
                                             reads=[mx, lnl], writes=[lse[it]])
                                    ob = oD[g][b * S:(b + 1) * S, :]
                                    lb = lseD[g][b * S:(b + 1) * S, :]
                                    if dil == 1:
                                        orow, lrow = ob[tt * 128:(tt + 1) * 128, :], lb[tt * 128:(tt + 1) * 128, :]
                                    else:
                                        orow = ob.rearrange("(i d) f -> d i f", d=dil)[cls, i0:i0 + 128, :]
                                        lrow = lb.rearrange("(i d) f -> d i f", d=dil)[cls, i0:i0 + 128, :]
                                    k.dma("sp", lambda e, it=it, orow=orow: e.dma_start(out=orow, in_=og[it][:]), reads=[og[it]], dst=r_o[g])
                                    k.dma("sp", lambda e, it=it, lrow=lrow: e.dma_start(out=lrow, in_=lse[it][:]), reads=[lse[it]], dst=r_lse[g])

                        with k.scope() as sl:
                            cosk = sl.sb("cosk", [64, S], F32)
                            sink = sl.sb("sink", [64, S], F32)
                            with k.scope() as srt:
                                rope_tables(srt, b, cosk, sink, 1.0)
                            wcq = sl.sb("wcq", [128, 16, 512], BF16, dma=True)
                            wckv = sl.sb("wckv", [128, 16, 512], BF16, dma=True)
                            wkr = sl.sb("wkr", [128, 16, 128], BF16, dma=True)
                            wload(wcq, win[:, 4608:5120], 512)
                            wload(wckv, win[:, 5120:5632], 512)
                            wv_ = win.rearrange("(c p) n -> p c n", p=128)
                            k.dma("pool", [lambda e: e.dma_start(out=wkr[:, :, 0:64], in_=wv_[:, :, 5632:5696]),
                                           lambda e: e.dma_start(out=wkr[:, :, 64:96], in_=wv_[:, :, 5664:5696]),
                                           lambda e: e.dma_start(out=wkr[:, :, 96:128], in_=wv_[:, :, 5632:5664])], dst=wkr)
                            gq = sl.sb("gq", [128, 4], F32, dma=True)
                            gkv = sl.sb("gkv", [128, 4], F32, dma=True)
                            k.dma("sp", lambda e: e.dma_start(out=gq[:], in_=Wl["q_norm_g"].rearrange("(c p) -> p c", p=128), allow_slow_non_contiguous=True), dst=gq)
                            k.dma("sp", lambda e: e.dma_start(out=gkv[:], in_=Wl["kv_norm_g"].rearrange("(c p) -> p c", p=128), allow_slow_non_contiguous=True), dst=gkv)
                            latf = sl.sb("latf", [128, 4, 512], F32)
                            sq = sl.sb("sq", [128, 4, 512], F32)
                            rs = sl.sb("rs", [128, 512], F32)
                            t1 = sl.sb("t1", [64, 512], F32)
                            t2 = sl.sb("t2", [64, 512], F32)
                            lps = [sl.ps(f"lps{i}", [128, 512]) for i in range(4)]
                            sps_ = sl.ps("ssq", [128, 512])
                            psr = sl.ps("psr", [64, 512])
                            pss = sl.ps("pss", [64, 512])
                            for (w_, gv, dst) in ((wcq, gq, cqn), (wckv, gkv, ckvn)):
                                for tb_ in range(4):
                                    blk = slice(tb_ * 512, (tb_ + 1) * 512)
                                    for c4 in range(4):
                                        for c in range(16):
                                            k.op("pe", lambda e, c4=c4, c=c, w_=w_, blk=blk: e.matmul(lps[c4][:], w_[:, c, c4 * 128:(c4 + 1) * 128], hT[:, c, blk], start=(c == 0), stop=(c == 15)),
                                                 reads=[w_, hT], writes=[lps[c4]])
                                        k.op("act", lambda e, c4=c4: e.copy(latf[:, c4, :], lps[c4][:]), reads=[lps[c4]], writes=[latf])
                                        k.op("act", lambda e, c4=c4: e.activation(sq[:, c4, :], lps[c4][:], AF.Square), reads=[lps[c4]], writes=[sq])
                                    for c4 in range(4):
                                        k.op("pe", lambda e, c4=c4: e.matmul(sps_[:], onesf[:], sq[:, c4, :], start=(c4 == 0), stop=(c4 == 3)),
                                             reads=[onesf, sq], writes=[sps_])
                                    k.op("act", lambda e: e.activation(rs[:], sps_[:], AF.Sqrt, bias=epsD[:, 0:1], scale=1.0 / 512.0), reads=[sps_, epsD], writes=[rs])
                                    k.op("dve", lambda e: e.reciprocal(rs[:], rs[:]), reads=[rs], writes=[rs])
                                    for c4 in range(4):
                                        k.op("dve", lambda e, c4=c4, gv=gv, dst=dst, blk=blk: e.scalar_tensor_tensor(dst[:, c4, blk], latf[:, c4, :], gv[:, c4:c4 + 1], rs[:], ALU.mult, ALU.mult),
                                             reads=[latf, gv, rs], writes=[dst])
                            for tb_ in range(4):
                                blk = slice(tb_ * 512, (tb_ + 1) * 512)
                                for c in range(16):
                                    k.op("pe", lambda e, c=c, blk=blk: e.matmul(psr[:], wkr[:, c, 0:64], hT[:, c, blk], start=(c == 0), stop=(c == 15)), reads=[wkr, hT], writes=[psr])
                                for c in range(16):
                                    k.op("pe", lambda e, c=c, blk=blk: e.matmul(pss[:], wkr[:, c, 64:128], hT[:, c, blk], start=(c == 0), stop=(c == 15)), reads=[wkr, hT], writes=[pss])
                                k.op("dve", lambda e, blk=blk: e.tensor_tensor(t1[:], psr[:], cosk[:, blk], ALU.mult), reads=[psr, cosk], writes=[t1])
                                k.op("dve", lambda e, blk=blk: e.tensor_tensor(t2[:], pss[:], sink[:, blk], ALU.mult), reads=[pss, sink], writes=[t2])
                                k.op("dve", lambda e, blk=blk: e.tensor_tensor(krT[:, blk], t1[:], t2[:], ALU.add), reads=[t1, t2], writes=[krT])

                        with k.scope() as sgt:
                            wg = [sgt.sb(f"wg{i}", [128, 16, 512], BF16, dma=True) for i in range(2)]
                            gsb = [sgt.sb(f"gsb{i}", [128, 512], BF16) for i in range(2)]
                            gps = [sgt.ps(f"gps{i}", [128, 512]) for i in range(2)]
                            m = 0
                            for n in range(8):
                                wi = wg[n % 2]
                                wload(wi, win[:, 5696 + n * 512:5696 + (n + 1) * 512], 512)
                                for tt in range(16):
                                    i = m % 2
                                    m += 1
                                    for c in range(16):
                                        k.op("pe", lambda e, i=i, c=c, tt=tt, wi=wi: e.matmul(gps[i][:], hT[:, c, tt * 128:(tt + 1) * 128], wi[:, c, :], start=(c == 0), stop=(c == 15)),
                                             reads=[hT, wi], writes=[gps[i]])
                                    k.op("act", lambda e, i=i: e.activation(gsb[i][:], gps[i][:], AF.Sigmoid), reads=[gps[i]], writes=[gsb[i]])
                                    r0 = b * S + tt * 128
                                    k.dma("sp", lambda e, i=i, r0=r0, n=n: e.dma_start(out=gD[r0:r0 + 128, n * 512:(n + 1) * 512], in_=gsb[i][:]), reads=[gsb[i]], dst=r_g)

                    with k.scope() as sa:
                        cosq = sa.sb("cosq", [64, S], F32)
                        sinq = sa.sb("sinq", [64, S], F32)
                        with k.scope() as srt:
                            rope_tables(srt, b, cosq, sinq, 192.0 ** -0.5)
                        wuq = sa.sb("wuq", [128, 4, 1536], BF16, dma=True)
                        wuqs = sa.sb("wuqs", [128, 4, 8, 64], BF16, dma=True)
                        wukv = sa.sb("wukv", [128, 4, 2048], BF16, dma=True)
                        wload(wuq, Wl["w_uq"], 1536)
                        wload(wukv, Wl["w_ukv"], 2048)
                        uqv = Wl["w_uq"].rearrange("(c p) (h x) -> p c h x", p=128, x=192)
                        k.dma("pool", [lambda e, c=c: e.dma_start(out=wuqs[:, c, :, 0:32], in_=uqv[:, c, :, 160:192]) for c in range(4)]
                              + [lambda e, c=c: e.dma_start(out=wuqs[:, c, :, 32:64], in_=uqv[:, c, :, 128:160]) for c in range(4)], dst=wuqs)
                        wukv_h = wukv[:, :, :].rearrange("p c (h x) -> p c h x", x=256)
                        for hh in range(2):
                            with k.scope() as sh2:
                                qnT = sh2.sb("qnT", [128, 4, S], BF16)
                                qrT = sh2.sb("qrT", [64, 4, S], BF16)
                                knT = sh2.sb("knT", [128, 4, S], BF16)
                                Vb = sh2.sb("Vb", [128, 16, 512], BF16)
                                with k.scope() as sp_:
                                    pn = [sp_.ps(f"pn{i}", [128, 512]) for i in range(2)]
                                    pr = sp_.ps("pr", [64, 512])
                                    pz = sp_.ps("pz", [64, 512])
                                    t1 = sp_.sb("t1", [64, 512], F32)
                                    t2 = sp_.sb("t2", [64, 512], F32)
                                    n = 0
                                    for hl in range(4):
                                        h = hh * 4 + hl
                                        for tb_ in range(4):
                                            blk = slice(tb_ * 512, (tb_ + 1) * 512)
                                            ps = pn[n % 2]
                                            n += 1
                                            for c in range(4):
                                                k.op("pe", lambda e, ps=ps, c=c, h=h, blk=blk: e.matmul(ps[:], wuq[:, c, h * 192:h * 192 + 128], cqn[:, c, blk], start=(c == 0), stop=(c == 3)),
                                                     reads=[wuq, cqn], writes=[ps])
                                            evac(qnT[:, hl, blk], ps[:], [ps], [qnT], scale=192.0 ** -0.5)
                                            for c in range(4):
                                                k.op("pe", lambda e, c=c, h=h, blk=blk: e.matmul(pr[:], wuq[:, c, h * 192 + 128:h * 192 + 192], cqn[:, c, blk], start=(c == 0), stop=(c == 3)),
                                                     reads=[wuq, cqn], writes=[pr])
                                            for c in range(4):
                                                k.op("pe", lambda e, c=c, h=h, blk=blk: e.matmul(pz[:], wuqs[:, c, h, :], cqn[:, c, blk], start=(c == 0), stop=(c == 3)),
                                                     reads=[wuqs, cqn], writes=[pz])
                                            k.op("dve", lambda e, blk=blk: e.tensor_tensor(t1[:], pr[:], cosq[:, blk], ALU.mult), reads=[pr, cosq], writes=[t1])
                                            k.op("dve", lambda e, blk=blk: e.tensor_tensor(t2[:], pz[:], sinq[:, blk], ALU.mult), reads=[pz, sinq], writes=[t2])
                                            k.op("dve", lambda e, blk=blk, hl=hl: e.tensor_tensor(qrT[:, hl, blk], t1[:], t2[:], ALU.add), reads=[t1, t2], writes=[qrT])
                                            ps = pn[n % 2]
                                            n += 1
                                            for c in range(4):
                                                k.op("pe", lambda e, ps=ps, c=c, h=h, blk=blk: e.matmul(ps[:], wukv[:, c, h * 256:h * 256 + 128], ckvn[:, c, blk], start=(c == 0), stop=(c == 3)),
                                                     reads=[wukv, ckvn], writes=[ps])
                                            evac(knT[:, hl, blk], ps[:], [ps], [knT])
                                    for tt in range(16):
                                        ps = pn[n % 2]
                                        n += 1
                                        for c in range(4):
                                            k.op("pe", lambda e, ps=ps, c=c, tt=tt: e.matmul(ps[:].rearrange("p (h x) -> p h x", h=4), ckvn[:, c, tt * 128:(tt + 1) * 128],
                                                                                          wukv_h[:, c, hh * 4:(hh + 1) * 4, 128:256], start=(c == 0), stop=(c == 3)),
                                                 reads=[wukv, ckvn], writes=[ps])
                                        evac(Vb[:, tt, :], ps[:], [ps], [Vb])
                                with k.scope() as sat:
                                    Sps = sat.ps("Sps", [128, S])
                                    ptp = [sat.ps(f"ptp{i}", [128, 8, 128], BF16) for i in range(2)]
                                    ops = sat.ps("ops", [128, 512])
                                    P = [sat.sb(f"P{i}", [128, S], BF16) for i in range(2)]
                                    pts = [sat.sb(f"pts{i}", [128, 16, 128], BF16) for i in range(2)]
                                    yb = [sat.sb(f"yb{i}", [128, 512], BF16) for i in range(2)]
                                    sm_ = [[sat.sb(f"sm{i}_{j}", [128, 1], F32) for j in range(4)] for i in range(2)]
                                    for qt in range(16):
                                        qs = slice(qt * 128, (qt + 1) * 128)
                                        for hl in range(4):
                                            ih = hl % 2
                                            mx, nmx, ll, rl = sm_[ih]
                                            for nk in range(4):
                                                ks = slice(nk * 512, (nk + 1) * 512)
                                                k.op("pe", lambda e, hl=hl, qs=qs, ks=ks: e.matmul(Sps[:, ks], qnT[:, hl, qs], knT[:, hl, ks], start=True, stop=False),
                                                     reads=[qnT, knT], writes=[Sps])
                                                k.op("pe", lambda e, hl=hl, qs=qs, ks=ks: e.matmul(Sps[:, ks], qrT[:, hl, qs], krT[:, ks], start=False, stop=True),
                                                     reads=[qrT, krT], writes=[Sps])
                                            k.op("dve", lambda e, mx=mx: e.reduce_max(mx[:], Sps[:], AX.X), reads=[Sps], writes=[mx])
                                            k.op("dve", lambda e, mx=mx, nmx=nmx: e.tensor_scalar(nmx[:], mx[:], -1.0, None, ALU.mult), reads=[mx], writes=[nmx])
                                            k.op("act", lambda e, ih=ih, nmx=nmx, ll=ll: e.activation(P[ih][:], Sps[:], AF.Exp, bias=nmx[:, 0:1], scale=1.0, accum_out=ll[:, 0:1]),
                                                 reads=[Sps, nmx], writes=[P[ih], ll])
                                            for hf in range(2):
                                                for j in range(8):
                                                    c = hf * 8 + j
                                                    k.op("pe", lambda e, ih=ih, hf=hf, j=j, c=c: e.transpose(ptp[hf][:, j, :], P[ih][:, c * 128:(c + 1) * 128], ident[:]),
                                                         reads=[P[ih], ident], writes=[ptp[hf]])
                                                evac(pts[ih][:, hf * 8:(hf + 1) * 8, :], ptp[hf][:], [ptp[hf]], [pts[ih]])
                                            for c in range(16):
                                                k.op("pe", lambda e, ih=ih, c=c, hl=hl: e.matmul(ops[:, hl * 128:(hl + 1) * 128], pts[ih][:, c, :], Vb[:, c, hl * 128:(hl + 1) * 128], start=(c == 0), stop=(c == 15)),
                                                     reads=[pts[ih], Vb], writes=[ops])
                                            k.op("dve", lambda e, ll=ll, rl=rl: e.reciprocal(rl[:], ll[:]), reads=[ll], writes=[rl])
                                            iy = qt % 2
                                            k.op("dve", lambda e, iy=iy, hl=hl, rl=rl: e.tensor_scalar(yb[iy][:, hl * 128:(hl + 1) * 128], ops[:, hl * 128:(hl + 1) * 128], rl[:, 0:1], None, ALU.mult),
                                                 reads=[ops, rl], writes=[yb[iy]])
                                        r0 = b * S + qt * 128
                                        k.dma("sp", lambda e, iy=iy, r0=r0: e.dma_start(out=ybD[r0:r0 + 128, hh * 512:(hh + 1) * 512], in_=yb[iy][:]), reads=[yb[iy]], dst=r_yb)

            def merge_layer(l, xin, r_xin):
                Wl = W[l]
                with k.scope() as sm:
                    wau = sm.sb("wau", [128, 4, D], BF16, dma=True)
                    wbu = sm.sb("wbu", [128, 8, D], BF16, dma=True)
                    wo = sm.sb("wo", [128, 16, D], BF16, dma=True)
                    wload(wau, Wl["w_a_up"], D)
                    wload(wbu, Wl["w_b_up"], D, 2)
                    wload(wo, Wl["w_o"], D, 4)
                    o_t = [sm.sb(f"o{g}", [128, 512], F32, dma=True) for g in range(3)]
                    ls = sm.sb("ls", [128, 3, 4], F32, dma=True)
                    ybt = sm.sb("ybt", [128, 1024], BF16, dma=True)
                    gt = sm.sb("gt", [128, 4096], BF16, dma=True)
                    xt = sm.sb("xt", [128, D], F32, dma=True)
                    gp = sm.sb("gp", [128, D], F32, dma=True)
                    mm = sm.sb("mm", [128, 4], F32)
                    ee = sm.sb("ee", [128, 3, 4], F32)
                    den = sm.sb("den", [128, 4], F32)
                    yaf = sm.sb("yaf", [128, 512], F32)
                    tmp = sm.sb("tmp", [128, 512], F32)
                    tmp2 = sm.sb("tmp2", [128, 512], F32)
                    ya = sm.sb("ya", [128, 512], BF16)
                    yaT = sm.sb("yaT", [128, 4, 128], BF16)
                    ybT = sm.sb("ybT", [128, 8, 128], BF16)
                    mg = sm.sb("mg", [128, D], BF16)
                    mT = sm.sb("mT", [128, 16, 128], BF16)
                    xn = sm.sb("xn", [128, D], F32)
                    ptp = [sm.ps(f"ptp{i}", [128, 8, 128], BF16) for i in range(2)]
                    ua = sm.ps("ua", [128, 512])
                    ub = sm.ps("ub", [128, 512])
                    ops = [sm.ps(f"ops{i}", [128, 512]) for i in range(2)]
                    for t in range(NT):
                        b = t // 16
                        r0 = t * 128
                        if t % 16 == 0:
                            k.dma("sp", lambda e, b=b: e.dma_start(out=gp[:], in_=mod_row(l, b, 2).broadcast_to([128, D])), reads=[r_mod], dst=gp)
                            k.op("dve", lambda e: e.tensor_scalar(gp[:], gp[:], 1.0, None, ALU.add), reads=[gp], writes=[gp])
                        for g in range(3):
                            k.dma("sp", lambda e, g=g, r0=r0: e.dma_start(out=o_t[g][:], in_=oD[g][r0:r0 + 128, :]), reads=[r_o[g]], dst=o_t[g])
                        k.dma("sp", [lambda e, g=g, r0=r0: e.dma_start(out=ls[:, g, :], in_=lseD[g][r0:r0 + 128, :]) for g in range(3)], reads=r_lse, dst=ls)
                        k.dma("sp", lambda e, r0=r0: e.dma_start(out=ybt[:], in_=ybD[r0:r0 + 128, :]), reads=[r_yb], dst=ybt)
                        k.dma("sp", lambda e, r0=r0: e.dma_start(out=gt[:], in_=gD[r0:r0 + 128, :]), reads=[r_g], dst=gt)
                        k.dma("sp", lambda e, r0=r0: e.dma_start(out=xt[:], in_=xin[r0:r0 + 128, :]), reads=[r_xin] if r_xin else [], dst=xt)
                        k.op("dve", lambda e: e.tensor_tensor(mm[:], ls[:, 0, :], ls[:, 1, :], ALU.max), reads=[ls], writes=[mm])
                        k.op("dve", lambda e: e.tensor_tensor(mm[:], mm[:], ls[:, 2, :], ALU.max), reads=[mm, ls], writes=[mm])
                        for g in range(3):
                            k.op("dve", lambda e, g=g: e.tensor_tensor(ee[:, g, :], ls[:, g, :], mm[:], ALU.subtract), reads=[ls, mm], writes=[ee])
                        k.op("act", lambda e: e.activation(ee[:], ee[:], AF.Exp), reads=[ee], writes=[ee])
                        k.op("dve", lambda e: e.tensor_tensor(den[:], ee[:, 0, :], ee[:, 1, :], ALU.add), reads=[ee], writes=[den])
                        k.op("dve", lambda e: e.tensor_tensor(den[:], den[:], ee[:, 2, :], ALU.add), reads=[den, ee], writes=[den])
                        k.op("dve", lambda e: e.reciprocal(den[:], den[:]), reads=[den], writes=[den])
                        for g in range(3):
                            k.op("dve", lambda e, g=g: e.tensor_tensor(ee[:, g, :], ee[:, g, :], den[:], ALU.mult), reads=[ee, den], writes=[ee])
                        for h in range(4):
                            hs = slice(h * 128, (h + 1) * 128)
                            k.op("dve", lambda e, h=h, hs=hs: e.tensor_scalar(yaf[:, hs], o_t[0][:, hs], ee[:, 0, h:h + 1], None, ALU.mult), reads=[o_t[0], ee], writes=[yaf])
                            k.op("dve", lambda e, h=h, hs=hs: e.scalar_tensor_tensor(yaf[:, hs], o_t[1][:, hs], ee[:, 1, h:h + 1], yaf[:, hs], ALU.mult, ALU.add), reads=[o_t[1], ee, yaf], writes=[yaf])
                            k.op("dve", lambda e, h=h, hs=hs: e.scalar_tensor_tensor(ya[:, hs], o_t[2][:, hs], ee[:, 2, h:h + 1], yaf[:, hs], ALU.mult, ALU.add), reads=[o_t[2], ee, yaf], writes=[ya])
                        for j in range(4):
                            k.op("pe", lambda e, j=j: e.transpose(ptp[0][:, j, :], ya[:, j * 128:(j + 1) * 128], ident[:]), reads=[ya, ident], writes=[ptp[0]])
                        evac(yaT[:], ptp[0][:, 0:4, :], [ptp[0]], [yaT])
                        for j in range(8):
                            k.op("pe", lambda e, j=j: e.transpose(ptp[1][:, j, :], ybt[:, j * 128:(j + 1) * 128], ident[:]), reads=[ybt, ident], writes=[ptp[1]])
                        evac(ybT[:], ptp[1][:], [ptp[1]], [ybT])
                        for n in range(4):
                            ns = slice(n * 512, (n + 1) * 512)
                            for c in range(4):
                                k.op("pe", lambda e, c=c, ns=ns: e.matmul(ua[:], yaT[:, c, :], wau[:, c, ns], start=(c == 0), stop=(c == 3)), reads=[yaT, wau], writes=[ua])
                            for c in range(8):
                                k.op("pe", lambda e, c=c, ns=ns: e.matmul(ub[:], ybT[:, c, :], wbu[:, c, ns], start=(c == 0), stop=(c == 7)), reads=[ybT, wbu], writes=[ub])
                            k.op("dve", lambda e, ns=ns: e.tensor_tensor(tmp[:], ua[:], gt[:, ns], ALU.mult), reads=[ua, gt], writes=[tmp])
                            k.op("dve", lambda e, n=n: e.tensor_tensor(tmp2[:], ub[:], gt[:, 2048 + n * 512:2048 + (n + 1) * 512], ALU.mult), reads=[ub, gt], writes=[tmp2])
                            k.op("pool", lambda e, ns=ns: e.tensor_tensor(mg[:, ns], tmp[:], tmp2[:], ALU.add), reads=[tmp, tmp2], writes=[mg])
                        for hf in range(2):
                            for j in range(8):
                                c = hf * 8 + j
                                k.op("pe", lambda e, hf=hf, j=j, c=c: e.transpose(ptp[hf][:, j, :], mg[:, c * 128:(c + 1) * 128], ident[:]), reads=[mg, ident], writes=[ptp[hf]])
                            evac(mT[:, hf * 8:(hf + 1) * 8, :], ptp[hf][:], [ptp[hf]], [mT])
                        for n in range(4):
                            ns = slice(n * 512, (n + 1) * 512)
                            op_ = ops[n % 2]
                            for c in range(16):
                                k.op("pe", lambda e, c=c, ns=ns, op_=op_: e.matmul(op_[:], mT[:, c, :], wo[:, c, ns], start=(c == 0), stop=(c == 15)), reads=[mT, wo], writes=[op_])
                            k.op("dve", lambda e, ns=ns, op_=op_: e.tensor_tensor(xn[:, ns], op_[:], gp[:, ns], ALU.mult), reads=[op_, gp], writes=[xn])
                            k.op("pool", lambda e, ns=ns: e.tensor_tensor(xn[:, ns], xn[:, ns], xt[:, ns], ALU.add), reads=[xn, xt], writes=[xn])
                        k.dma("sp", lambda e, r0=r0: e.dma_start(out=xaD[r0:r0 + 128, :], in_=xn[:]), reads=[xn], dst=r_xa)

            def moe_round(l, rnd, last):
                Wl = W[l]
                t0 = rnd * (TR // 128)
                with k.scope() as smo:
                    sl_i = smo.sb("sl_i", [128, 32, 2], I32)
                    wts = smo.sb("wts", [128, 32, 2], F32)
                    with k.scope() as s1:
                        wr = s1.sb("wr", [128, 16, 72], F32, dma=True)
                        k.dma("sp", [lambda e: e.dma_start(out=wr[:, :, 0:8], in_=Wl["w_grp"].rearrange("(c p) n -> p c n", p=128)),
                                     lambda e: e.dma_start(out=wr[:, :, 8:72], in_=Wl["w_exp"].rearrange("(c p) n -> p c n", p=128))], dst=wr)
                        br = s1.sb("br", [128, 72], F32, dma=True)
                        k.dma("sp", [lambda e: e.dma_start(out=br[:, 0:8], in_=Wl["b_grp"].broadcast_to([128, 8])),
                                     lambda e: e.dma_start(out=br[:, 8:72], in_=Wl["b_exp"].broadcast_to([128, 64]))], dst=br)
                        A2 = s1.sb("A2", [128, D], F32, dma=True)
                        B2 = s1.sb("B2", [128, D], F32, dma=True)
                        G2 = bcast(s1, "G2", Wl["ln2_g"], D)
                        R = s1.sb("R", [128, 64], BF16)
                        k.op("pool", lambda e: e.memset(R[:], 0.0), writes=[R])
                        xt = [s1.sb(f"xt{i}", [128, D], F32, dma=True) for i in range(2)]
                        junk = s1.sb("junk", [128, D], F32)
                        h2f = s1.sb("h2f", [128, D], F32)
                        h2b = [s1.sb(f"h2b{i}", [128, D], BF16) for i in range(2)]
                        h2T = s1.sb("h2T", [128, 16, 128], F32)
                        st = s1.sb("st", [128, 2], F32)
                        lg = s1.sb("lg", [128, 72], F32)
                        m8 = s1.sb("m8", [128, 8], F32)
                        s8 = s1.sb("s8", [128, 8], F32)
                        sc_ = s1.sb("sc_", [128, 16], F32)
                        eg = s1.sb("eg", [128, 8], F32)
                        Gm = s1.sb("Gm", [128, 8], F32)
                        lm = s1.sb("lm", [128, 64], F32)
                        E1 = s1.sb("E1", [128, 64], F32)
                        E2 = s1.sb("E2", [128, 64], F32)
                        Ab = s1.sb("Ab", [128, 64], BF16)
                        cnt = s1.sb("cnt", [128, 64], F32)
                        tq = s1.sb("tq", [128, 64], F32)
                        slf = s1.sb("slf", [128, 2], F32)
                        ptf = [s1.ps(f"ptf{i}", [128, 4, 128], F32) for i in range(2)]
                        lps = s1.ps("lps", [128, 72])
                        cps = s1.ps("cps", [128, 64])
                        for tl in range(TR // 128):
                            t = t0 + tl
                            b = t // 16
                            i = tl % 2
                            r0 = t * 128
                            if t % 16 == 0:
                                k.dma("sp", lambda e, b=b: e.dma_start(out=A2[:], in_=mod_row(l, b, 4).broadcast_to([128, D])), reads=[r_mod], dst=A2)
                                k.dma("sp", lambda e, b=b: e.dma_start(out=B2[:], in_=mod_row(l, b, 3).broadcast_to([128, D])), reads=[r_mod], dst=B2)
                                k.op("dve", lambda e: e.scalar_tensor_tensor(A2[:], A2[:], 1.0, G2[:], ALU.add, ALU.mult), reads=[A2, G2], writes=[A2])
                            k.dma("sp", lambda e, i=i, r0=r0: e.dma_start(out=xt[i][:], in_=xaD[r0:r0 + 128, :]), reads=[r_xa], dst=xt[i])
                            ln_stats(cst, xt[i], junk, st)
                            k.op("dve", lambda e, i=i: e.scalar_tensor_tensor(junk[:], xt[i][:], st[:, 1:2], A2[:], ALU.mult, ALU.mult), reads=[xt[i], st, A2], writes=[junk])
                            k.op("dve", lambda e: e.tensor_tensor(h2f[:], junk[:], B2[:], ALU.add), reads=[junk, B2], writes=[h2f])
                            k.op("act", lambda e, i=i: e.copy(h2b[i][:], h2f[:]), reads=[h2f], writes=[h2b[i]])
                            for c4 in range(4):
                                pf = ptf[c4 % 2]
                                for j in range(4):
                                    c = c4 * 4 + j
                                    k.op("pe", lambda e, pf=pf, j=j, c=c: e.transpose(pf[:, j, :], h2f[:, c * 128:(c + 1) * 128], identf[:]), reads=[h2f, identf], writes=[pf])
                                evac(h2T[:, c4 * 4:(c4 + 1) * 4, :], pf[:], [pf], [h2T])
                            for c in range(16):
                                k.op("pe", lambda e, c=c: e.matmul(lps[:], h2T[:, c, :], wr[:, c, :], start=(c == 0), stop=(c == 15)), reads=[h2T, wr], writes=[lps])
                            k.op("dve", lambda e: e.tensor_tensor(lg[:], lps[:], br[:], ALU.add), reads=[lps, br], writes=[lg])
                            k.op("dve", lambda e: e.max(m8[:], lg[:, 0:8]), reads=[lg], writes=[m8])
                            k.op("dve", lambda e: e.tensor_scalar(sc_[:, 0:1], m8[:, 0:1], -1.0, None, ALU.mult), reads=[m8], writes=[sc_])
                            k.op("act", lambda e: e.activation(eg[:], lg[:, 0:8], AF.Exp, bias=sc_[:, 0:1], scale=1.0, accum_out=sc_[:, 1:2]), reads=[lg, sc_], writes=[eg, sc_])
                            k.op("dve", lambda e: e.reciprocal(sc_[:, 2:3], sc_[:, 1:2]), reads=[sc_], writes=[sc_])
                            k.op("dve", lambda e: e.tensor_scalar(Gm[:], lg[:, 0:8], m8[:, 0:1], None, ALU.is_equal), reads=[lg, m8], writes=[Gm])
                            k.op("dve", lambda e: e.tensor_scalar(eg[:], Gm[:], 1.0, BIG, ALU.subtract, ALU.mult), reads=[Gm], writes=[eg])
                            for g in range(8):
                                k.op("dve", lambda e, g=g: e.tensor_scalar(lm[:, g * 8:(g + 1) * 8], lg[:, 8 + g * 8:16 + g * 8], eg[:, g:g + 1], None, ALU.add), reads=[lg, eg], writes=[lm])
                            k.op("dve", lambda e: e.max(s8[:], lm[:]), reads=[lm], writes=[s8])
                            k.op("dve", lambda e: e.tensor_scalar(E1[:], lm[:], s8[:, 0:1], None, ALU.is_equal), reads=[lm, s8], writes=[E1])
                            k.op("dve", lambda e: e.tensor_scalar(E2[:], lm[:], s8[:, 1:2], None, ALU.is_equal), reads=[lm, s8], writes=[E2])
                            k.op("dve", lambda e: e.tensor_scalar(sc_[:, 3:4], s8[:, 0:1], -1.0, None, ALU.mult), reads=[s8], writes=[sc_])
                            k.op("act", lambda e: e.activation(sc_[:, 4:5], s8[:, 1:2], AF.Exp, bias=sc_[:, 3:4], scale=1.0), reads=[s8, sc_], writes=[sc_])
                            k.op("dve", lambda e: e.tensor_scalar(sc_[:, 5:6], sc_[:, 4:5], 1.0, None, ALU.add), reads=[sc_], writes=[sc_])
                            k.op("dve", lambda e: e.reciprocal(sc_[:, 5:6], sc_[:, 5:6]), reads=[sc_], writes=[sc_])
                            k.op("dve", lambda e: e.tensor_tensor(sc_[:, 6:7], sc_[:, 2:3], sc_[:, 5:6], ALU.mult), reads=[sc_], writes=[sc_])
                            k.op("dve", lambda e: e.tensor_tensor(sc_[:, 7:8], sc_[:, 6:7], sc_[:, 4:5], ALU.mult), reads=[sc_], writes=[sc_])
                            k.op("dve", lambda e: e.tensor_tensor(Ab[:], E1[:], E2[:], ALU.add), reads=[E1, E2], writes=[Ab])
                            k.op("pe", lambda e: e.matmul(cps[:], LT[:], Ab[:], start=True, stop=False), reads=[LT, Ab], writes=[cps])
                            k.op("pe", lambda e: e.matmul(cps[:], onesb[:], R[:], start=False, stop=True), reads=[onesb, R], writes=[cps])
                            k.op("dve", lambda e: e.tensor_copy(cnt[:], cps[:]), reads=[cps], writes=[cnt])
                            k.op("pool", lambda e: e.tensor_tensor(R[:], R[:], Ab[:], ALU.add), reads=[R, Ab], writes=[R])
                            for kk_, Ek in ((0, E1), (1, E2)):
                                k.op("dve", lambda e, Ek=Ek: e.tensor_tensor(tq[:], Ek[:], cnt[:], ALU.mult), reads=[Ek, cnt], writes=[tq])
                                k.op("dve", lambda e, kk_=kk_: e.reduce_sum(sc_[:, 8 + kk_:9 + kk_], tq[:], AX.X), reads=[tq], writes=[sc_])
                                k.op("dve", lambda e, Ek=Ek: e.tensor_tensor(tq[:], Ek[:], eC[:], ALU.mult), reads=[Ek, eC], writes=[tq])
                                k.op("dve", lambda e, kk_=kk_: e.reduce_sum(sc_[:, 10 + kk_:11 + kk_], tq[:], AX.X), reads=[tq], writes=[sc_])
                                k.op("dve", lambda e, kk_=kk_: e.tensor_scalar(sc_[:, 12 + kk_:13 + kk_], sc_[:, 8 + kk_:9 + kk_], float(C_CAP), None, ALU.is_lt), reads=[sc_], writes=[sc_])
                                k.op("dve", lambda e, kk_=kk_: e.tensor_tensor(sc_[:, 8 + kk_:9 + kk_], sc_[:, 8 + kk_:9 + kk_], sc_[:, 10 + kk_:11 + kk_], ALU.add), reads=[sc_], writes=[sc_])
                                k.op("dve", lambda e, kk_=kk_: e.tensor_scalar(sc_[:, 8 + kk_:9 + kk_], sc_[:, 8 + kk_:9 + kk_], float(-NSLOT), None, ALU.add), reads=[sc_], writes=[sc_])
                                k.op("dve", lambda e, kk_=kk_: e.tensor_tensor(sc_[:, 8 + kk_:9 + kk_], sc_[:, 8 + kk_:9 + kk_], sc_[:, 12 + kk_:13 + kk_], ALU.mult), reads=[sc_], writes=[sc_])
                                k.op("dve", lambda e, kk_=kk_: e.tensor_scalar(slf[:, kk_:kk_ + 1], sc_[:, 8 + kk_:9 + kk_], float(NSLOT), None, ALU.add), reads=[sc_], writes=[slf])
                                k.op("dve", lambda e, kk_=kk_, tl=tl: e.tensor_tensor(wts[:, tl, kk_:kk_ + 1], sc_[:, 6 + kk_:7 + kk_], sc_[:, 12 + kk_:13 + kk_], ALU.mult), reads=[sc_], writes=[wts])
                            k.op("dve", lambda e, tl=tl: e.tensor_copy(sl_i[:, tl, :], slf[:]), reads=[slf], writes=[sl_i])
                            for kk_ in range(2):
                                k.dma("pool", lambda e, i=i, tl=tl, kk_=kk_: e.indirect_dma_start(
                                    out=XsD[:, :], out_offset=bass.IndirectOffsetOnAxis(ap=sl_i[:, tl, kk_:kk_ + 1], axis=0),
                                    in_=h2b[i][:, :], in_offset=None), reads=[h2b[i], sl_i], dst=r_xs)
                        if "slotD" in ext:
                            k.dma("sp", [lambda e: e.dma_start(out=slotD[t0 * 128:t0 * 128 + TR, 0:2].rearrange("(t p) k -> p t k", p=128), in_=wts[:], allow_slow_non_contiguous=True)],
                                  reads=[wts], dst=r_slot)

                    with k.scope() as s2:
                        wgu = [s2.sb(f"wgu{i}", [128, 16, 1024], BF16, dma=True) for i in range(2)]
                        wd = [s2.sb(f"wd{i}", [128, 4, D], BF16, dma=True) for i in range(2)]
                        xsl = [s2.sb(f"xsl{i}", [128, 2, D], BF16, dma=True) for i in range(2)]
                        xT = s2.sb("xT", [128, 16, 256], BF16)
                        sg_ = s2.sb("sg_", [128, 256], F32)
                        aT = s2.sb("aT", [128, 4, 256], BF16)
                        ysb = [s2.sb(f"ysb{i}", [128, D], F32) for i in range(2)]
                        ptp = [s2.ps(f"ptp{i}", [128, 8, 128], BF16) for i in range(2)]
                        gps = s2.ps("gps", [128, 256])
                        ups = s2.ps("ups", [128, 256])
                        yps = [s2.ps(f"yps{i}", [128, 512]) for i in range(2)]
                        for ex in range(NE):
                            i = ex % 2
                            gi, ei = ex // 8, ex % 8
                            wload(wgu[i], Wl["w_gu"][gi][ei], 1024, 4)
                            wload(wd[i], Wl["w_down"][gi][ei], D, 2)
                            s0 = ex * C_CAP
                            k.dma("sp", [lambda e, i=i, s=s, s0=s0: e.dma_start(out=xsl[i][:, s, :], in_=XsD[s0 + s * 128:s0 + (s + 1) * 128, :]) for s in range(2)],
                                  reads=[r_xs], dst=xsl[i])
                            for s in range(2):
                                for hf in range(2):
                                    for j in range(8):
                                        c = hf * 8 + j
                                        k.op("pe", lambda e, i=i, s=s, hf=hf, j=j, c=c: e.transpose(ptp[hf][:, j, :], xsl[i][:, s, c * 128:(c + 1) * 128], ident[:]),
                                             reads=[xsl[i], ident], writes=[ptp[hf]])
                                    evac(xT[:, hf * 8:(hf + 1) * 8, s * 128:(s + 1) * 128], ptp[hf][:], [ptp[hf]], [xT])
                            for j in range(4):
                                for c in range(16):
                                    k.op("pe", lambda e, i=i, j=j, c=c: e.matmul(gps[:], wgu[i][:, c, j * 128:(j + 1) * 128], xT[:, c, :], start=(c == 0), stop=(c == 15)),
                                         reads=[wgu[i], xT], writes=[gps])
                                for c in range(16):
                                    k.op("pe", lambda e, i=i, j=j, c=c: e.matmul(ups[:], wgu[i][:, c, 512 + j * 128:512 + (j + 1) * 128], xT[:, c, :], start=(c == 0), stop=(c == 15)),
                                         reads=[wgu[i], xT], writes=[ups])
                                k.op("act", lambda e: e.activation(sg_[:], gps[:], AF.Silu), reads=[gps], writes=[sg_])
                                k.op("dve", lambda e, j=j: e.tensor_tensor(aT[:, j, :], sg_[:], ups[:], ALU.mult), reads=[sg_, ups], writes=[aT])
                            for s in range(2):
                                for n in range(4):
                                    yp = yps[n % 2]
                                    for j in range(4):
                                        k.op("pe", lambda e, i=i, s=s, n=n, j=j, yp=yp: e.matmul(yp[:], aT[:, j, s * 128:(s + 1) * 128], wd[i][:, j, n * 512:(n + 1) * 512], start=(j == 0), stop=(j == 3)),
                                             reads=[aT, wd[i]], writes=[yp])
                                    evac(ysb[s][:, n * 512:(n + 1) * 512], yp[:], [yp], [ysb[s]])
                                k.dma("sp", lambda e, s=s, s0=s0: e.dma_start(out=YsD[s0 + s * 128:s0 + (s + 1) * 128, :], in_=ysb[s][:]), reads=[ysb[s]], dst=r_ys)

                    with k.scope() as s3:
                        gp = s3.sb("gp", [128, D], F32, dma=True)
                        y1 = [s3.sb(f"y1_{i}", [128, D], F32, dma=True) for i in range(2)]
                        y2 = [s3.sb(f"y2_{i}", [128, D], F32, dma=True) for i in range(2)]
                        xt = [s3.sb(f"xt{i}", [128, D], F32, dma=True) for i in range(2)]
                        xn = [s3.sb(f"xn{i}", [128, D], F32) for i in range(2)]
                        junk = s3.sb("junk", [128, D], F32)
                        st = s3.sb("st", [128, 2], F32)
                        fg = bcast(s3, "fg", fin_g, D) if last else None
                        for tl in range(TR // 128):
                            t = t0 + tl
                            b = t // 16
                            i = tl % 2
                            r0 = t * 128
                            if t % 16 == 0:
                                k.dma("sp", lambda e, b=b: e.dma_start(out=gp[:], in_=mod_row(l, b, 5).broadcast_to([128, D])), reads=[r_mod], dst=gp)
                                k.op("dve", lambda e: e.tensor_scalar(gp[:], gp[:], 1.0, None, ALU.add), reads=[gp], writes=[gp])
                            for kk_, yy in ((0, y1[i]), (1, y2[i])):
                                k.dma("pool", lambda e, yy=yy, tl=tl, kk_=kk_: e.indirect_dma_start(
                                    out=yy[:, :], out_offset=None, in_=YsD[:, :],
                                    in_offset=bass.IndirectOffsetOnAxis(ap=sl_i[:, tl, kk_:kk_ + 1], axis=0)), reads=[r_ys, sl_i], dst=yy)
                            k.dma("sp", lambda e, i=i, r0=r0: e.dma_start(out=xt[i][:], in_=xaD[r0:r0 + 128, :]), reads=[r_xa], dst=xt[i])
                            k.op("dve", lambda e, i=i, tl=tl: e.tensor_scalar(y1[i][:], y1[i][:], wts[:, tl, 0:1], None, ALU.mult), reads=[y1[i], wts], writes=[y1[i]])
                            k.op("dve", lambda e, i=i, tl=tl: e.scalar_tensor_tensor(y1[i][:], y2[i][:], wts[:, tl, 1:2], y1[i][:], ALU.mult, ALU.add), reads=[y1[i], y2[i], wts], writes=[y1[i]])
                            k.op("pool", lambda e, i=i: e.tensor_tensor(y1[i][:], y1[i][:], gp[:], ALU.mult), reads=[y1[i], gp], writes=[y1[i]])
                            k.op("dve", lambda e, i=i: e.tensor_tensor(xn[i][:], y1[i][:], xt[i][:], ALU.add), reads=[y1[i], xt[i]], writes=[xn[i]])
                            if last:
                                ln_stats(cst, xn[i], junk, st)
                                k.op("dve", lambda e, i=i: e.scalar_tensor_tensor(xn[i][:], xn[i][:], st[:, 1:2], fg[:], ALU.mult, ALU.mult), reads=[xn[i], st, fg], writes=[xn[i]])
                                k.dma("sp", lambda e, i=i, r0=r0: e.dma_start(out=out_d[r0:r0 + 128, :], in_=xn[i][:]), reads=[xn[i]], dst=r_out)
                            else:
                                k.dma("sp", lambda e, i=i, r0=r0: e.dma_start(out=xbD[r0:r0 + 128, :], in_=xn[i][:]), reads=[xn[i]], dst=r_xb)

            xin, r_xin = x_d, None
            for l in range(nl_attn):
                for b in range(NB):
                    attn_seq(l, b, xin, r_xin)
                merge_layer(l, xin, r_xin)
                if l < nl_moe:
                    last = (stop is None and l == NL - 1)
                    for rnd in range(NR):
                        moe_round(l, rnd, last)
                    xin, r_xin = xbD, r_xb
            if stop is not None:
                with k.scope() as sd:
                    t_ = sd.sb("dbgt", [128, D], F32, dma=True)
                    src, rs_ = (modD, r_mod) if stop == "mod" else ((xaD, r_xa) if stop == "attn" else (xbD, r_xb))
                    if stop == "mod":
                        k.dma("sp", lambda e: e.dma_start(out=t_[0:2 * NB, :], in_=modD.rearrange("l b (j n) -> (l b) j n", n=D)[:, 0, :]), reads=[r_mod], dst=t_)
                        k.dma("sp", lambda e: e.dma_start(out=out_d[0:2 * NB, :], in_=t_[0:2 * NB, :]), reads=[t_], dst=r_out)
                    else:
                        for t in range(NT):
                            k.dma("sp", lambda e, t=t: e.dma_start(out=t_[:], in_=src[t * 128:(t + 1) * 128, :]), reads=[rs_], dst=t_)
                            k.dma("sp", lambda e, t=t: e.dma_start(out=out_d[t * 128:(t + 1) * 128, :], in_=t_[:]), reads=[t_], dst=r_out)
        k.barrier()
    return nc


def make_in_maps(inputs, n_cores, NB, NL=2, NG=8, stop=None):
    f = lambda a: np.ascontiguousarray(a)
    shared = {}
    run_attn = stop != "mod"
    run_moe = stop not in ("mod", "attn")
    for l in range(NL if stop is None else 1):
        shared[f"w_ada_{l}"] = f(inputs["w_ada"][l])
        shared[f"b_ada_{l}"] = f(inputs["b_ada"][l][None])
        if run_attn:
            shared[f"ln1_g_{l}"] = f(inputs["ln1_g"][l][None])
            for n in ("w_in", "q_norm_g", "w_uq", "kv_norm_g", "w_ukv", "w_a_up", "w_b_up", "w_o"):
                shared[f"{n}_{l}"] = f(inputs[n][l])
        if run_moe:
            shared[f"ln2_g_{l}"] = f(inputs["ln2_g"][l][None])
            shared[f"w_grp_{l}"] = f(inputs["w_grp"][l])
            shared[f"b_grp_{l}"] = f(inputs["b_grp"][l][None])
            shared[f"w_exp_{l}"] = f(inputs["w_exp"][l])
            shared[f"b_exp_{l}"] = f(inputs["b_exp"][l][None])
            for g in range(NG):
                shared[f"w_gu_{l}_{g}"] = f(inputs["w_gu"][l][g * 8:(g + 1) * 8])
                shared[f"w_down_{l}_{g}"] = f(inputs["w_down"][l][g * 8:(g + 1) * 8])
    if stop is None:
        shared["final_g"] = f(inputs["final_g"][None])
    maps = []
    for c in range(n_cores):
        m = dict(shared)
        sl = slice(c * NB, (c + 1) * NB)
        m["x"] = f(inputs["x"][sl]).reshape(NB * S, D)
        m["c"] = f(inputs["c"][sl])
        m["positions"] = f(inputs["positions"][sl]).astype(np.int32)
        maps.append(m)
    return maps


def kernel(**inputs):
    inputs = {k_: np.asarray(v) for k_, v in inputs.items()}
    n = N_CORES
    NB = 16 // n
    nc = build(NB)
    maps = make_in_maps(inputs, n, NB)
    res = run_bass_kernel_spmd(nc, maps, core_ids=list(range(n)))
    out = np.concatenate([r["out"].reshape(NB, S, D) for r in res.results], axis=0)
    return out.astype(np.float32)
```

```python
import math
from contextlib import ExitStack, contextmanager

import numpy as np
import concourse.bass as bass
import concourse.mybir as mybir
from concourse.bass_utils import run_bass_kernel_spmd

F32 = mybir.dt.float32
BF16 = mybir.dt.bfloat16
I32 = mybir.dt.int32
ALU = mybir.AluOpType
AF = mybir.ActivationFunctionType
AX = mybir.AxisListType

S = 2048
D = 2048
NE_FULL = 64
C_CAP = 256
TR = 4096
EPS = 1e-6
BIG = 1.0e9
IN_COLS = 9792
N_CORES = 8
SAME_ENG_WAIT = True


class Res:
    def __init__(self, k, name, dma=False, multi=False):
        self.name = name
        self.w = None
        self.r = {}
        self.multi = multi
        self.sem = None
        self.cnt = 0
        if dma:
            self.sem, self.cnt = k.take_sem()
            k.live.append(self)


class Tl:
    def __init__(self, t, res):
        self.t = t
        self.res = res

    def __getitem__(self, i):
        return self.t[i]


class Eng:
    def __init__(self, k, name, h):
        self.name = name
        self.h = h
        self.sem = k.new_sem("e_" + name)
        self.cnt = 0
        self.waited = {}
        self.pend_r = []
        self.pend_w = []

    def wait(self, tok):
        if tok is None:
            return
        sem, val = tok
        if sem is self.sem and (self.name == "pe" or (self.name in ("act", "dve") and not SAME_ENG_WAIT)):
            return
        if self.waited.get(id(sem), 0) >= val:
            return
        self.waited[id(sem)] = val
        self.h.wait_ge(sem, val)


class Scope:
    def __init__(self, k):
        self.k = k
        self.stack = ExitStack()
        self.res = []

    def sb(self, name, shape, dt, dma=False):
        self.k.uid += 1
        t = self.stack.enter_context(self.k.nc.sbuf_tensor(f"{name}_{self.k.uid}", shape, dt))
        r = Res(self.k, name, dma)
        self.res.append(r)
        return Tl(t, r)

    def ps(self, name, shape, dt=F32):
        self.k.uid += 1
        t = self.stack.enter_context(self.k.nc.psum_tensor(f"{name}_{self.k.uid}", shape, dt))
        r = Res(self.k, name, False)
        self.res.append(r)
        return Tl(t, r)


class K:
    def __init__(self, nc, stack):
        self.nc = nc
        self.stack = stack
        self.free = []
        self.live = []
        self.uid = 0
        self.eng = {
            "pe": Eng(self, "pe", nc.tensor), "act": Eng(self, "act", nc.scalar),
            "dve": Eng(self, "dve", nc.vector), "pool": Eng(self, "pool", nc.gpsimd),
            "sp": Eng(self, "sp", nc.sync),
        }

    def new_sem(self, name):
        self.uid += 1
        self.nsem = getattr(self, "nsem", 0) + 1
        return self.stack.enter_context(self.nc.semaphore(f"{name}_{self.uid}"))

    def take_sem(self):
        while self.free:
            sem, cnt = self.free.pop()
            if cnt < 24000:
                return (sem, cnt)
        return (self.new_sem("d"), 0)

    def res(self, name, dma=True, multi=True):
        return Res(self, name, dma, multi)

    def barrier(self):
        toks = [(e.sem, e.cnt) for e in self.eng.values() if e.cnt]
        toks += [(r.sem, r.cnt) for r in self.live if r.cnt]
        for e in self.eng.values():
            for t in toks:
                e.wait(t)
        for e in self.eng.values():
            if e.cnt > 24000:
                e.sem = self.new_sem("e_" + e.name)
                e.cnt = 0

    @contextmanager
    def scope(self):
        sc = Scope(self)
        try:
            yield sc
        except BaseException:
            import traceback
            if not getattr(self, "_tb_done", False):
                traceback.print_exc()
                self._tb_done = True
            raise
        else:
            self.barrier()
            for r in sc.res:
                if r.sem is not None:
                    self.live.remove(r)
                    self.free.append((r.sem, r.cnt))
            sc.stack.close()

    @staticmethod
    def _r(x):
        return x if isinstance(x, Res) else x.res

    def _deps(self, e, reads, writes):
        for x in reads:
            e.wait(self._r(x).w)
        for x in writes:
            x = self._r(x)
            if not x.multi:
                e.wait(x.w)
            for tok in list(x.r.values()):
                e.wait(tok)

    def op(self, en, fn, reads=(), writes=(), inc=True):
        e = self.eng[en]
        self._deps(e, reads, writes)
        if not inc:
            fn(e.h)
            e.pend_r.extend(reads)
            e.pend_w.extend(writes)
            return None
        e.cnt += 1
        tok = (e.sem, e.cnt)
        fn(e.h).then_inc(e.sem, 1)
        for x in list(reads) + e.pend_r:
            self._r(x).r[id(e.sem)] = tok
        for x in list(writes) + e.pend_w:
            x = self._r(x)
            x.w = tok
            x.r = {}
        e.pend_r, e.pend_w = [], []
        return tok

    def dma(self, en, fns, reads=(), dst=None, inc=16):
        e = self.eng[en]
        d = self._r(dst)
        self._deps(e, reads, [d])
        if not isinstance(fns, (list, tuple)):
            fns = [fns]
        for fn in fns:
            d.cnt += inc
            fn(e.h).then_inc(d.sem, inc)
        tok = (d.sem, d.cnt)
        for x in reads:
            self._r(x).r[id(d.sem)] = tok
        d.w = tok
        if not d.multi:
            d.r = {}
        return tok


def build(NB, NL=2, NG=8, stop=None, ext=()):
    T = NB * S
    NT = T // 128
    NE = NG * 8
    NR = T // TR
    nc = bass.Bass("TRN2", target_bir_lowering=False)

    def din(name, shape, dt=F32):
        return nc.dram_tensor(name, shape, dt, kind="ExternalInput").ap()

    def dscr(name, shape, dt=F32):
        kind = "ExternalOutput" if name in ext else "Internal"
        return nc.dram_tensor(name, shape, dt, kind=kind).ap()

    run_attn = stop != "mod"
    run_moe = stop not in ("mod", "attn")
    nl_attn = NL if stop is None else (1 if run_attn else 0)
    nl_moe = NL if stop is None else (1 if run_moe else 0)

    x_d = din("x", [T, D])
    c_d = din("c", [NB, D])
    pos_d = din("positions", [NB, S], I32)
    W = {}
    for l in range(NL if stop is None else 1):
        W[l] = dict(
            w_ada=din(f"w_ada_{l}", [D, 6 * D]), b_ada=din(f"b_ada_{l}", [1, 6 * D]))
        if run_attn:
            W[l].update(
                ln1_g=din(f"ln1_g_{l}", [1, D]),
                w_in=din(f"w_in_{l}", [D, IN_COLS]),
                q_norm_g=din(f"q_norm_g_{l}", [512]), w_uq=din(f"w_uq_{l}", [512, 1536]),
                kv_norm_g=din(f"kv_norm_g_{l}", [512]), w_ukv=din(f"w_ukv_{l}", [512, 2048]),
                w_a_up=din(f"w_a_up_{l}", [512, D]), w_b_up=din(f"w_b_up_{l}", [1024, D]),
                w_o=din(f"w_o_{l}", [D, D]))
        if run_moe:
            W[l].update(
                ln2_g=din(f"ln2_g_{l}", [1, D]),
                w_grp=din(f"w_grp_{l}", [D, 8]), b_grp=din(f"b_grp_{l}", [1, 8]),
                w_exp=din(f"w_exp_{l}", [D, 64]), b_exp=din(f"b_exp_{l}", [1, 64]),
                w_gu=[din(f"w_gu_{l}_{g}", [8, D, 1024]) for g in range(NG)],
                w_down=[din(f"w_down_{l}_{g}", [8, 512, D]) for g in range(NG)])
    fin_g = din("final_g", [1, D]) if stop is None else None
    out_d = nc.dram_tensor("out", [T, D], F32, kind="ExternalOutput").ap()

    modD = dscr("modD", [2, NB, 6 * D])
    xaD = dscr("xaD", [T, D])
    xbD = dscr("xbD", [T, D])
    oD = [dscr(f"oD{g}", [T, 512]) for g in range(3)]
    lseD = [dscr(f"lseD{g}", [T, 4]) for g in range(3)]
    ybD = dscr("ybD", [T, 1024], BF16)
    gD = dscr("gD", [T, 4096], BF16)
    NSLOT = NE * C_CAP
    XsD = dscr("XsD", [NSLOT + 128, D], BF16)
    YsD = dscr("YsD", [NSLOT + 128, D])
    slotD = dscr("slotD", [T, 4])

    slopes = [2.0 ** (-8.0 * (n + 1) / 12.0) for n in range(12)]

    with ExitStack() as stack:
        k = K(nc, stack)
        r_mod = k.res("modD")
        r_xa = k.res("xaD")
        r_xb = k.res("xbD")
        r_o = [k.res(f"oD{g}") for g in range(3)]
        r_lse = [k.res(f"lseD{g}") for g in range(3)]
        r_yb = k.res("ybD")
        r_g = k.res("gD")
        r_xs = k.res("XsD")
        r_ys = k.res("YsD")
        r_out = k.res("out")
        r_slot = k.res("slotD")

        with k.scope() as g0:
            identf = g0.sb("identf", [128, 128], F32)
            ident = g0.sb("ident", [128, 128], BF16)
            onesf = g0.sb("onesf", [128, 128], F32)
            onesb = g0.sb("onesb", [128, 128], BF16)
            LTf = g0.sb("LTf", [128, 128], F32)
            LT = g0.sb("LT", [128, 128], BF16)
            mband = g0.sb("mband", [128, 384], F32)
            eCi = g0.sb("eCi", [128, 64], I32)
            eC = g0.sb("eC", [128, 64], F32)
            invi = g0.sb("invi", [64, 1], I32)
            inv = g0.sb("inv", [64, 1], F32)
            sgn = g0.sb("sgn", [64, 1], F32)

            k.op("pool", lambda e: e.memset(onesf[:], 1.0), writes=[onesf])
            k.op("pool", lambda e: e.memset(identf[:], 1.0), writes=[identf])
            k.op("pool", lambda e: e.affine_select(identf[:], identf[:], [[-1, 128]], ALU.is_equal, 0.0,
                                                    base=0, channel_multiplier=1), reads=[identf], writes=[identf])
            k.op("dve", lambda e: e.tensor_copy(ident[:], identf[:]), reads=[identf], writes=[ident])
            k.op("dve", lambda e: e.tensor_copy(onesb[:], onesf[:]), reads=[onesf], writes=[onesb])
            k.op("pool", lambda e: e.affine_select(LTf[:], onesf[:], [[1, 128]], ALU.is_ge, 0.0,
                                                    base=-1, channel_multiplier=-1), reads=[onesf], writes=[LTf])
            k.op("dve", lambda e: e.tensor_copy(LT[:], LTf[:]), reads=[LTf], writes=[LT])
            k.op("pool", lambda e: e.memset(mband[:], 0.0), writes=[mband])
            k.op("pool", lambda e: e.affine_select(mband[:], mband[:], [[1, 384]], ALU.is_ge, BIG,
                                                    base=-64, channel_multiplier=-1), reads=[mband], writes=[mband])
            k.op("pool", lambda e: e.affine_select(mband[:], mband[:], [[-1, 384]], ALU.is_ge, BIG,
                                                    base=192, channel_multiplier=1), reads=[mband], writes=[mband])
            k.op("pool", lambda e: e.iota(eCi[:], [[C_CAP, 64]], base=0, channel_multiplier=0), writes=[eCi])
            k.op("dve", lambda e: e.tensor_copy(eC[:], eCi[:]), reads=[eCi], writes=[eC])
            k.op("pool", lambda e: e.iota(invi[0:32, :], [[0, 1]], base=0, channel_multiplier=1), writes=[invi])
            k.op("pool", lambda e: e.iota(invi[32:64, :], [[0, 1]], base=0, channel_multiplier=1), writes=[invi])
            k.op("dve", lambda e: e.tensor_copy(inv[:], invi[:]), reads=[invi], writes=[inv])
            k.op("act", lambda e: e.activation(inv[:], inv[:], AF.Exp, scale=-math.log(10000.0) / 32.0),
                 reads=[inv], writes=[inv])
            k.op("pool", lambda e: e.memset(sgn[0:32, :], -1.0), writes=[sgn])
            k.op("pool", lambda e: e.memset(sgn[32:64, :], 1.0), writes=[sgn])
            epsD = g0.sb("epsD", [128, 1], F32)
            k.op("pool", lambda e: e.memset(epsD[:], EPS), writes=[epsD])
            cst = {"eps": epsD}
            if run_moe:
                with k.scope() as sz:
                    zeros = sz.sb("zeros", [128, D], F32)
                    k.op("pool", lambda e: e.memset(zeros[:], 0.0), writes=[zeros])
                    k.dma("sp", lambda e: e.dma_start(out=YsD[NSLOT:NSLOT + 128, :], in_=zeros[:]), reads=[zeros], dst=r_ys)

            def bcast(sc, name, row_ap, n, reads=()):
                t = sc.sb(name, [128, n], F32, dma=True)
                k.dma("sp", lambda e: e.dma_start(out=t[:], in_=row_ap.broadcast_to([128, n])), reads=reads, dst=t)
                return t

            rr = [0]

            def evac(out_ap, in_ap, reads, writes, scale=None):
                rr[0] += 1
                if rr[0] % 2:
                    if scale is None:
                        k.op("act", lambda e: e.copy(out_ap, in_ap), reads=reads, writes=writes)
                    else:
                        k.op("act", lambda e: e.mul(out_ap, in_ap, scale), reads=reads, writes=writes)
                else:
                    if scale is None:
                        k.op("dve", lambda e: e.tensor_copy(out_ap, in_ap), reads=reads, writes=writes)
                    else:
                        k.op("dve", lambda e: e.tensor_scalar(out_ap, in_ap, scale, None, ALU.mult),
                             reads=reads, writes=writes)

            def wload(t, src_ap, ncol, nsplit=1):
                v = src_ap.rearrange("(c p) n -> p c n", p=128)
                step = ncol // nsplit
                k.dma("pool", [lambda e, i=i: e.dma_start(out=t[:, :, i * step:(i + 1) * step],
                                                          in_=v[:, :, i * step:(i + 1) * step])
                               for i in range(nsplit)], dst=t)

            with k.scope() as sm:
                cT = sm.sb("cT", [128, 16, NB], F32, dma=True)
                csT = sm.sb("csT", [128, 16, NB], F32)
                k.dma("sp", [lambda e, b=b: e.dma_start(out=cT[:, :, b], in_=c_d[b].rearrange("(c p) -> p c", p=128),
                                                        allow_slow_non_contiguous=True) for b in range(NB)], dst=cT)
                k.op("act", lambda e: e.activation(csT[:], cT[:], AF.Silu), reads=[cT], writes=[csT])
                csb = sm.sb("csb", [128, 16, NB], BF16)
                k.op("dve", lambda e: e.tensor_copy(csb[:], csT[:]), reads=[csT], writes=[csb])
                wb = [sm.sb(f"wada{i}", [128, 16, 512], BF16, dma=True) for i in range(3)]
                mps = [sm.ps(f"mps{i}", [NB, 512]) for i in range(2)]
                msb = [sm.sb(f"msb{i}", [NB, 512], F32) for i in range(2)]
                for l in W:
                    bsb = sm.sb(f"bsb{l}", [NB, 6 * D], F32, dma=True)
                    k.dma("sp", lambda e, l=l: e.dma_start(out=bsb[:], in_=W[l]["b_ada"].broadcast_to([NB, 6 * D])), dst=bsb)
                    wv = W[l]["w_ada"].rearrange("(c p) n -> p c n", p=128)
                    for n in range(24):
                        i = n % 2
                        wi_ = n % 3
                        k.dma("pool", [lambda e, n=n, i=wi_, h=h: e.dma_start(out=wb[i][:, h * 8:(h + 1) * 8, :],
                                                                          in_=wv[:, h * 8:(h + 1) * 8, n * 512:(n + 1) * 512])
                                     for h in range(2)], dst=wb[wi_])
                        for c in range(16):
                            k.op("pe", lambda e, c=c, i=i, wi_=wi_: e.matmul(mps[i][:], csb[:, c, :], wb[wi_][:, c, :],
                                                                    start=(c == 0), stop=(c == 15)),
                                 reads=[csb, wb[wi_]], writes=[mps[i]], inc=(c == 15))
                        k.op("dve", lambda e, n=n, i=i: e.tensor_tensor(msb[i][:], mps[i][:], bsb[:, n * 512:(n + 1) * 512], ALU.add),
                             reads=[mps[i], bsb], writes=[msb[i]])
                        k.dma("sp", lambda e, n=n, i=i, l=l: e.dma_start(out=modD[l, :, n * 512:(n + 1) * 512], in_=msb[i][:]),
                              reads=[msb[i]], dst=r_mod)

            def mod_row(l, b, j):
                return modD[l, b:b + 1, j * D:(j + 1) * D]

            def ln_stats(sc_tiles, xt, junk, st):
                k.op("act", lambda e: e.activation(junk[:], xt[:], AF.Square, accum_out=st[:, 0:1]),
                     reads=[xt], writes=[junk, st])
                k.op("act", lambda e: e.activation(st[:, 1:2], st[:, 0:1], AF.Sqrt, bias=sc_tiles["eps"][:, 0:1], scale=1.0 / D),
                     reads=[st, sc_tiles["eps"]], writes=[st])
                k.op("dve", lambda e: e.reciprocal(st[:, 1:2], st[:, 1:2]), reads=[st], writes=[st])

            def rope_tables(sc, b, cos_t, sin_t, scale):
                pki = sc.sb("pki", [64, S], I32, dma=True)
                ang = sc.sb("ang", [64, S], F32)
                kk = sc.sb("kk", [64, S], F32)
                kki = sc.sb("kki", [64, S], I32)
                k.dma("sp", lambda e: e.dma_start(out=pki[:], in_=pos_d[b:b + 1, :].broadcast_to([64, S])), dst=pki)
                k.op("dve", lambda e: e.tensor_copy(ang[:], pki[:]), reads=[pki], writes=[ang])
                k.op("dve", lambda e: e.tensor_scalar(ang[:], ang[:], inv[:, 0:1], None, ALU.mult), reads=[ang, inv], writes=[ang])
                TWO_PI = 2.0 * math.pi

                def reduce_sin(dst, shift):
                    k.op("dve", lambda e: e.tensor_scalar(kk[:], ang[:], shift, 1.0 / TWO_PI, ALU.add, ALU.mult), reads=[ang], writes=[kk])
                    k.op("dve", lambda e: e.tensor_copy(kki[:], kk[:]), reads=[kk], writes=[kki])
                    k.op("dve", lambda e: e.tensor_copy(kk[:], kki[:]), reads=[kki], writes=[kk])
                    k.op("dve", lambda e: e.scalar_tensor_tensor(kk[:], kk[:], -TWO_PI, ang[:], ALU.mult, ALU.add), reads=[kk, ang], writes=[kk])
                    if shift:
                        k.op("dve", lambda e: e.tensor_scalar(kk[:], kk[:], shift, None, ALU.add), reads=[kk], writes=[kk])
                    k.op("dve", lambda e: e.tensor_scalar(dst[:], kk[:], math.pi, -TWO_PI, ALU.is_gt, ALU.mult), reads=[kk], writes=[dst])
                    k.op("dve", lambda e: e.tensor_tensor(kk[:], kk[:], dst[:], ALU.add), reads=[kk, dst], writes=[kk])
                    k.op("dve", lambda e: e.tensor_scalar(dst[:], kk[:], -math.pi, TWO_PI, ALU.is_lt, ALU.mult), reads=[kk], writes=[dst])
                    k.op("dve", lambda e: e.tensor_tensor(kk[:], kk[:], dst[:], ALU.add), reads=[kk, dst], writes=[kk])
                    k.op("dve", lambda e: e.tensor_scalar(kk[:], kk[:], math.pi, -math.pi, ALU.min, ALU.max), reads=[kk], writes=[kk])
                    k.op("act", lambda e: e.activation(dst[:], kk[:], AF.Sin), reads=[kk], writes=[dst])

                reduce_sin(sin_t, 0.0)
                reduce_sin(cos_t, math.pi / 2.0)
                k.op("dve", lambda e: e.tensor_scalar(sin_t[:], sin_t[:], sgn[:, 0:1], scale, ALU.mult, ALU.mult), reads=[sin_t, sgn], writes=[sin_t])
                if scale != 1.0:
                    k.op("dve", lambda e: e.tensor_scalar(cos_t[:], cos_t[:], scale, None, ALU.mult), reads=[cos_t], writes=[cos_t])

            def attn_seq(l, b, xin, r_xin):
                Wl = W[l]
                win = Wl["w_in"]
                with k.scope() as so:
                    cqn = so.sb("cqn", [128, 4, S], BF16)
                    ckvn = so.sb("ckvn", [128, 4, S], BF16)
                    krT = so.sb("krT", [64, S], BF16)
                    with k.scope() as sh:
                        hT = sh.sb("hT", [128, 16, S], BF16)
                        with k.scope() as s1:
                            A1 = bcast(s1, "A1", mod_row(l, b, 1), D, reads=[r_mod])
                            B1 = bcast(s1, "B1", mod_row(l, b, 0), D, reads=[r_mod])
                            G1 = bcast(s1, "G1", Wl["ln1_g"], D)
                            k.op("dve", lambda e: e.scalar_tensor_tensor(A1[:], A1[:], 1.0, G1[:], ALU.add, ALU.mult),
                                 reads=[A1, G1], writes=[A1])
                            xt = [s1.sb(f"xt{i}", [128, D], F32, dma=True) for i in range(2)]
                            junk = s1.sb("junk", [128, D], F32)
                            hb = [s1.sb(f"hb{i}", [128, D], BF16) for i in range(2)]
                            st = [s1.sb(f"st{i}", [128, 2], F32) for i in range(2)]
                            pT = [s1.ps(f"pT{i}", [128, 8, 128], BF16) for i in range(2)]
                            for tt in range(16):
                                i = tt % 2
                                r0 = b * S + tt * 128
                                k.dma("sp", lambda e, i=i, r0=r0: e.dma_start(out=xt[i][:], in_=xin[r0:r0 + 128, :]),
                                      reads=[r_xin] if r_xin else [], dst=xt[i])
                                ln_stats(cst, xt[i], junk, st[i])
                                k.op("dve", lambda e, i=i: e.scalar_tensor_tensor(junk[:], xt[i][:], st[i][:, 1:2], A1[:], ALU.mult, ALU.mult),
                                     reads=[xt[i], st[i], A1], writes=[junk])
                                k.op("dve", lambda e, i=i: e.tensor_tensor(hb[i][:], junk[:], B1[:], ALU.add),
                                     reads=[junk, B1], writes=[hb[i]])
                                for hf in range(2):
                                    for j in range(8):
                                        c = hf * 8 + j
                                        k.op("pe", lambda e, i=i, hf=hf, j=j, c=c: e.transpose(pT[hf][:, j, :], hb[i][:, c * 128:(c + 1) * 128], ident[:]),
                                             reads=[hb[i], ident], writes=[pT[hf]], inc=(j == 8 - 1))
                                    evac(hT[:, hf * 8:(hf + 1) * 8, tt * 128:(tt + 1) * 128], pT[hf][:], [pT[hf]], [hT])

                        for g, dil in enumerate((1, 4, 16)):
                            Lc = S // dil
                            tpc = Lc // 128

                            def hblk(c, tb_):
                                if dil == 1:
                                    return hT[:, c, tb_ * 512:(tb_ + 1) * 512]
                                v = hT[:, c, :].rearrange("p (i d) -> p d i", d=dil)
                                if dil == 4:
                                    return v[:, tb_, :]
                                return v[:, 4 * tb_:4 * tb_ + 4, :]

                            def htile(c, tt):
                                if dil == 1:
                                    return hT[:, c, tt * 128:(tt + 1) * 128]
                                v = hT[:, c, :].rearrange("p (i d) -> p d i", d=dil)
                                if dil == 4:
                                    return v[:, tt // 4, (tt % 4) * 128:(tt % 4 + 1) * 128]
                                return v[:, tt, :]

                            def pso(ps):
                                return ps[:].rearrange("p (r i) -> p r i", r=4) if dil == 16 else ps[:]

                            with k.scope() as sg:
                                qT = sg.sb("qT", [128, 4, S], BF16)
                                kT = sg.sb("kT", [128, 4, S], BF16)
                                V = sg.sb("V", [128, 16, 512], BF16)
                                with k.scope() as sw:
                                    wq = sw.sb("wq", [128, 16, 512], BF16, dma=True)
                                    wk = sw.sb("wk", [128, 16, 512], BF16, dma=True)
                                    wv = sw.sb("wv", [128, 16, 512], BF16, dma=True)
                                    wload(wq, win[:, g * 512:(g + 1) * 512], 512)
                                    wload(wk, win[:, 1536 + g * 512:1536 + (g + 1) * 512], 512)
                                    wload(wv, win[:, 3072 + g * 512:3072 + (g + 1) * 512], 512)
                                    pp = [sw.ps(f"pp{i}", [128, 512]) for i in range(2)]
                                    n = 0
                                    for (w_, dst, scl) in ((wq, qT, 128.0 ** -0.5), (wk, kT, None)):
                                        for h in range(4):
                                            for tb_ in range(4):
                                                ps = pp[n % 2]
                                                n += 1
                                                for c in range(16):
                                                    k.op("pe", lambda e, ps=ps, w_=w_, h=h, c=c, tb_=tb_: e.matmul(
                                                        pso(ps), w_[:, c, h * 128:(h + 1) * 128], hblk(c, tb_), start=(c == 0), stop=(c == 15)),
                                                        reads=[w_, hT], writes=[ps], inc=(c == 15))
                                                evac(dst[:, h, tb_ * 512:(tb_ + 1) * 512], ps[:], [ps], [dst], scale=scl)
                                    for tt in range(16):
                                        ps = pp[n % 2]
                                        n += 1
                                        for c in range(16):
                                            k.op("pe", lambda e, ps=ps, c=c, tt=tt: e.matmul(ps[:], htile(c, tt), wv[:, c, :], start=(c == 0), stop=(c == 15)),
                                                 reads=[wv, hT], writes=[ps], inc=(c == 15))
                                        evac(V[:, tt, :], ps[:], [ps], [V])
                                pqi = sg.sb("pqi", [128, 16], I32, dma=True)
                                pq = sg.sb("pq", [128, 16], F32)
                                pki = sg.sb("pki", [128, S], I32, dma=True)
                                pk = sg.sb("pk", [128, S], F32)
                                pb = pos_d[b]
                                if dil == 1:
                                    k.dma("sp", lambda e: e.dma_start(out=pqi[:], in_=pb.rearrange("(t a) -> a t", a=128), allow_slow_non_contiguous=True), dst=pqi)
                                elif dil == 4:
                                    k.dma("sp", lambda e: e.dma_start(out=pqi[:].rearrange("a (r q) -> a r q", r=4),
                                                                      in_=pb.rearrange("(q a r) -> a r q", q=4, a=128, r=4), allow_slow_non_contiguous=True), dst=pqi)
                                else:
                                    k.dma("sp", lambda e: e.dma_start(out=pqi[:], in_=pb.rearrange("(a r) -> a r", r=16)), dst=pqi)
                                k.dma("sp", lambda e: e.dma_start(out=pki[:], in_=pos_d[b:b + 1, :].broadcast_to([128, S])), dst=pki)
                                k.op("dve", lambda e: e.tensor_copy(pq[:], pqi[:]), reads=[pqi], writes=[pq])
                                k.op("dve", lambda e: e.tensor_scalar(pq[:], pq[:], -1.0, None, ALU.mult), reads=[pq], writes=[pq])
                                k.op("dve", lambda e: e.tensor_copy(pk[:], pki[:]), reads=[pki], writes=[pk])
                                dist = [sg.sb(f"dist{i}", [128, 384], F32) for i in range(2)]
                                ssb = [sg.sb(f"ssb{i}", [128, 384], F32) for i in range(2)]
                                pb16 = [sg.sb(f"pb{i}", [128, 384], BF16) for i in range(2)]
                                pts = [sg.sb(f"pts{i}", [128, 3, 128], BF16) for i in range(2)]
                                og = [sg.sb(f"og{i}", [128, 512], F32) for i in range(2)]
                                lse = [sg.sb(f"lse{i}", [128, 4], F32) for i in range(2)]
                                sm_ = [[sg.sb(f"sm{i}_{j}", [128, 1], F32) for j in range(5)] for i in range(2)]
                                sps = [sg.ps(f"sps{i}", [128, 512]) for i in range(2)]
                                ptp = [sg.ps(f"ptp{i}", [128, 4, 128], BF16) for i in range(2)]
                                ops = [sg.ps(f"ops{i}", [128, 512]) for i in range(2)]
                                for tt in range(16):
                                    it = tt % 2
                                    cls, ti = tt // tpc, tt % tpc
                                    i0 = ti * 128
                                    pbase = cls * Lc
                                    lo, hi = max(0, i0 - 128), min(Lc, i0 + 256)
                                    w = hi - lo
                                    c0 = 128 - (i0 - lo)
                                    nch = w // 128
                                    if dil == 1:
                                        pkv = pk[:, lo:hi]
                                    else:
                                        pkv = pk[:, :].rearrange("p (i d) -> p d i", d=dil)[:, cls, lo:hi]
                                    k.op("act", lambda e, it=it, pkv=pkv, tt=tt, w=w: e.activation(dist[it][:, :w], pkv, AF.Abs, bias=pq[:, tt:tt + 1], scale=1.0),
                                         reads=[pk, pq], writes=[dist[it]])
                                    k.op("dve", lambda e, it=it, w=w, c0=c0: e.tensor_tensor(dist[it][:, :w], dist[it][:, :w], mband[:, c0:c0 + w], ALU.add),
                                         reads=[dist[it], mband], writes=[dist[it]])
                                    for h in range(4):
                                        ih = h % 2
                                        mx, nmx, ll, rl, lnl = sm_[ih]
                                        slope = slopes[g * 4 + h]
                                        q0 = pbase + i0
                                        k.op("pe", lambda e, ih=ih, h=h, q0=q0, w=w, lo=lo, pbase=pbase: e.matmul(
                                            sps[ih][:, :w], qT[:, h, q0:q0 + 128], kT[:, h, pbase + lo:pbase + lo + w], start=True, stop=True),
                                            reads=[qT, kT], writes=[sps[ih]])
                                        k.op("dve", lambda e, ih=ih, it=it, w=w, slope=slope: e.scalar_tensor_tensor(
                                            ssb[ih][:, :w], dist[it][:, :w], -slope, sps[ih][:, :w], ALU.mult, ALU.add),
                                            reads=[dist[it], sps[ih]], writes=[ssb[ih]])
                                        k.op("dve", lambda e, ih=ih, w=w, mx=mx: e.reduce_max(mx[:], ssb[ih][:, :w], AX.X), reads=[ssb[ih]], writes=[mx])
                                        k.op("dve", lambda e, mx=mx, nmx=nmx: e.tensor_scalar(nmx[:], mx[:], -1.0, None, ALU.mult), reads=[mx], writes=[nmx])
                                        k.op("act", lambda e, ih=ih, w=w, nmx=nmx, ll=ll: e.activation(pb16[ih][:, :w], ssb[ih][:, :w], AF.Exp, bias=nmx[:, 0:1], scale=1.0, accum_out=ll[:, 0:1]),
                                             reads=[ssb[ih], nmx], writes=[pb16[ih], ll])
                                        for j in range(nch):
                                            k.op("pe", lambda e, ih=ih, j=j: e.transpose(ptp[ih][:, j, :], pb16[ih][:, j * 128:(j + 1) * 128], ident[:]),
                                                 reads=[pb16[ih], ident], writes=[ptp[ih]], inc=(j == nch - 1))
                                        evac(pts[ih][:, :nch, :], ptp[ih][:, :nch, :], [ptp[ih]], [pts[ih]])
                                        vt0 = (pbase + lo) // 128
                                        for j in range(nch):
                                            k.op("pe", lambda e, it=it, ih=ih, j=j, h=h, vt0=vt0, nch=nch: e.matmul(
                                                ops[it][:, h * 128:(h + 1) * 128], pts[ih][:, j, :], V[:, vt0 + j, h * 128:(h + 1) * 128],
                                                start=(j == 0), stop=(j == nch - 1)), reads=[pts[ih], V], writes=[ops[it]], inc=(j == nch - 1))
                                        k.op("dve", lambda e, ll=ll, rl=rl: e.reciprocal(rl[:], ll[:]), reads=[ll], writes=[rl])
                                        k.op("dve", lambda e, it=it, h=h, rl=rl: e.tensor_scalar(og[it][:, h * 128:(h + 1) * 128], ops[it][:, h * 128:(h + 1) * 128], rl[:, 0:1], None, ALU.mult),
                                             reads=[ops[it], rl], writes=[og[it]])
                                        k.op("act", lambda e, ll=ll, lnl=lnl: e.activation(lnl[:], ll[:], AF.Ln), reads=[ll], writes=[lnl])
                                        k.op("dve", lambda e, it=it, h=h, mx=mx, lnl=lnl: e.tensor_tensor(lse[it][:, h:h + 1], mx[:], lnl[:], ALU.add),
                                             reads=[mx, lnl], writes=[lse[it]])
                                    ob = oD[g][b * S:(b + 1) * S, :]
                                    lb = lseD[g][b * S:(b + 1) * S, :]
                                    if dil == 1:
                                        orow, lrow = ob[tt * 128:(tt + 1) * 128, :], lb[tt * 128:(tt + 1) * 128, :]
                                    else:
                                        orow = ob.rearrange("(i d) f -> d i f", d=dil)[cls, i0:i0 + 128, :]
                                        lrow = lb.rearrange("(i d) f -> d i f", d=dil)[cls, i0:i0 + 128, :]
                                    k.dma("sp", lambda e, it=it, orow=orow: e.dma_start(out=orow, in_=og[it][:]), reads=[og[it]], dst=r_o[g])
                                    k.dma("sp", lambda e, it=it, lrow=lrow: e.dma_start(out=lrow, in_=lse[it][:]), reads=[lse[it]], dst=r_lse[g])

                        with k.scope() as sl:
                            cosk = sl.sb("cosk", [64, S], F32)
                            sink = sl.sb("sink", [64, S], F32)
                            with k.scope() as srt:
                                rope_tables(srt, b, cosk, sink, 1.0)
                            wcq = sl.sb("wcq", [128, 16, 512], BF16, dma=True)
                            wckv = sl.sb("wckv", [128, 16, 512], BF16, dma=True)
                            wkr = sl.sb("wkr", [128, 16, 128], BF16, dma=True)
                            wload(wcq, win[:, 4608:5120], 512)
                            wload(wckv, win[:, 5120:5632], 512)
                            wv_ = win.rearrange("(c p) n -> p c n", p=128)
                            k.dma("pool", [lambda e: e.dma_start(out=wkr[:, :, 0:64], in_=wv_[:, :, 5632:5696]),
                                           lambda e: e.dma_start(out=wkr[:, :, 64:96], in_=wv_[:, :, 5664:5696]),
                                           lambda e: e.dma_start(out=wkr[:, :, 96:128], in_=wv_[:, :, 5632:5664])], dst=wkr)
                            gq = sl.sb("gq", [128, 4], F32, dma=True)
                            gkv = sl.sb("gkv", [128, 4], F32, dma=True)
                            k.dma("sp", lambda e: e.dma_start(out=gq[:], in_=Wl["q_norm_g"].rearrange("(c p) -> p c", p=128), allow_slow_non_contiguous=True), dst=gq)
                            k.dma("sp", lambda e: e.dma_start(out=gkv[:], in_=Wl["kv_norm_g"].rearrange("(c p) -> p c", p=128), allow_slow_non_contiguous=True), dst=gkv)
                            latf = sl.sb("latf", [128, 4, 512], F32)
                            sq = sl.sb("sq", [128, 4, 512], F32)
                            rs = sl.sb("rs", [128, 512], F32)
                            t1 = sl.sb("t1", [64, 512], F32)
                            t2 = sl.sb("t2", [64, 512], F32)
                            lps = [sl.ps(f"lps{i}", [128, 512]) for i in range(4)]
                            sps_ = sl.ps("ssq", [128, 512])
                            psr = sl.ps("psr", [64, 512])
                            pss = sl.ps("pss", [64, 512])
                            for (w_, gv, dst) in ((wcq, gq, cqn), (wckv, gkv, ckvn)):
                                for tb_ in range(4):
                                    blk = slice(tb_ * 512, (tb_ + 1) * 512)
                                    for c4 in range(4):
                                        for c in range(16):
                                            k.op("pe", lambda e, c4=c4, c=c, w_=w_, blk=blk: e.matmul(lps[c4][:], w_[:, c, c4 * 128:(c4 + 1) * 128], hT[:, c, blk], start=(c == 0), stop=(c == 15)),
                                                 reads=[w_, hT], writes=[lps[c4]], inc=(c == 15))
                                        k.op("act", lambda e, c4=c4: e.copy(latf[:, c4, :], lps[c4][:]), reads=[lps[c4]], writes=[latf])
                                        k.op("act", lambda e, c4=c4: e.activation(sq[:, c4, :], lps[c4][:], AF.Square), reads=[lps[c4]], writes=[sq])
                                    for c4 in range(4):
                                        k.op("pe", lambda e, c4=c4: e.matmul(sps_[:], onesf[:], sq[:, c4, :], start=(c4 == 0), stop=(c4 == 3)),
                                             reads=[onesf, sq], writes=[sps_], inc=(c4 == 3))
                                    k.op("act", lambda e: e.activation(rs[:], sps_[:], AF.Sqrt, bias=epsD[:, 0:1], scale=1.0 / 512.0), reads=[sps_, epsD], writes=[rs])
                                    k.op("dve", lambda e: e.reciprocal(rs[:], rs[:]), reads=[rs], writes=[rs])
                                    for c4 in range(4):
                                        k.op("dve", lambda e, c4=c4, gv=gv, dst=dst, blk=blk: e.scalar_tensor_tensor(dst[:, c4, blk], latf[:, c4, :], gv[:, c4:c4 + 1], rs[:], ALU.mult, ALU.mult),
                                             reads=[latf, gv, rs], writes=[dst])
                            for tb_ in range(4):
                                blk = slice(tb_ * 512, (tb_ + 1) * 512)
                                for c in range(16):
                                    k.op("pe", lambda e, c=c, blk=blk: e.matmul(psr[:], wkr[:, c, 0:64], hT[:, c, blk], start=(c == 0), stop=(c == 15)), reads=[wkr, hT], writes=[psr], inc=(c == 15))
                                for c in range(16):
                                    k.op("pe", lambda e, c=c, blk=blk: e.matmul(pss[:], wkr[:, c, 64:128], hT[:, c, blk], start=(c == 0), stop=(c == 15)), reads=[wkr, hT], writes=[pss], inc=(c == 15))
                                k.op("dve", lambda e, blk=blk: e.tensor_tensor(t1[:], psr[:], cosk[:, blk], ALU.mult), reads=[psr, cosk], writes=[t1])
                                k.op("dve", lambda e, blk=blk: e.tensor_tensor(t2[:], pss[:], sink[:, blk], ALU.mult), reads=[pss, sink], writes=[t2])
                                k.op("dve", lambda e, blk=blk: e.tensor_tensor(krT[:, blk], t1[:], t2[:], ALU.add), reads=[t1, t2], writes=[krT])

                        with k.scope() as sgt:
                            wg = [sgt.sb(f"wg{i}", [128, 16, 512], BF16, dma=True) for i in range(2)]
                            gsb = [sgt.sb(f"gsb{i}", [128, 512], BF16) for i in range(2)]
                            gps = [sgt.ps(f"gps{i}", [128, 512]) for i in range(2)]
                            m = 0
                            for n in range(8):
                                wi = wg[n % 2]
                                wload(wi, win[:, 5696 + n * 512:5696 + (n + 1) * 512], 512)
                                for tt in range(16):
                                    i = m % 2
                                    m += 1
                                    for c in range(16):
                                        k.op("pe", lambda e, i=i, c=c, tt=tt, wi=wi: e.matmul(gps[i][:], hT[:, c, tt * 128:(tt + 1) * 128], wi[:, c, :], start=(c == 0), stop=(c == 15)),
                                             reads=[hT, wi], writes=[gps[i]], inc=(c == 15))
                                    k.op("act", lambda e, i=i: e.activation(gsb[i][:], gps[i][:], AF.Sigmoid), reads=[gps[i]], writes=[gsb[i]])
                                    r0 = b * S + tt * 128
                                    k.dma("sp", lambda e, i=i, r0=r0, n=n: e.dma_start(out=gD[r0:r0 + 128, n * 512:(n + 1) * 512], in_=gsb[i][:]), reads=[gsb[i]], dst=r_g)

                    with k.scope() as sa:
                        cosq = sa.sb("cosq", [64, S], F32)
                        sinq = sa.sb("sinq", [64, S], F32)
                        with k.scope() as srt:
                            rope_tables(srt, b, cosq, sinq, 192.0 ** -0.5)
                        wuq = sa.sb("wuq", [128, 4, 1536], BF16, dma=True)
                        wuqs = sa.sb("wuqs", [128, 4, 8, 64], BF16, dma=True)
                        wukv = sa.sb("wukv", [128, 4, 2048], BF16, dma=True)
                        wload(wuq, Wl["w_uq"], 1536)
                        wload(wukv, Wl["w_ukv"], 2048)
                        uqv = Wl["w_uq"].rearrange("(c p) (h x) -> p c h x", p=128, x=192)
                        k.dma("pool", [lambda e, c=c: e.dma_start(out=wuqs[:, c, :, 0:32], in_=uqv[:, c, :, 160:192]) for c in range(4)]
                              + [lambda e, c=c: e.dma_start(out=wuqs[:, c, :, 32:64], in_=uqv[:, c, :, 128:160]) for c in range(4)], dst=wuqs)
                        wukv_h = wukv[:, :, :].rearrange("p c (h x) -> p c h x", x=256)
                        for hh in range(2):
                            with k.scope() as sh2:
                                qnT = sh2.sb("qnT", [128, 4, S], BF16)
                                qrT = sh2.sb("qrT", [64, 4, S], BF16)
                                knT = sh2.sb("knT", [128, 4, S], BF16)
                                Vb = sh2.sb("Vb", [128, 16, 512], BF16)
                                with k.scope() as sp_:
                                    pn = [sp_.ps(f"pn{i}", [128, 512]) for i in range(2)]
                                    pr = sp_.ps("pr", [64, 512])
                                    pz = sp_.ps("pz", [64, 512])
                                    t1 = sp_.sb("t1", [64, 512], F32)
                                    t2 = sp_.sb("t2", [64, 512], F32)
                                    n = 0
                                    for hl in range(4):
                                        h = hh * 4 + hl
                                        for tb_ in range(4):
                                            blk = slice(tb_ * 512, (tb_ + 1) * 512)
                                            ps = pn[n % 2]
                                            n += 1
                                            for c in range(4):
                                                k.op("pe", lambda e, ps=ps, c=c, h=h, blk=blk: e.matmul(ps[:], wuq[:, c, h * 192:h * 192 + 128], cqn[:, c, blk], start=(c == 0), stop=(c == 3)),
                                                     reads=[wuq, cqn], writes=[ps], inc=(c == 3))
                                            evac(qnT[:, hl, blk], ps[:], [ps], [qnT], scale=192.0 ** -0.5)
                                            for c in range(4):
                                                k.op("pe", lambda e, c=c, h=h, blk=blk: e.matmul(pr[:], wuq[:, c, h * 192 + 128:h * 192 + 192], cqn[:, c, blk], start=(c == 0), stop=(c == 3)),
                                                     reads=[wuq, cqn], writes=[pr], inc=(c == 3))
                                            for c in range(4):
                                                k.op("pe", lambda e, c=c, h=h, blk=blk: e.matmul(pz[:], wuqs[:, c, h, :], cqn[:, c, blk], start=(c == 0), stop=(c == 3)),
                                                     reads=[wuqs, cqn], writes=[pz], inc=(c == 3))
                                            k.op("dve", lambda e, blk=blk: e.tensor_tensor(t1[:], pr[:], cosq[:, blk], ALU.mult), reads=[pr, cosq], writes=[t1])
                                            k.op("dve", lambda e, blk=blk: e.tensor_tensor(t2[:], pz[:], sinq[:, blk], ALU.mult), reads=[pz, sinq], writes=[t2])
                                            k.op("dve", lambda e, blk=blk, hl=hl: e.tensor_tensor(qrT[:, hl, blk], t1[:], t2[:], ALU.add), reads=[t1, t2], writes=[qrT])
                                            ps = pn[n % 2]
                                            n += 1
                                            for c in range(4):
                                                k.op("pe", lambda e, ps=ps, c=c, h=h, blk=blk: e.matmul(ps[:], wukv[:, c, h * 256:h * 256 + 128], ckvn[:, c, blk], start=(c == 0), stop=(c == 3)),
                                                     reads=[wukv, ckvn], writes=[ps], inc=(c == 3))
                                            evac(knT[:, hl, blk], ps[:], [ps], [knT])
                                    for tt in range(16):
                                        ps = pn[n % 2]
                                        n += 1
                                        for c in range(4):
                                            k.op("pe", lambda e, ps=ps, c=c, tt=tt: e.matmul(ps[:].rearrange("p (h x) -> p h x", h=4), ckvn[:, c, tt * 128:(tt + 1) * 128],
                                                                                          wukv_h[:, c, hh * 4:(hh + 1) * 4, 128:256], start=(c == 0), stop=(c == 3)),
                                                 reads=[wukv, ckvn], writes=[ps], inc=(c == 3))
                                        evac(Vb[:, tt, :], ps[:], [ps], [Vb])
                                with k.scope() as sat:
                                    Sps = sat.ps("Sps", [128, S])
                                    ptp = [sat.ps(f"ptp{i}", [128, 8, 128], BF16) for i in range(2)]
                                    ops = sat.ps("ops", [128, 512])
                                    P = [sat.sb(f"P{i}", [128, S], BF16) for i in range(2)]
                                    pts = [sat.sb(f"pts{i}", [128, 16, 128], BF16) for i in range(2)]
                                    yb = [sat.sb(f"yb{i}", [128, 512], BF16) for i in range(2)]
                                    sm_ = [[sat.sb(f"sm{i}_{j}", [128, 1], F32) for j in range(4)] for i in range(2)]
                                    steps = [(qt, hl) for qt in range(16) for hl in range(4)]

                                    def stage_a(i):
                                        qt, hl = steps[i]
                                        ih = i % 2
                                        qs = slice(qt * 128, (qt + 1) * 128)
                                        mx, nmx, ll, rl = sm_[ih]
                                        for nk in range(4):
                                            ks = slice(nk * 512, (nk + 1) * 512)
                                            k.op("pe", lambda e, hl=hl, qs=qs, ks=ks: e.matmul(Sps[:, ks], qnT[:, hl, qs], knT[:, hl, ks], start=True, stop=False),
                                                 reads=[qnT, knT], writes=[Sps], inc=False)
                                            k.op("pe", lambda e, hl=hl, qs=qs, ks=ks: e.matmul(Sps[:, ks], qrT[:, hl, qs], krT[:, ks], start=False, stop=True),
                                                 reads=[qrT, krT], writes=[Sps], inc=(nk == 3))
                                        k.op("dve", lambda e, mx=mx: e.reduce_max(mx[:], Sps[:], AX.X), reads=[Sps], writes=[mx])
                                        k.op("dve", lambda e, mx=mx, nmx=nmx: e.tensor_scalar(nmx[:], mx[:], -1.0, None, ALU.mult), reads=[mx], writes=[nmx])
                                        k.op("act", lambda e, ih=ih, nmx=nmx, ll=ll: e.activation(P[ih][:], Sps[:], AF.Exp, bias=nmx[:, 0:1], scale=1.0, accum_out=ll[:, 0:1]),
                                             reads=[Sps, nmx], writes=[P[ih], ll])

                                    def stage_b(i):
                                        qt, hl = steps[i]
                                        ih = i % 2
                                        iy = qt % 2
                                        mx, nmx, ll, rl = sm_[ih]
                                        for hf in range(2):
                                            for j in range(8):
                                                c = hf * 8 + j
                                                k.op("pe", lambda e, ih=ih, hf=hf, j=j, c=c: e.transpose(ptp[hf][:, j, :], P[ih][:, c * 128:(c + 1) * 128], ident[:]),
                                                     reads=[P[ih], ident], writes=[ptp[hf]], inc=(j == 7))
                                            evac(pts[ih][:, hf * 8:(hf + 1) * 8, :], ptp[hf][:], [ptp[hf]], [pts[ih]])
                                        for c in range(16):
                                            k.op("pe", lambda e, ih=ih, c=c, hl=hl: e.matmul(ops[:, hl * 128:(hl + 1) * 128], pts[ih][:, c, :], Vb[:, c, hl * 128:(hl + 1) * 128], start=(c == 0), stop=(c == 15)),
                                                 reads=[pts[ih], Vb], writes=[ops], inc=(c == 15))
                                        k.op("dve", lambda e, ll=ll, rl=rl: e.reciprocal(rl[:], ll[:]), reads=[ll], writes=[rl])
                                        k.op("dve", lambda e, iy=iy, hl=hl, rl=rl: e.tensor_scalar(yb[iy][:, hl * 128:(hl + 1) * 128], ops[:, hl * 128:(hl + 1) * 128], rl[:, 0:1], None, ALU.mult),
                                             reads=[ops, rl], writes=[yb[iy]])
                                        if hl == 3:
                                            r0 = b * S + qt * 128
                                            k.dma("sp", lambda e, iy=iy, r0=r0: e.dma_start(out=ybD[r0:r0 + 128, hh * 512:(hh + 1) * 512], in_=yb[iy][:]), reads=[yb[iy]], dst=r_yb)

                                    for i in range(len(steps) + 1):
                                        if i < len(steps):
                                            stage_a(i)
                                        if i >= 1:
                                            stage_b(i - 1)

            def merge_layer(l, xin, r_xin):
                Wl = W[l]
                with k.scope() as sm:
                    wau = sm.sb("wau", [128, 4, D], BF16, dma=True)
                    wbu = sm.sb("wbu", [128, 8, D], BF16, dma=True)
                    wo = sm.sb("wo", [128, 16, D], BF16, dma=True)
                    wload(wau, Wl["w_a_up"], D)
                    wload(wbu, Wl["w_b_up"], D, 2)
                    wload(wo, Wl["w_o"], D, 4)
                    o_t2 = [[sm.sb(f"o{g}_{i}", [128, 512], F32, dma=True) for g in range(3)] for i in range(2)]
                    ls2 = [sm.sb(f"ls{i}", [128, 3, 4], F32, dma=True) for i in range(2)]
                    ybt2 = [sm.sb(f"ybt{i}", [128, 1024], BF16, dma=True) for i in range(2)]
                    gt2 = [sm.sb(f"gt{i}", [128, 4096], BF16, dma=True) for i in range(2)]
                    xt2 = [sm.sb(f"xt{i}", [128, D], F32, dma=True) for i in range(2)]
                    gp = sm.sb("gp", [128, D], F32, dma=True)
                    mm = sm.sb("mm", [128, 4], F32)
                    ee = sm.sb("ee", [128, 3, 4], F32)
                    den = sm.sb("den", [128, 4], F32)
                    yaf = sm.sb("yaf", [128, 512], F32)
                    tmp = sm.sb("tmp", [128, 512], F32)
                    tmp2 = sm.sb("tmp2", [128, 512], F32)
                    ya = sm.sb("ya", [128, 512], BF16)
                    yaT = sm.sb("yaT", [128, 4, 128], BF16)
                    ybT = sm.sb("ybT", [128, 8, 128], BF16)
                    mg = sm.sb("mg", [128, D], BF16)
                    mT = sm.sb("mT", [128, 16, 128], BF16)
                    xn = sm.sb("xn", [128, D], F32)
                    ptp = [sm.ps(f"ptp{i}", [128, 8, 128], BF16) for i in range(2)]
                    ua = sm.ps("ua", [128, 512])
                    ub = sm.ps("ub", [128, 512])
                    ops = [sm.ps(f"ops{i}", [128, 512]) for i in range(2)]
                    def issue_loads(t):
                        i_ = t % 2
                        r0 = t * 128
                        for g in range(3):
                            k.dma("sp", lambda e, g=g, r0=r0, i_=i_: e.dma_start(out=o_t2[i_][g][:], in_=oD[g][r0:r0 + 128, :]), reads=[r_o[g]], dst=o_t2[i_][g])
                        k.dma("sp", [lambda e, g=g, r0=r0, i_=i_: e.dma_start(out=ls2[i_][:, g, :], in_=lseD[g][r0:r0 + 128, :]) for g in range(3)], reads=r_lse, dst=ls2[i_])
                        k.dma("sp", lambda e, r0=r0, i_=i_: e.dma_start(out=ybt2[i_][:], in_=ybD[r0:r0 + 128, :]), reads=[r_yb], dst=ybt2[i_])
                        k.dma("sp", lambda e, r0=r0, i_=i_: e.dma_start(out=gt2[i_][:], in_=gD[r0:r0 + 128, :]), reads=[r_g], dst=gt2[i_])
                        k.dma("sp", lambda e, r0=r0, i_=i_: e.dma_start(out=xt2[i_][:], in_=xin[r0:r0 + 128, :]), reads=[r_xin] if r_xin else [], dst=xt2[i_])

                    issue_loads(0)
                    for t in range(NT):
                        b = t // 16
                        r0 = t * 128
                        o_t, ls, ybt, gt, xt = o_t2[t % 2], ls2[t % 2], ybt2[t % 2], gt2[t % 2], xt2[t % 2]
                        if t % 16 == 0:
                            k.dma("sp", lambda e, b=b: e.dma_start(out=gp[:], in_=mod_row(l, b, 2).broadcast_to([128, D])), reads=[r_mod], dst=gp)
                            k.op("dve", lambda e: e.tensor_scalar(gp[:], gp[:], 1.0, None, ALU.add), reads=[gp], writes=[gp])
                        if t + 1 < NT:
                            issue_loads(t + 1)
                        k.op("dve", lambda e: e.tensor_tensor(mm[:], ls[:, 0, :], ls[:, 1, :], ALU.max), reads=[ls], writes=[mm])
                        k.op("dve", lambda e: e.tensor_tensor(mm[:], mm[:], ls[:, 2, :], ALU.max), reads=[mm, ls], writes=[mm])
                        for g in range(3):
                            k.op("dve", lambda e, g=g: e.tensor_tensor(ee[:, g, :], ls[:, g, :], mm[:], ALU.subtract), reads=[ls, mm], writes=[ee])
                        k.op("act", lambda e: e.activation(ee[:], ee[:], AF.Exp), reads=[ee], writes=[ee])
                        k.op("dve", lambda e: e.tensor_tensor(den[:], ee[:, 0, :], ee[:, 1, :], ALU.add), reads=[ee], writes=[den])
                        k.op("dve", lambda e: e.tensor_tensor(den[:], den[:], ee[:, 2, :], ALU.add), reads=[den, ee], writes=[den])
                        k.op("dve", lambda e: e.reciprocal(den[:], den[:]), reads=[den], writes=[den])
                        for g in range(3):
                            k.op("dve", lambda e, g=g: e.tensor_tensor(ee[:, g, :], ee[:, g, :], den[:], ALU.mult), reads=[ee, den], writes=[ee])
                        for h in range(4):
                            hs = slice(h * 128, (h + 1) * 128)
                            k.op("dve", lambda e, h=h, hs=hs: e.tensor_scalar(yaf[:, hs], o_t[0][:, hs], ee[:, 0, h:h + 1], None, ALU.mult), reads=[o_t[0], ee], writes=[yaf])
                            k.op("dve", lambda e, h=h, hs=hs: e.scalar_tensor_tensor(yaf[:, hs], o_t[1][:, hs], ee[:, 1, h:h + 1], yaf[:, hs], ALU.mult, ALU.add), reads=[o_t[1], ee, yaf], writes=[yaf])
                            k.op("dve", lambda e, h=h, hs=hs: e.scalar_tensor_tensor(ya[:, hs], o_t[2][:, hs], ee[:, 2, h:h + 1], yaf[:, hs], ALU.mult, ALU.add), reads=[o_t[2], ee, yaf], writes=[ya])
                        for j in range(4):
                            k.op("pe", lambda e, j=j: e.transpose(ptp[0][:, j, :], ya[:, j * 128:(j + 1) * 128], ident[:]), reads=[ya, ident], writes=[ptp[0]], inc=(j == 4 - 1))
                        evac(yaT[:], ptp[0][:, 0:4, :], [ptp[0]], [yaT])
                        for j in range(8):
                            k.op("pe", lambda e, j=j: e.transpose(ptp[1][:, j, :], ybt[:, j * 128:(j + 1) * 128], ident[:]), reads=[ybt, ident], writes=[ptp[1]], inc=(j == 8 - 1))
                        evac(ybT[:], ptp[1][:], [ptp[1]], [ybT])
                        for n in range(4):
                            ns = slice(n * 512, (n + 1) * 512)
                            for c in range(4):
                                k.op("pe", lambda e, c=c, ns=ns: e.matmul(ua[:], yaT[:, c, :], wau[:, c, ns], start=(c == 0), stop=(c == 3)), reads=[yaT, wau], writes=[ua], inc=(c == 3))
                            for c in range(8):
                                k.op("pe", lambda e, c=c, ns=ns: e.matmul(ub[:], ybT[:, c, :], wbu[:, c, ns], start=(c == 0), stop=(c == 7)), reads=[ybT, wbu], writes=[ub], inc=(c == 7))
                            k.op("dve", lambda e, ns=ns: e.tensor_tensor(tmp[:], ua[:], gt[:, ns], ALU.mult), reads=[ua, gt], writes=[tmp])
                            k.op("dve", lambda e, n=n: e.tensor_tensor(tmp2[:], ub[:], gt[:, 2048 + n * 512:2048 + (n + 1) * 512], ALU.mult), reads=[ub, gt], writes=[tmp2])
                            k.op("pool", lambda e, ns=ns: e.tensor_tensor(mg[:, ns], tmp[:], tmp2[:], ALU.add), reads=[tmp, tmp2], writes=[mg])
                        for hf in range(2):
                            for j in range(8):
                                c = hf * 8 + j
                                k.op("pe", lambda e, hf=hf, j=j, c=c: e.transpose(ptp[hf][:, j, :], mg[:, c * 128:(c + 1) * 128], ident[:]), reads=[mg, ident], writes=[ptp[hf]], inc=(j == 8 - 1))
                            evac(mT[:, hf * 8:(hf + 1) * 8, :], ptp[hf][:], [ptp[hf]], [mT])
                        for n in range(4):
                            ns = slice(n * 512, (n + 1) * 512)
                            op_ = ops[n % 2]
                            for c in range(16):
                                k.op("pe", lambda e, c=c, ns=ns, op_=op_: e.matmul(op_[:], mT[:, c, :], wo[:, c, ns], start=(c == 0), stop=(c == 15)), reads=[mT, wo], writes=[op_], inc=(c == 15))
                            k.op("dve", lambda e, ns=ns, op_=op_: e.tensor_tensor(xn[:, ns], op_[:], gp[:, ns], ALU.mult), reads=[op_, gp], writes=[xn])
                            k.op("pool", lambda e, ns=ns: e.tensor_tensor(xn[:, ns], xn[:, ns], xt[:, ns], ALU.add), reads=[xn, xt], writes=[xn])
                        k.dma("sp", lambda e, r0=r0: e.dma_start(out=xaD[r0:r0 + 128, :], in_=xn[:]), reads=[xn], dst=r_xa)

            def moe_round(l, rnd, last):
                Wl = W[l]
                t0 = rnd * (TR // 128)
                with k.scope() as smo:
                    sl_i = smo.sb("sl_i", [128, 32, 2], I32)
                    wts = smo.sb("wts", [128, 32, 2], F32)
                    with k.scope() as s1:
                        wr = s1.sb("wr", [128, 16, 72], F32, dma=True)
                        k.dma("sp", [lambda e: e.dma_start(out=wr[:, :, 0:8], in_=Wl["w_grp"].rearrange("(c p) n -> p c n", p=128)),
                                     lambda e: e.dma_start(out=wr[:, :, 8:72], in_=Wl["w_exp"].rearrange("(c p) n -> p c n", p=128))], dst=wr)
                        br = s1.sb("br", [128, 72], F32, dma=True)
                        k.dma("sp", [lambda e: e.dma_start(out=br[:, 0:8], in_=Wl["b_grp"].broadcast_to([128, 8])),
                                     lambda e: e.dma_start(out=br[:, 8:72], in_=Wl["b_exp"].broadcast_to([128, 64]))], dst=br)
                        A2 = s1.sb("A2", [128, D], F32, dma=True)
                        B2 = s1.sb("B2", [128, D], F32, dma=True)
                        G2 = bcast(s1, "G2", Wl["ln2_g"], D)
                        R = s1.sb("R", [128, 64], BF16)
                        k.op("pool", lambda e: e.memset(R[:], 0.0), writes=[R])
                        xt = [s1.sb(f"xt{i}", [128, D], F32, dma=True) for i in range(2)]
                        junk = s1.sb("junk", [128, D], F32)
                        h2f = s1.sb("h2f", [128, D], F32)
                        h2b = [s1.sb(f"h2b{i}", [128, D], BF16) for i in range(2)]
                        h2T = s1.sb("h2T", [128, 16, 128], F32)
                        st = s1.sb("st", [128, 2], F32)
                        lg = s1.sb("lg", [128, 72], F32)
                        m8 = s1.sb("m8", [128, 8], F32)
                        s8 = s1.sb("s8", [128, 8], F32)
                        sc_ = s1.sb("sc_", [128, 16], F32)
                        eg = s1.sb("eg", [128, 8], F32)
                        Gm = s1.sb("Gm", [128, 8], F32)
                        lm = s1.sb("lm", [128, 64], F32)
                        E1 = s1.sb("E1", [128, 64], F32)
                        E2 = s1.sb("E2", [128, 64], F32)
                        Ab = s1.sb("Ab", [128, 64], BF16)
                        cnt = s1.sb("cnt", [128, 64], F32)
                        tq = s1.sb("tq", [128, 64], F32)
                        slf = s1.sb("slf", [128, 2], F32)
                        ptf = [s1.ps(f"ptf{i}", [128, 4, 128], F32) for i in range(2)]
                        lps = s1.ps("lps", [128, 72])
                        cps = s1.ps("cps", [128, 64])
                        for tl in range(TR // 128):
                            t = t0 + tl
                            b = t // 16
                            i = tl % 2
                            r0 = t * 128
                            if t % 16 == 0:
                                k.dma("sp", lambda e, b=b: e.dma_start(out=A2[:], in_=mod_row(l, b, 4).broadcast_to([128, D])), reads=[r_mod], dst=A2)
                                k.dma("sp", lambda e, b=b: e.dma_start(out=B2[:], in_=mod_row(l, b, 3).broadcast_to([128, D])), reads=[r_mod], dst=B2)
                                k.op("dve", lambda e: e.scalar_tensor_tensor(A2[:], A2[:], 1.0, G2[:], ALU.add, ALU.mult), reads=[A2, G2], writes=[A2])
                            k.dma("sp", lambda e, i=i, r0=r0: e.dma_start(out=xt[i][:], in_=xaD[r0:r0 + 128, :]), reads=[r_xa], dst=xt[i])
                            ln_stats(cst, xt[i], junk, st)
                            k.op("dve", lambda e, i=i: e.scalar_tensor_tensor(junk[:], xt[i][:], st[:, 1:2], A2[:], ALU.mult, ALU.mult), reads=[xt[i], st, A2], writes=[junk])
                            k.op("dve", lambda e: e.tensor_tensor(h2f[:], junk[:], B2[:], ALU.add), reads=[junk, B2], writes=[h2f])
                            k.op("act", lambda e, i=i: e.copy(h2b[i][:], h2f[:]), reads=[h2f], writes=[h2b[i]])
                            for c4 in range(4):
                                pf = ptf[c4 % 2]
                                for j in range(4):
                                    c = c4 * 4 + j
                                    k.op("pe", lambda e, pf=pf, j=j, c=c: e.transpose(pf[:, j, :], h2f[:, c * 128:(c + 1) * 128], identf[:]), reads=[h2f, identf], writes=[pf], inc=(j == 4 - 1))
                                evac(h2T[:, c4 * 4:(c4 + 1) * 4, :], pf[:], [pf], [h2T])
                            for c in range(16):
                                k.op("pe", lambda e, c=c: e.matmul(lps[:], h2T[:, c, :], wr[:, c, :], start=(c == 0), stop=(c == 15)), reads=[h2T, wr], writes=[lps], inc=(c == 15))
                            k.op("dve", lambda e: e.tensor_tensor(lg[:], lps[:], br[:], ALU.add), reads=[lps, br], writes=[lg])
                            k.op("dve", lambda e: e.max(m8[:], lg[:, 0:8]), reads=[lg], writes=[m8])
                            k.op("dve", lambda e: e.tensor_scalar(sc_[:, 0:1], m8[:, 0:1], -1.0, None, ALU.mult), reads=[m8], writes=[sc_])
                            k.op("act", lambda e: e.activation(eg[:], lg[:, 0:8], AF.Exp, bias=sc_[:, 0:1], scale=1.0, accum_out=sc_[:, 1:2]), reads=[lg, sc_], writes=[eg, sc_])
                            k.op("dve", lambda e: e.reciprocal(sc_[:, 2:3], sc_[:, 1:2]), reads=[sc_], writes=[sc_])
                            k.op("dve", lambda e: e.tensor_scalar(Gm[:], lg[:, 0:8], m8[:, 0:1], None, ALU.is_equal), reads=[lg, m8], writes=[Gm])
                            k.op("dve", lambda e: e.tensor_scalar(eg[:], Gm[:], 1.0, BIG, ALU.subtract, ALU.mult), reads=[Gm], writes=[eg])
                            for g in range(8):
                                k.op("dve", lambda e, g=g: e.tensor_scalar(lm[:, g * 8:(g + 1) * 8], lg[:, 8 + g * 8:16 + g * 8], eg[:, g:g + 1], None, ALU.add), reads=[lg, eg], writes=[lm])
                            k.op("dve", lambda e: e.max(s8[:], lm[:]), reads=[lm], writes=[s8])
                            k.op("dve", lambda e: e.tensor_scalar(E1[:], lm[:], s8[:, 0:1], None, ALU.is_equal), reads=[lm, s8], writes=[E1])
                            k.op("dve", lambda e: e.tensor_scalar(E2[:], lm[:], s8[:, 1:2], None, ALU.is_equal), reads=[lm, s8], writes=[E2])
                            k.op("dve", lambda e: e.tensor_scalar(sc_[:, 3:4], s8[:, 0:1], -1.0, None, ALU.mult), reads=[s8], writes=[sc_])
                            k.op("act", lambda e: e.activation(sc_[:, 4:5], s8[:, 1:2], AF.Exp, bias=sc_[:, 3:4], scale=1.0), reads=[s8, sc_], writes=[sc_])
                            k.op("dve", lambda e: e.tensor_scalar(sc_[:, 5:6], sc_[:, 4:5], 1.0, None, ALU.add), reads=[sc_], writes=[sc_])
                            k.op("dve", lambda e: e.reciprocal(sc_[:, 5:6], sc_[:, 5:6]), reads=[sc_], writes=[sc_])
                            k.op("dve", lambda e: e.tensor_tensor(sc_[:, 6:7], sc_[:, 2:3], sc_[:, 5:6], ALU.mult), reads=[sc_], writes=[sc_])
                            k.op("dve", lambda e: e.tensor_tensor(sc_[:, 7:8], sc_[:, 6:7], sc_[:, 4:5], ALU.mult), reads=[sc_], writes=[sc_])
                            k.op("dve", lambda e: e.tensor_tensor(Ab[:], E1[:], E2[:], ALU.add), reads=[E1, E2], writes=[Ab])
                            k.op("pe", lambda e: e.matmul(cps[:], LT[:], Ab[:], start=True, stop=False), reads=[LT, Ab], writes=[cps])
                            k.op("pe", lambda e: e.matmul(cps[:], onesb[:], R[:], start=False, stop=True), reads=[onesb, R], writes=[cps])
                            k.op("dve", lambda e: e.tensor_copy(cnt[:], cps[:]), reads=[cps], writes=[cnt])
                            k.op("pool", lambda e: e.tensor_tensor(R[:], R[:], Ab[:], ALU.add), reads=[R, Ab], writes=[R])
                            for kk_, Ek in ((0, E1), (1, E2)):
                                k.op("dve", lambda e, Ek=Ek: e.tensor_tensor(tq[:], Ek[:], cnt[:], ALU.mult), reads=[Ek, cnt], writes=[tq])
                                k.op("dve", lambda e, kk_=kk_: e.reduce_sum(sc_[:, 8 + kk_:9 + kk_], tq[:], AX.X), reads=[tq], writes=[sc_])
                                k.op("dve", lambda e, Ek=Ek: e.tensor_tensor(tq[:], Ek[:], eC[:], ALU.mult), reads=[Ek, eC], writes=[tq])
                                k.op("dve", lambda e, kk_=kk_: e.reduce_sum(sc_[:, 10 + kk_:11 + kk_], tq[:], AX.X), reads=[tq], writes=[sc_])
                                k.op("dve", lambda e, kk_=kk_: e.tensor_scalar(sc_[:, 12 + kk_:13 + kk_], sc_[:, 8 + kk_:9 + kk_], float(C_CAP), None, ALU.is_lt), reads=[sc_], writes=[sc_])
                                k.op("dve", lambda e, kk_=kk_: e.tensor_tensor(sc_[:, 8 + kk_:9 + kk_], sc_[:, 8 + kk_:9 + kk_], sc_[:, 10 + kk_:11 + kk_], ALU.add), reads=[sc_], writes=[sc_])
                                k.op("dve", lambda e, kk_=kk_: e.tensor_scalar(sc_[:, 8 + kk_:9 + kk_], sc_[:, 8 + kk_:9 + kk_], float(-NSLOT), None, ALU.add), reads=[sc_], writes=[sc_])
                                k.op("dve", lambda e, kk_=kk_: e.tensor_tensor(sc_[:, 8 + kk_:9 + kk_], sc_[:, 8 + kk_:9 + kk_], sc_[:, 12 + kk_:13 + kk_], ALU.mult), reads=[sc_], writes=[sc_])
                                k.op("dve", lambda e, kk_=kk_: e.tensor_scalar(slf[:, kk_:kk_ + 1], sc_[:, 8 + kk_:9 + kk_], float(NSLOT), None, ALU.add), reads=[sc_], writes=[slf])
                                k.op("dve", lambda e, kk_=kk_, tl=tl: e.tensor_tensor(wts[:, tl, kk_:kk_ + 1], sc_[:, 6 + kk_:7 + kk_], sc_[:, 12 + kk_:13 + kk_], ALU.mult), reads=[sc_], writes=[wts])
                            k.op("dve", lambda e, tl=tl: e.tensor_copy(sl_i[:, tl, :], slf[:]), reads=[slf], writes=[sl_i])
                            for kk_ in range(2):
                                k.dma("pool", lambda e, i=i, tl=tl, kk_=kk_: e.indirect_dma_start(
                                    out=XsD[:, :], out_offset=bass.IndirectOffsetOnAxis(ap=sl_i[:, tl, kk_:kk_ + 1], axis=0),
                                    in_=h2b[i][:, :], in_offset=None), reads=[h2b[i], sl_i], dst=r_xs)
                        if "slotD" in ext:
                            k.dma("sp", [lambda e: e.dma_start(out=slotD[t0 * 128:t0 * 128 + TR, 0:2].rearrange("(t p) k -> p t k", p=128), in_=wts[:], allow_slow_non_contiguous=True)],
                                  reads=[wts], dst=r_slot)

                    with k.scope() as s2:
                        wgu = [s2.sb(f"wgu{i}", [128, 16, 1024], BF16, dma=True) for i in range(2)]
                        wd = [s2.sb(f"wd{i}", [128, 4, D], BF16, dma=True) for i in range(2)]
                        xsl = [s2.sb(f"xsl{i}", [128, 2, D], BF16, dma=True) for i in range(2)]
                        xT = s2.sb("xT", [128, 16, 256], BF16)
                        sg_ = s2.sb("sg_", [128, 256], F32)
                        aT = s2.sb("aT", [128, 4, 256], BF16)
                        ysb = [s2.sb(f"ysb{i}", [128, D], F32) for i in range(2)]
                        ptp = [s2.ps(f"ptp{i}", [128, 8, 128], BF16) for i in range(2)]
                        gps = s2.ps("gps", [128, 256])
                        ups = s2.ps("ups", [128, 256])
                        yps = [s2.ps(f"yps{i}", [128, 512]) for i in range(2)]
                        for ex in range(NE):
                            i = ex % 2
                            gi, ei = ex // 8, ex % 8
                            wload(wgu[i], Wl["w_gu"][gi][ei], 1024, 4)
                            wload(wd[i], Wl["w_down"][gi][ei], D, 2)
                            s0 = ex * C_CAP
                            k.dma("sp", [lambda e, i=i, s=s, s0=s0: e.dma_start(out=xsl[i][:, s, :], in_=XsD[s0 + s * 128:s0 + (s + 1) * 128, :]) for s in range(2)],
                                  reads=[r_xs], dst=xsl[i])
                            for s in range(2):
                                for hf in range(2):
                                    for j in range(8):
                                        c = hf * 8 + j
                                        k.op("pe", lambda e, i=i, s=s, hf=hf, j=j, c=c: e.transpose(ptp[hf][:, j, :], xsl[i][:, s, c * 128:(c + 1) * 128], ident[:]),
                                             reads=[xsl[i], ident], writes=[ptp[hf]], inc=(j == 8 - 1))
                                    evac(xT[:, hf * 8:(hf + 1) * 8, s * 128:(s + 1) * 128], ptp[hf][:], [ptp[hf]], [xT])
                            for j in range(4):
                                for c in range(16):
                                    k.op("pe", lambda e, i=i, j=j, c=c: e.matmul(gps[:], wgu[i][:, c, j * 128:(j + 1) * 128], xT[:, c, :], start=(c == 0), stop=(c == 15)),
                                         reads=[wgu[i], xT], writes=[gps], inc=(c == 15))
                                for c in range(16):
                                    k.op("pe", lambda e, i=i, j=j, c=c: e.matmul(ups[:], wgu[i][:, c, 512 + j * 128:512 + (j + 1) * 128], xT[:, c, :], start=(c == 0), stop=(c == 15)),
                                         reads=[wgu[i], xT], writes=[ups], inc=(c == 15))
                                k.op("act", lambda e: e.activation(sg_[:], gps[:], AF.Silu), reads=[gps], writes=[sg_])
                                k.op("dve", lambda e, j=j: e.tensor_tensor(aT[:, j, :], sg_[:], ups[:], ALU.mult), reads=[sg_, ups], writes=[aT])
                            for s in range(2):
                                for n in range(4):
                                    yp = yps[n % 2]
                                    for j in range(4):
                                        k.op("pe", lambda e, i=i, s=s, n=n, j=j, yp=yp: e.matmul(yp[:], aT[:, j, s * 128:(s + 1) * 128], wd[i][:, j, n * 512:(n + 1) * 512], start=(j == 0), stop=(j == 3)),
                                             reads=[aT, wd[i]], writes=[yp], inc=(j == 3))
                                    evac(ysb[s][:, n * 512:(n + 1) * 512], yp[:], [yp], [ysb[s]])
                                k.dma("sp", lambda e, s=s, s0=s0: e.dma_start(out=YsD[s0 + s * 128:s0 + (s + 1) * 128, :], in_=ysb[s][:]), reads=[ysb[s]], dst=r_ys)

                    with k.scope() as s3:
                        gp = s3.sb("gp", [128, D], F32, dma=True)
                        y1 = [s3.sb(f"y1_{i}", [128, D], F32, dma=True) for i in range(2)]
                        y2 = [s3.sb(f"y2_{i}", [128, D], F32, dma=True) for i in range(2)]
                        xt = [s3.sb(f"xt{i}", [128, D], F32, dma=True) for i in range(2)]
                        xn = [s3.sb(f"xn{i}", [128, D], F32) for i in range(2)]
                        junk = s3.sb("junk", [128, D], F32)
                        st = s3.sb("st", [128, 2], F32)
                        fg = bcast(s3, "fg", fin_g, D) if last else None
                        for tl in range(TR // 128):
                            t = t0 + tl
                            b = t // 16
                            i = tl % 2
                            r0 = t * 128
                            if t % 16 == 0:
                                k.dma("sp", lambda e, b=b: e.dma_start(out=gp[:], in_=mod_row(l, b, 5).broadcast_to([128, D])), reads=[r_mod], dst=gp)
                                k.op("dve", lambda e: e.tensor_scalar(gp[:], gp[:], 1.0, None, ALU.add), reads=[gp], writes=[gp])
                            for kk_, yy in ((0, y1[i]), (1, y2[i])):
                                k.dma("pool", lambda e, yy=yy, tl=tl, kk_=kk_: e.indirect_dma_start(
                                    out=yy[:, :], out_offset=None, in_=YsD[:, :],
                                    in_offset=bass.IndirectOffsetOnAxis(ap=sl_i[:, tl, kk_:kk_ + 1], axis=0)), reads=[r_ys, sl_i], dst=yy)
                            k.dma("sp", lambda e, i=i, r0=r0: e.dma_start(out=xt[i][:], in_=xaD[r0:r0 + 128, :]), reads=[r_xa], dst=xt[i])
                            k.op("dve", lambda e, i=i, tl=tl: e.tensor_scalar(y1[i][:], y1[i][:], wts[:, tl, 0:1], None, ALU.mult), reads=[y1[i], wts], writes=[y1[i]])
                            k.op("dve", lambda e, i=i, tl=tl: e.scalar_tensor_tensor(y1[i][:], y2[i][:], wts[:, tl, 1:2], y1[i][:], ALU.mult, ALU.add), reads=[y1[i], y2[i], wts], writes=[y1[i]])
                            k.op("pool", lambda e, i=i: e.tensor_tensor(y1[i][:], y1[i][:], gp[:], ALU.mult), reads=[y1[i], gp], writes=[y1[i]])
                            k.op("dve", lambda e, i=i: e.tensor_tensor(xn[i][:], y1[i][:], xt[i][:], ALU.add), reads=[y1[i], xt[i]], writes=[xn[i]])
                            if last:
                                ln_stats(cst, xn[i], junk, st)
                                k.op("dve", lambda e, i=i: e.scalar_tensor_tensor(xn[i][:], xn[i][:], st[:, 1:2], fg[:], ALU.mult, ALU.mult), reads=[xn[i], st, fg], writes=[xn[i]])
                                k.dma("sp", lambda e, i=i, r0=r0: e.dma_start(out=out_d[r0:r0 + 128, :], in_=xn[i][:]), reads=[xn[i]], dst=r_out)
                            else:
                                k.dma("sp", lambda e, i=i, r0=r0: e.dma_start(out=xbD[r0:r0 + 128, :], in_=xn[i][:]), reads=[xn[i]], dst=r_xb)

            xin, r_xin = x_d, None
            for l in range(nl_attn):
                for b in range(NB):
                    attn_seq(l, b, xin, r_xin)
                merge_layer(l, xin, r_xin)
                if l < nl_moe:
                    last = (stop is None and l == NL - 1)
                    for rnd in range(NR):
                        moe_round(l, rnd, last)
                    xin, r_xin = xbD, r_xb
            if stop is not None:
                with k.scope() as sd:
                    t_ = sd.sb("dbgt", [128, D], F32, dma=True)
                    src, rs_ = (modD, r_mod) if stop == "mod" else ((xaD, r_xa) if stop == "attn" else (xbD, r_xb))
                    if stop == "mod":
                        k.dma("sp", lambda e: e.dma_start(out=t_[0:2 * NB, :], in_=modD.rearrange("l b (j n) -> (l b) j n", n=D)[:, 0, :]), reads=[r_mod], dst=t_)
                        k.dma("sp", lambda e: e.dma_start(out=out_d[0:2 * NB, :], in_=t_[0:2 * NB, :]), reads=[t_], dst=r_out)
                    else:
                        for t in range(NT):
                            k.dma("sp", lambda e, t=t: e.dma_start(out=t_[:], in_=src[t * 128:(t + 1) * 128, :]), reads=[rs_], dst=t_)
                            k.dma("sp", lambda e, t=t: e.dma_start(out=out_d[t * 128:(t + 1) * 128, :], in_=t_[:]), reads=[t_], dst=r_out)
        k.barrier()
    return nc


def make_in_maps(inputs, n_cores, NB, NL=2, NG=8, stop=None):
    f = lambda a: np.ascontiguousarray(a)
    shared = {}
    run_attn = stop != "mod"
    run_moe = stop not in ("mod", "attn")
    for l in range(NL if stop is None else 1):
        shared[f"w_ada_{l}"] = f(inputs["w_ada"][l])
        shared[f"b_ada_{l}"] = f(inputs["b_ada"][l][None])
        if run_attn:
            shared[f"ln1_g_{l}"] = f(inputs["ln1_g"][l][None])
            for n in ("w_in", "q_norm_g", "w_uq", "kv_norm_g", "w_ukv", "w_a_up", "w_b_up", "w_o"):
                shared[f"{n}_{l}"] = f(inputs[n][l])
        if run_moe:
            shared[f"ln2_g_{l}"] = f(inputs["ln2_g"][l][None])
            shared[f"w_grp_{l}"] = f(inputs["w_grp"][l])
            shared[f"b_grp_{l}"] = f(inputs["b_grp"][l][None])
            shared[f"w_exp_{l}"] = f(inputs["w_exp"][l])
            shared[f"b_exp_{l}"] = f(inputs["b_exp"][l][None])
            for g in range(NG):
                shared[f"w_gu_{l}_{g}"] = f(inputs["w_gu"][l][g * 8:(g + 1) * 8])
                shared[f"w_down_{l}_{g}"] = f(inputs["w_down"][l][g * 8:(g + 1) * 8])
    if stop is None:
        shared["final_g"] = f(inputs["final_g"][None])
    maps = []
    for c in range(n_cores):
        m = dict(shared)
        sl = slice(c * NB, (c + 1) * NB)
        m["x"] = f(inputs["x"][sl]).reshape(NB * S, D)
        m["c"] = f(inputs["c"][sl])
        m["positions"] = f(inputs["positions"][sl]).astype(np.int32)
        maps.append(m)
    return maps


def kernel(**inputs):
    inputs = {k_: np.asarray(v) for k_, v in inputs.items()}
    n = N_CORES
    NB = 16 // n
    nc = build(NB)
    maps = make_in_maps(inputs, n, NB)
    res = run_bass_kernel_spmd(nc, maps, core_ids=list(range(n)))
    out = np.concatenate([r["out"].reshape(NB, S, D) for r in res.results], axis=0)
    return out.astype(np.float32)
```

```python
import math
from contextlib import ExitStack, contextmanager

import numpy as np
import concourse.bass as bass
import concourse.mybir as mybir
from concourse.bass_utils import run_bass_kernel_spmd

F32 = mybir.dt.float32
BF16 = mybir.dt.bfloat16
I32 = mybir.dt.int32
ALU = mybir.AluOpType
AF = mybir.ActivationFunctionType
AX = mybir.AxisListType

S = 2048
D = 2048
NE_FULL = 64
C_CAP = 256
TR = 4096
EPS = 1e-6
BIG = 1.0e9
IN_COLS = 9792
N_CORES = 8
SAME_ENG_WAIT = True


class Res:
    def __init__(self, k, name, dma=False, multi=False):
        self.name = name
        self.w = None
        self.r = {}
        self.multi = multi
        self.sem = None
        self.cnt = 0
        if dma:
            self.sem, self.cnt = k.take_sem()
            k.live.append(self)


class Tl:
    def __init__(self, t, res):
        self.t = t
        self.res = res

    def __getitem__(self, i):
        return self.t[i]


class Eng:
    def __init__(self, k, name, h):
        self.name = name
        self.h = h
        self.sem = k.new_sem("e_" + name)
        self.cnt = 0
        self.waited = {}
        self.pend_r = []
        self.pend_w = []

    def wait(self, tok):
        if tok is None:
            return
        sem, val = tok
        if sem is self.sem and (self.name == "pe" or (self.name in ("act", "dve") and not SAME_ENG_WAIT)):
            return
        if self.waited.get(id(sem), 0) >= val:
            return
        self.waited[id(sem)] = val
        self.h.wait_ge(sem, val)


class Scope:
    def __init__(self, k):
        self.k = k
        self.stack = ExitStack()
        self.res = []

    def sb(self, name, shape, dt, dma=False):
        self.k.uid += 1
        t = self.stack.enter_context(self.k.nc.sbuf_tensor(f"{name}_{self.k.uid}", shape, dt))
        r = Res(self.k, name, dma)
        self.res.append(r)
        return Tl(t, r)

    def ps(self, name, shape, dt=F32):
        self.k.uid += 1
        t = self.stack.enter_context(self.k.nc.psum_tensor(f"{name}_{self.k.uid}", shape, dt))
        r = Res(self.k, name, False)
        self.res.append(r)
        return Tl(t, r)


class K:
    def __init__(self, nc, stack):
        self.nc = nc
        self.stack = stack
        self.free = []
        self.live = []
        self.uid = 0
        self.eng = {
            "pe": Eng(self, "pe", nc.tensor), "act": Eng(self, "act", nc.scalar),
            "dve": Eng(self, "dve", nc.vector), "pool": Eng(self, "pool", nc.gpsimd),
            "sp": Eng(self, "sp", nc.sync),
        }

    def new_sem(self, name):
        self.uid += 1
        self.nsem = getattr(self, "nsem", 0) + 1
        return self.stack.enter_context(self.nc.semaphore(f"{name}_{self.uid}"))

    def take_sem(self):
        while self.free:
            sem, cnt = self.free.pop()
            if cnt < 24000:
                return (sem, cnt)
        return (self.new_sem("d"), 0)

    def res(self, name, dma=True, multi=True):
        return Res(self, name, dma, multi)

    def barrier(self):
        toks = [(e.sem, e.cnt) for e in self.eng.values() if e.cnt]
        toks += [(r.sem, r.cnt) for r in self.live if r.cnt]
        for e in self.eng.values():
            for t in toks:
                e.wait(t)
        for e in self.eng.values():
            if e.cnt > 24000:
                e.sem = self.new_sem("e_" + e.name)
                e.cnt = 0

    @contextmanager
    def scope(self):
        sc = Scope(self)
        try:
            yield sc
        except BaseException:
            import traceback
            if not getattr(self, "_tb_done", False):
                traceback.print_exc()
                self._tb_done = True
            raise
        else:
            self.barrier()
            for r in sc.res:
                if r.sem is not None:
                    self.live.remove(r)
                    self.free.append((r.sem, r.cnt))
            sc.stack.close()

    @staticmethod
    def _r(x):
        return x if isinstance(x, Res) else x.res

    def _deps(self, e, reads, writes):
        for x in reads:
            e.wait(self._r(x).w)
        for x in writes:
            x = self._r(x)
            if not x.multi:
                e.wait(x.w)
            for tok in list(x.r.values()):
                e.wait(tok)

    def op(self, en, fn, reads=(), writes=(), inc=True):
        e = self.eng[en]
        self._deps(e, reads, writes)
        if not inc:
            fn(e.h)
            e.pend_r.extend(reads)
            e.pend_w.extend(writes)
            return None
        e.cnt += 1
        tok = (e.sem, e.cnt)
        fn(e.h).then_inc(e.sem, 1)
        for x in list(reads) + e.pend_r:
            self._r(x).r[id(e.sem)] = tok
        for x in list(writes) + e.pend_w:
            x = self._r(x)
            x.w = tok
            x.r = {}
        e.pend_r, e.pend_w = [], []
        return tok

    def dma(self, en, fns, reads=(), dst=None, inc=16):
        e = self.eng[en]
        d = self._r(dst)
        self._deps(e, reads, [d])
        if not isinstance(fns, (list, tuple)):
            fns = [fns]
        for fn in fns:
            d.cnt += inc
            fn(e.h).then_inc(d.sem, inc)
        tok = (d.sem, d.cnt)
        for x in reads:
            self._r(x).r[id(d.sem)] = tok
        d.w = tok
        if not d.multi:
            d.r = {}
        return tok


def build(NB, NL=2, NG=8, stop=None, ext=()):
    T = NB * S
    NT = T // 128
    NE = NG * 8
    NR = T // TR
    nc = bass.Bass("TRN2", target_bir_lowering=False)

    def din(name, shape, dt=F32):
        return nc.dram_tensor(name, shape, dt, kind="ExternalInput").ap()

    def dscr(name, shape, dt=F32):
        kind = "ExternalOutput" if name in ext else "Internal"
        return nc.dram_tensor(name, shape, dt, kind=kind).ap()

    run_attn = stop != "mod"
    run_moe = stop not in ("mod", "attn")
    nl_attn = NL if stop is None else (1 if run_attn else 0)
    nl_moe = NL if stop is None else (1 if run_moe else 0)

    x_d = din("x", [T, D])
    c_d = din("c", [NB, D])
    pos_d = din("positions", [NB, S], I32)
    W = {}
    for l in range(NL if stop is None else 1):
        W[l] = dict(
            w_ada=din(f"w_ada_{l}", [D, 6 * D]), b_ada=din(f"b_ada_{l}", [1, 6 * D]))
        if run_attn:
            W[l].update(
                ln1_g=din(f"ln1_g_{l}", [1, D]),
                w_in=din(f"w_in_{l}", [D, IN_COLS]),
                q_norm_g=din(f"q_norm_g_{l}", [512]), w_uq=din(f"w_uq_{l}", [512, 1536]),
                kv_norm_g=din(f"kv_norm_g_{l}", [512]), w_ukv=din(f"w_ukv_{l}", [512, 2048]),
                w_a_up=din(f"w_a_up_{l}", [512, D]), w_b_up=din(f"w_b_up_{l}", [1024, D]),
                w_o=din(f"w_o_{l}", [D, D]))
        if run_moe:
            W[l].update(
                ln2_g=din(f"ln2_g_{l}", [1, D]),
                w_grp=din(f"w_grp_{l}", [D, 8]), b_grp=din(f"b_grp_{l}", [1, 8]),
                w_exp=din(f"w_exp_{l}", [D, 64]), b_exp=din(f"b_exp_{l}", [1, 64]),
                w_gu=[din(f"w_gu_{l}_{g}", [8, D, 1024]) for g in range(NG)],
                w_down=[din(f"w_down_{l}_{g}", [8, 512, D]) for g in range(NG)])
    fin_g = din("final_g", [1, D]) if stop is None else None
    out_d = nc.dram_tensor("out", [T, D], F32, kind="ExternalOutput").ap()

    modD = dscr("modD", [2, NB, 6 * D])
    xaD = dscr("xaD", [T, D])
    xbD = dscr("xbD", [T, D])
    oD = [dscr(f"oD{g}", [T, 512]) for g in range(3)]
    lseD = [dscr(f"lseD{g}", [T, 4]) for g in range(3)]
    ybD = dscr("ybD", [T, 1024], BF16)
    gD = dscr("gD", [T, 4096], BF16)
    NSLOT = NE * C_CAP
    XsD = dscr("XsD", [NSLOT + 128, D], BF16)
    YsD = dscr("YsD", [NSLOT + 128, D])
    slotD = dscr("slotD", [T, 4])

    slopes = [2.0 ** (-8.0 * (n + 1) / 12.0) for n in range(12)]

    with ExitStack() as stack:
        k = K(nc, stack)
        r_mod = k.res("modD")
        r_xa = k.res("xaD")
        r_xb = k.res("xbD")
        r_o = [k.res(f"oD{g}") for g in range(3)]
        r_lse = [k.res(f"lseD{g}") for g in range(3)]
        r_yb = k.res("ybD")
        r_g = k.res("gD")
        r_xs = k.res("XsD")
        r_ys = k.res("YsD")
        r_out = k.res("out")
        r_slot = k.res("slotD")

        with k.scope() as g0:
            identf = g0.sb("identf", [128, 128], F32)
            ident = g0.sb("ident", [128, 128], BF16)
            onesf = g0.sb("onesf", [128, 128], F32)
            onesb = g0.sb("onesb", [128, 128], BF16)
            LTf = g0.sb("LTf", [128, 128], F32)
            LT = g0.sb("LT", [128, 128], BF16)
            mband = g0.sb("mband", [128, 384], F32)
            eCi = g0.sb("eCi", [128, 64], I32)
            eC = g0.sb("eC", [128, 64], F32)
            invi = g0.sb("invi", [64, 1], I32)
            inv = g0.sb("inv", [64, 1], F32)
            sgn = g0.sb("sgn", [64, 1], F32)

            k.op("pool", lambda e: e.memset(onesf[:], 1.0), writes=[onesf])
            k.op("pool", lambda e: e.memset(identf[:], 1.0), writes=[identf])
            k.op("pool", lambda e: e.affine_select(identf[:], identf[:], [[-1, 128]], ALU.is_equal, 0.0,
                                                    base=0, channel_multiplier=1), reads=[identf], writes=[identf])
            k.op("dve", lambda e: e.tensor_copy(ident[:], identf[:]), reads=[identf], writes=[ident])
            k.op("dve", lambda e: e.tensor_copy(onesb[:], onesf[:]), reads=[onesf], writes=[onesb])
            k.op("pool", lambda e: e.affine_select(LTf[:], onesf[:], [[1, 128]], ALU.is_ge, 0.0,
                                                    base=-1, channel_multiplier=-1), reads=[onesf], writes=[LTf])
            k.op("dve", lambda e: e.tensor_copy(LT[:], LTf[:]), reads=[LTf], writes=[LT])
            k.op("pool", lambda e: e.memset(mband[:], 0.0), writes=[mband])
            k.op("pool", lambda e: e.affine_select(mband[:], mband[:], [[1, 384]], ALU.is_ge, BIG,
                                                    base=-64, channel_multiplier=-1), reads=[mband], writes=[mband])
            k.op("pool", lambda e: e.affine_select(mband[:], mband[:], [[-1, 384]], ALU.is_ge, BIG,
                                                    base=192, channel_multiplier=1), reads=[mband], writes=[mband])
            k.op("pool", lambda e: e.iota(eCi[:], [[C_CAP, 64]], base=0, channel_multiplier=0), writes=[eCi])
            k.op("dve", lambda e: e.tensor_copy(eC[:], eCi[:]), reads=[eCi], writes=[eC])
            k.op("pool", lambda e: e.iota(invi[0:32, :], [[0, 1]], base=0, channel_multiplier=1), writes=[invi])
            k.op("pool", lambda e: e.iota(invi[32:64, :], [[0, 1]], base=0, channel_multiplier=1), writes=[invi])
            k.op("dve", lambda e: e.tensor_copy(inv[:], invi[:]), reads=[invi], writes=[inv])
            k.op("act", lambda e: e.activation(inv[:], inv[:], AF.Exp, scale=-math.log(10000.0) / 32.0),
                 reads=[inv], writes=[inv])
            k.op("pool", lambda e: e.memset(sgn[0:32, :], -1.0), writes=[sgn])
            k.op("pool", lambda e: e.memset(sgn[32:64, :], 1.0), writes=[sgn])
            epsD = g0.sb("epsD", [128, 1], F32)
            k.op("pool", lambda e: e.memset(epsD[:], EPS), writes=[epsD])
            cst = {"eps": epsD}
            if run_moe:
                with k.scope() as sz:
                    zeros = sz.sb("zeros", [128, D], F32)
                    k.op("pool", lambda e: e.memset(zeros[:], 0.0), writes=[zeros])
                    k.dma("sp", lambda e: e.dma_start(out=YsD[NSLOT:NSLOT + 128, :], in_=zeros[:]), reads=[zeros], dst=r_ys)

            def bcast(sc, name, row_ap, n, reads=()):
                t = sc.sb(name, [128, n], F32, dma=True)
                k.dma("sp", lambda e: e.dma_start(out=t[:], in_=row_ap.broadcast_to([128, n])), reads=reads, dst=t)
                return t

            rr = [0]

            def evac(out_ap, in_ap, reads, writes, scale=None, eng=None):
                rr[0] += 1
                if (eng == "act") or (eng is None and rr[0] % 2):
                    if scale is None:
                        k.op("act", lambda e: e.copy(out_ap, in_ap), reads=reads, writes=writes)
                    else:
                        k.op("act", lambda e: e.mul(out_ap, in_ap, scale), reads=reads, writes=writes)
                else:
                    if scale is None:
                        k.op("dve", lambda e: e.tensor_copy(out_ap, in_ap), reads=reads, writes=writes)
                    else:
                        k.op("dve", lambda e: e.tensor_scalar(out_ap, in_ap, scale, None, ALU.mult),
                             reads=reads, writes=writes)

            def wload(t, src_ap, ncol, nsplit=1):
                v = src_ap.rearrange("(c p) n -> p c n", p=128)
                step = ncol // nsplit
                k.dma("pool", [lambda e, i=i: e.dma_start(out=t[:, :, i * step:(i + 1) * step],
                                                          in_=v[:, :, i * step:(i + 1) * step])
                               for i in range(nsplit)], dst=t)

            with k.scope() as sm:
                cT = sm.sb("cT", [128, 16, NB], F32, dma=True)
                csT = sm.sb("csT", [128, 16, NB], F32)
                k.dma("sp", [lambda e, b=b: e.dma_start(out=cT[:, :, b], in_=c_d[b].rearrange("(c p) -> p c", p=128),
                                                        allow_slow_non_contiguous=True) for b in range(NB)], dst=cT)
                k.op("act", lambda e: e.activation(csT[:], cT[:], AF.Silu), reads=[cT], writes=[csT])
                csb = sm.sb("csb", [128, 16, NB], BF16)
                k.op("dve", lambda e: e.tensor_copy(csb[:], csT[:]), reads=[csT], writes=[csb])
                wb = [sm.sb(f"wada{i}", [128, 16, 512], BF16, dma=True) for i in range(3)]
                mps = [sm.ps(f"mps{i}", [NB, 512]) for i in range(2)]
                msb = [sm.sb(f"msb{i}", [NB, 512], F32) for i in range(2)]
                for l in W:
                    bsb = sm.sb(f"bsb{l}", [NB, 6 * D], F32, dma=True)
                    k.dma("sp", lambda e, l=l: e.dma_start(out=bsb[:], in_=W[l]["b_ada"].broadcast_to([NB, 6 * D])), dst=bsb)
                    wv = W[l]["w_ada"].rearrange("(c p) n -> p c n", p=128)
                    for n in range(24):
                        i = n % 2
                        wi_ = n % 3
                        k.dma("pool", [lambda e, n=n, i=wi_, h=h: e.dma_start(out=wb[i][:, h * 8:(h + 1) * 8, :],
                                                                          in_=wv[:, h * 8:(h + 1) * 8, n * 512:(n + 1) * 512])
                                     for h in range(2)], dst=wb[wi_])
                        for c in range(16):
                            k.op("pe", lambda e, c=c, i=i, wi_=wi_: e.matmul(mps[i][:], csb[:, c, :], wb[wi_][:, c, :],
                                                                    start=(c == 0), stop=(c == 15)),
                                 reads=[csb, wb[wi_]], writes=[mps[i]], inc=(c == 15))
                        k.op("dve", lambda e, n=n, i=i: e.tensor_tensor(msb[i][:], mps[i][:], bsb[:, n * 512:(n + 1) * 512], ALU.add),
                             reads=[mps[i], bsb], writes=[msb[i]])
                        k.dma("sp", lambda e, n=n, i=i, l=l: e.dma_start(out=modD[l, :, n * 512:(n + 1) * 512], in_=msb[i][:]),
                              reads=[msb[i]], dst=r_mod)

            def mod_row(l, b, j):
                return modD[l, b:b + 1, j * D:(j + 1) * D]

            def ln_stats(sc_tiles, xt, junk, st):
                k.op("act", lambda e: e.activation(junk[:], xt[:], AF.Square, accum_out=st[:, 0:1]),
                     reads=[xt], writes=[junk, st])
                k.op("act", lambda e: e.activation(st[:, 1:2], st[:, 0:1], AF.Sqrt, bias=sc_tiles["eps"][:, 0:1], scale=1.0 / D),
                     reads=[st, sc_tiles["eps"]], writes=[st])
                k.op("dve", lambda e: e.reciprocal(st[:, 1:2], st[:, 1:2]), reads=[st], writes=[st])

            def rope_tables(sc, b, cos_t, sin_t, scale):
                pki = sc.sb("pki", [64, S], I32, dma=True)
                ang = sc.sb("ang", [64, S], F32)
                kk = sc.sb("kk", [64, S], F32)
                kki = sc.sb("kki", [64, S], I32)
                k.dma("sp", lambda e: e.dma_start(out=pki[:], in_=pos_d[b:b + 1, :].broadcast_to([64, S])), dst=pki)
                k.op("dve", lambda e: e.tensor_copy(ang[:], pki[:]), reads=[pki], writes=[ang])
                k.op("dve", lambda e: e.tensor_scalar(ang[:], ang[:], inv[:, 0:1], None, ALU.mult), reads=[ang, inv], writes=[ang])
                TWO_PI = 2.0 * math.pi

                def reduce_sin(dst, shift):
                    k.op("dve", lambda e: e.tensor_scalar(kk[:], ang[:], shift, 1.0 / TWO_PI, ALU.add, ALU.mult), reads=[ang], writes=[kk])
                    k.op("dve", lambda e: e.tensor_copy(kki[:], kk[:]), reads=[kk], writes=[kki])
                    k.op("dve", lambda e: e.tensor_copy(kk[:], kki[:]), reads=[kki], writes=[kk])
                    k.op("dve", lambda e: e.scalar_tensor_tensor(kk[:], kk[:], -TWO_PI, ang[:], ALU.mult, ALU.add), reads=[kk, ang], writes=[kk])
                    if shift:
                        k.op("dve", lambda e: e.tensor_scalar(kk[:], kk[:], shift, None, ALU.add), reads=[kk], writes=[kk])
                    k.op("dve", lambda e: e.tensor_scalar(dst[:], kk[:], math.pi, -TWO_PI, ALU.is_gt, ALU.mult), reads=[kk], writes=[dst])
                    k.op("dve", lambda e: e.tensor_tensor(kk[:], kk[:], dst[:], ALU.add), reads=[kk, dst], writes=[kk])
                    k.op("dve", lambda e: e.tensor_scalar(dst[:], kk[:], -math.pi, TWO_PI, ALU.is_lt, ALU.mult), reads=[kk], writes=[dst])
                    k.op("dve", lambda e: e.tensor_tensor(kk[:], kk[:], dst[:], ALU.add), reads=[kk, dst], writes=[kk])
                    k.op("dve", lambda e: e.tensor_scalar(kk[:], kk[:], math.pi, -math.pi, ALU.min, ALU.max), reads=[kk], writes=[kk])
                    k.op("act", lambda e: e.activation(dst[:], kk[:], AF.Sin), reads=[kk], writes=[dst])

                reduce_sin(sin_t, 0.0)
                reduce_sin(cos_t, math.pi / 2.0)
                k.op("dve", lambda e: e.tensor_scalar(sin_t[:], sin_t[:], sgn[:, 0:1], scale, ALU.mult, ALU.mult), reads=[sin_t, sgn], writes=[sin_t])
                if scale != 1.0:
                    k.op("dve", lambda e: e.tensor_scalar(cos_t[:], cos_t[:], scale, None, ALU.mult), reads=[cos_t], writes=[cos_t])

            def attn_seq(l, b, xin, r_xin):
                Wl = W[l]
                win = Wl["w_in"]
                with k.scope() as so:
                    cqn = so.sb("cqn", [128, 4, S], BF16)
                    ckvn = so.sb("ckvn", [128, 4, S], BF16)
                    krT = so.sb("krT", [64, S], BF16)
                    with k.scope() as sh:
                        hT = sh.sb("hT", [128, 16, S], BF16)
                        with k.scope() as s1:
                            A1 = bcast(s1, "A1", mod_row(l, b, 1), D, reads=[r_mod])
                            B1 = bcast(s1, "B1", mod_row(l, b, 0), D, reads=[r_mod])
                            G1 = bcast(s1, "G1", Wl["ln1_g"], D)
                            k.op("dve", lambda e: e.scalar_tensor_tensor(A1[:], A1[:], 1.0, G1[:], ALU.add, ALU.mult),
                                 reads=[A1, G1], writes=[A1])
                            xt = [s1.sb(f"xt{i}", [128, D], F32, dma=True) for i in range(2)]
                            junk = s1.sb("junk", [128, D], F32)
                            hb = [s1.sb(f"hb{i}", [128, D], BF16) for i in range(2)]
                            st = [s1.sb(f"st{i}", [128, 2], F32) for i in range(2)]
                            pT = [s1.ps(f"pT{i}", [128, 8, 128], BF16) for i in range(2)]
                            for tt in range(16):
                                i = tt % 2
                                r0 = b * S + tt * 128
                                k.dma("sp", lambda e, i=i, r0=r0: e.dma_start(out=xt[i][:], in_=xin[r0:r0 + 128, :]),
                                      reads=[r_xin] if r_xin else [], dst=xt[i])
                                ln_stats(cst, xt[i], junk, st[i])
                                k.op("dve", lambda e, i=i: e.scalar_tensor_tensor(junk[:], xt[i][:], st[i][:, 1:2], A1[:], ALU.mult, ALU.mult),
                                     reads=[xt[i], st[i], A1], writes=[junk])
                                k.op("dve", lambda e, i=i: e.tensor_tensor(hb[i][:], junk[:], B1[:], ALU.add),
                                     reads=[junk, B1], writes=[hb[i]])
                                for hf in range(2):
                                    for j in range(8):
                                        c = hf * 8 + j
                                        k.op("pe", lambda e, i=i, hf=hf, j=j, c=c: e.transpose(pT[hf][:, j, :], hb[i][:, c * 128:(c + 1) * 128], ident[:]),
                                             reads=[hb[i], ident], writes=[pT[hf]], inc=(j == 8 - 1))
                                    evac(hT[:, hf * 8:(hf + 1) * 8, tt * 128:(tt + 1) * 128], pT[hf][:], [pT[hf]], [hT])

                        for g, dil in enumerate((1, 4, 16)):
                            Lc = S // dil
                            tpc = Lc // 128

                            def hblk(c, tb_):
                                if dil == 1:
                                    return hT[:, c, tb_ * 512:(tb_ + 1) * 512]
                                v = hT[:, c, :].rearrange("p (i d) -> p d i", d=dil)
                                if dil == 4:
                                    return v[:, tb_, :]
                                return v[:, 4 * tb_:4 * tb_ + 4, :]

                            def htile(c, tt):
                                if dil == 1:
                                    return hT[:, c, tt * 128:(tt + 1) * 128]
                                v = hT[:, c, :].rearrange("p (i d) -> p d i", d=dil)
                                if dil == 4:
                                    return v[:, tt // 4, (tt % 4) * 128:(tt % 4 + 1) * 128]
                                return v[:, tt, :]

                            def pso(ps):
                                return ps[:].rearrange("p (r i) -> p r i", r=4) if dil == 16 else ps[:]

                            with k.scope() as sg:
                                qT = sg.sb("qT", [128, 4, S], BF16)
                                kT = sg.sb("kT", [128, 4, S], BF16)
                                V = sg.sb("V", [128, 16, 512], BF16)
                                with k.scope() as sw:
                                    wq = sw.sb("wq", [128, 16, 512], BF16, dma=True)
                                    wk = sw.sb("wk", [128, 16, 512], BF16, dma=True)
                                    wv = sw.sb("wv", [128, 16, 512], BF16, dma=True)
                                    wload(wq, win[:, g * 512:(g + 1) * 512], 512)
                                    wload(wk, win[:, 1536 + g * 512:1536 + (g + 1) * 512], 512)
                                    wload(wv, win[:, 3072 + g * 512:3072 + (g + 1) * 512], 512)
                                    pp = [sw.ps(f"pp{i}", [128, 512]) for i in range(2)]
                                    n = 0
                                    for (w_, dst, scl) in ((wq, qT, 128.0 ** -0.5), (wk, kT, None)):
                                        for h in range(4):
                                            for tb_ in range(4):
                                                ps = pp[n % 2]
                                                n += 1
                                                for c in range(16):
                                                    k.op("pe", lambda e, ps=ps, w_=w_, h=h, c=c, tb_=tb_: e.matmul(
                                                        pso(ps), w_[:, c, h * 128:(h + 1) * 128], hblk(c, tb_), start=(c == 0), stop=(c == 15)),
                                                        reads=[w_, hT], writes=[ps], inc=(c == 15))
                                                evac(dst[:, h, tb_ * 512:(tb_ + 1) * 512], ps[:], [ps], [dst], scale=scl)
                                    for tt in range(16):
                                        ps = pp[n % 2]
                                        n += 1
                                        for c in range(16):
                                            k.op("pe", lambda e, ps=ps, c=c, tt=tt: e.matmul(ps[:], htile(c, tt), wv[:, c, :], start=(c == 0), stop=(c == 15)),
                                                 reads=[wv, hT], writes=[ps], inc=(c == 15))
                                        evac(V[:, tt, :], ps[:], [ps], [V])
                                pqi = sg.sb("pqi", [128, 16], I32, dma=True)
                                pq = sg.sb("pq", [128, 16], F32)
                                pki = sg.sb("pki", [128, S], I32, dma=True)
                                pk = sg.sb("pk", [128, S], F32)
                                pb = pos_d[b]
                                if dil == 1:
                                    k.dma("sp", lambda e: e.dma_start(out=pqi[:], in_=pb.rearrange("(t a) -> a t", a=128), allow_slow_non_contiguous=True), dst=pqi)
                                elif dil == 4:
                                    k.dma("sp", lambda e: e.dma_start(out=pqi[:].rearrange("a (r q) -> a r q", r=4),
                                                                      in_=pb.rearrange("(q a r) -> a r q", q=4, a=128, r=4), allow_slow_non_contiguous=True), dst=pqi)
                                else:
                                    k.dma("sp", lambda e: e.dma_start(out=pqi[:], in_=pb.rearrange("(a r) -> a r", r=16)), dst=pqi)
                                k.dma("sp", lambda e: e.dma_start(out=pki[:], in_=pos_d[b:b + 1, :].broadcast_to([128, S])), dst=pki)
                                k.op("dve", lambda e: e.tensor_copy(pq[:], pqi[:]), reads=[pqi], writes=[pq])
                                k.op("dve", lambda e: e.tensor_scalar(pq[:], pq[:], -1.0, None, ALU.mult), reads=[pq], writes=[pq])
                                k.op("dve", lambda e: e.tensor_copy(pk[:], pki[:]), reads=[pki], writes=[pk])
                                dist = [sg.sb(f"dist{i}", [128, 384], F32) for i in range(2)]
                                ssb = [sg.sb(f"ssb{i}", [128, 384], F32) for i in range(2)]
                                pb16 = [sg.sb(f"pb{i}", [128, 384], BF16) for i in range(2)]
                                pts = [sg.sb(f"pts{i}", [128, 3, 128], BF16) for i in range(2)]
                                og = [sg.sb(f"og{i}", [128, 512], F32) for i in range(2)]
                                lse = [sg.sb(f"lse{i}", [128, 4], F32) for i in range(2)]
                                sm_ = [[sg.sb(f"sm{i}_{j}", [128, 1], F32) for j in range(5)] for i in range(2)]
                                sps = [sg.ps(f"sps{i}", [128, 512]) for i in range(2)]
                                ptp = [sg.ps(f"ptp{i}", [128, 4, 128], BF16) for i in range(2)]
                                ops = [sg.ps(f"ops{i}", [128, 512]) for i in range(2)]
                                for tt in range(16):
                                    it = tt % 2
                                    cls, ti = tt // tpc, tt % tpc
                                    i0 = ti * 128
                                    pbase = cls * Lc
                                    lo, hi = max(0, i0 - 128), min(Lc, i0 + 256)
                                    w = hi - lo
                                    c0 = 128 - (i0 - lo)
                                    nch = w // 128
                                    if dil == 1:
                                        pkv = pk[:, lo:hi]
                                    else:
                                        pkv = pk[:, :].rearrange("p (i d) -> p d i", d=dil)[:, cls, lo:hi]
                                    k.op("act", lambda e, it=it, pkv=pkv, tt=tt, w=w: e.activation(dist[it][:, :w], pkv, AF.Abs, bias=pq[:, tt:tt + 1], scale=1.0),
                                         reads=[pk, pq], writes=[dist[it]])
                                    k.op("dve", lambda e, it=it, w=w, c0=c0: e.tensor_tensor(dist[it][:, :w], dist[it][:, :w], mband[:, c0:c0 + w], ALU.add),
                                         reads=[dist[it], mband], writes=[dist[it]])
                                    for h in range(4):
                                        ih = h % 2
                                        mx, nmx, ll, rl, lnl = sm_[ih]
                                        slope = slopes[g * 4 + h]
                                        q0 = pbase + i0
                                        k.op("pe", lambda e, ih=ih, h=h, q0=q0, w=w, lo=lo, pbase=pbase: e.matmul(
                                            sps[ih][:, :w], qT[:, h, q0:q0 + 128], kT[:, h, pbase + lo:pbase + lo + w], start=True, stop=True),
                                            reads=[qT, kT], writes=[sps[ih]])
                                        k.op("dve", lambda e, ih=ih, it=it, w=w, slope=slope: e.scalar_tensor_tensor(
                                            ssb[ih][:, :w], dist[it][:, :w], -slope, sps[ih][:, :w], ALU.mult, ALU.add),
                                            reads=[dist[it], sps[ih]], writes=[ssb[ih]])
                                        k.op("dve", lambda e, ih=ih, w=w, mx=mx: e.reduce_max(mx[:], ssb[ih][:, :w], AX.X), reads=[ssb[ih]], writes=[mx])
                                        k.op("dve", lambda e, mx=mx, nmx=nmx: e.tensor_scalar(nmx[:], mx[:], -1.0, None, ALU.mult), reads=[mx], writes=[nmx])
                                        k.op("act", lambda e, ih=ih, w=w, nmx=nmx, ll=ll: e.activation(pb16[ih][:, :w], ssb[ih][:, :w], AF.Exp, bias=nmx[:, 0:1], scale=1.0, accum_out=ll[:, 0:1]),
                                             reads=[ssb[ih], nmx], writes=[pb16[ih], ll])
                                        for j in range(nch):
                                            k.op("pe", lambda e, ih=ih, j=j: e.transpose(ptp[ih][:, j, :], pb16[ih][:, j * 128:(j + 1) * 128], ident[:]),
                                                 reads=[pb16[ih], ident], writes=[ptp[ih]], inc=(j == nch - 1))
                                        evac(pts[ih][:, :nch, :], ptp[ih][:, :nch, :], [ptp[ih]], [pts[ih]])
                                        vt0 = (pbase + lo) // 128
                                        for j in range(nch):
                                            k.op("pe", lambda e, it=it, ih=ih, j=j, h=h, vt0=vt0, nch=nch: e.matmul(
                                                ops[it][:, h * 128:(h + 1) * 128], pts[ih][:, j, :], V[:, vt0 + j, h * 128:(h + 1) * 128],
                                                start=(j == 0), stop=(j == nch - 1)), reads=[pts[ih], V], writes=[ops[it]], inc=(j == nch - 1))
                                        k.op("dve", lambda e, ll=ll, rl=rl: e.reciprocal(rl[:], ll[:]), reads=[ll], writes=[rl])
                                        k.op("dve", lambda e, it=it, h=h, rl=rl: e.tensor_scalar(og[it][:, h * 128:(h + 1) * 128], ops[it][:, h * 128:(h + 1) * 128], rl[:, 0:1], None, ALU.mult),
                                             reads=[ops[it], rl], writes=[og[it]])
                                        k.op("act", lambda e, ll=ll, lnl=lnl: e.activation(lnl[:], ll[:], AF.Ln), reads=[ll], writes=[lnl])
                                        k.op("dve", lambda e, it=it, h=h, mx=mx, lnl=lnl: e.tensor_tensor(lse[it][:, h:h + 1], mx[:], lnl[:], ALU.add),
                                             reads=[mx, lnl], writes=[lse[it]])
                                    ob = oD[g][b * S:(b + 1) * S, :]
                                    lb = lseD[g][b * S:(b + 1) * S, :]
                                    if dil == 1:
                                        orow, lrow = ob[tt * 128:(tt + 1) * 128, :], lb[tt * 128:(tt + 1) * 128, :]
                                    else:
                                        orow = ob.rearrange("(i d) f -> d i f", d=dil)[cls, i0:i0 + 128, :]
                                        lrow = lb.rearrange("(i d) f -> d i f", d=dil)[cls, i0:i0 + 128, :]
                                    k.dma("sp", lambda e, it=it, orow=orow: e.dma_start(out=orow, in_=og[it][:]), reads=[og[it]], dst=r_o[g])
                                    k.dma("sp", lambda e, it=it, lrow=lrow: e.dma_start(out=lrow, in_=lse[it][:]), reads=[lse[it]], dst=r_lse[g])

                        with k.scope() as sl:
                            cosk = sl.sb("cosk", [64, S], F32)
                            sink = sl.sb("sink", [64, S], F32)
                            with k.scope() as srt:
                                rope_tables(srt, b, cosk, sink, 1.0)
                            wcq = sl.sb("wcq", [128, 16, 512], BF16, dma=True)
                            wckv = sl.sb("wckv", [128, 16, 512], BF16, dma=True)
                            wkr = sl.sb("wkr", [128, 16, 128], BF16, dma=True)
                            wload(wcq, win[:, 4608:5120], 512)
                            wload(wckv, win[:, 5120:5632], 512)
                            wv_ = win.rearrange("(c p) n -> p c n", p=128)
                            k.dma("pool", [lambda e: e.dma_start(out=wkr[:, :, 0:64], in_=wv_[:, :, 5632:5696]),
                                           lambda e: e.dma_start(out=wkr[:, :, 64:96], in_=wv_[:, :, 5664:5696]),
                                           lambda e: e.dma_start(out=wkr[:, :, 96:128], in_=wv_[:, :, 5632:5664])], dst=wkr)
                            gq = sl.sb("gq", [128, 4], F32, dma=True)
                            gkv = sl.sb("gkv", [128, 4], F32, dma=True)
                            k.dma("sp", lambda e: e.dma_start(out=gq[:], in_=Wl["q_norm_g"].rearrange("(c p) -> p c", p=128), allow_slow_non_contiguous=True), dst=gq)
                            k.dma("sp", lambda e: e.dma_start(out=gkv[:], in_=Wl["kv_norm_g"].rearrange("(c p) -> p c", p=128), allow_slow_non_contiguous=True), dst=gkv)
                            latf = sl.sb("latf", [128, 4, 512], F32)
                            sq = sl.sb("sq", [128, 4, 512], F32)
                            rs = sl.sb("rs", [128, 512], F32)
                            t1 = sl.sb("t1", [64, 512], F32)
                            t2 = sl.sb("t2", [64, 512], F32)
                            lps = [sl.ps(f"lps{i}", [128, 512]) for i in range(4)]
                            sps_ = sl.ps("ssq", [128, 512])
                            psr = sl.ps("psr", [64, 512])
                            pss = sl.ps("pss", [64, 512])
                            for (w_, gv, dst) in ((wcq, gq, cqn), (wckv, gkv, ckvn)):
                                for tb_ in range(4):
                                    blk = slice(tb_ * 512, (tb_ + 1) * 512)
                                    for c4 in range(4):
                                        for c in range(16):
                                            k.op("pe", lambda e, c4=c4, c=c, w_=w_, blk=blk: e.matmul(lps[c4][:], w_[:, c, c4 * 128:(c4 + 1) * 128], hT[:, c, blk], start=(c == 0), stop=(c == 15)),
                                                 reads=[w_, hT], writes=[lps[c4]], inc=(c == 15))
                                        k.op("act", lambda e, c4=c4: e.copy(latf[:, c4, :], lps[c4][:]), reads=[lps[c4]], writes=[latf])
                                        k.op("act", lambda e, c4=c4: e.activation(sq[:, c4, :], lps[c4][:], AF.Square), reads=[lps[c4]], writes=[sq])
                                    for c4 in range(4):
                                        k.op("pe", lambda e, c4=c4: e.matmul(sps_[:], onesf[:], sq[:, c4, :], start=(c4 == 0), stop=(c4 == 3)),
                                             reads=[onesf, sq], writes=[sps_], inc=(c4 == 3))
                                    k.op("act", lambda e: e.activation(rs[:], sps_[:], AF.Sqrt, bias=epsD[:, 0:1], scale=1.0 / 512.0), reads=[sps_, epsD], writes=[rs])
                                    k.op("dve", lambda e: e.reciprocal(rs[:], rs[:]), reads=[rs], writes=[rs])
                                    for c4 in range(4):
                                        k.op("dve", lambda e, c4=c4, gv=gv, dst=dst, blk=blk: e.scalar_tensor_tensor(dst[:, c4, blk], latf[:, c4, :], gv[:, c4:c4 + 1], rs[:], ALU.mult, ALU.mult),
                                             reads=[latf, gv, rs], writes=[dst])
                            for tb_ in range(4):
                                blk = slice(tb_ * 512, (tb_ + 1) * 512)
                                for c in range(16):
                                    k.op("pe", lambda e, c=c, blk=blk: e.matmul(psr[:], wkr[:, c, 0:64], hT[:, c, blk], start=(c == 0), stop=(c == 15)), reads=[wkr, hT], writes=[psr], inc=(c == 15))
                                for c in range(16):
                                    k.op("pe", lambda e, c=c, blk=blk: e.matmul(pss[:], wkr[:, c, 64:128], hT[:, c, blk], start=(c == 0), stop=(c == 15)), reads=[wkr, hT], writes=[pss], inc=(c == 15))
                                k.op("dve", lambda e, blk=blk: e.tensor_tensor(t1[:], psr[:], cosk[:, blk], ALU.mult), reads=[psr, cosk], writes=[t1])
                                k.op("dve", lambda e, blk=blk: e.tensor_tensor(t2[:], pss[:], sink[:, blk], ALU.mult), reads=[pss, sink], writes=[t2])
                                k.op("dve", lambda e, blk=blk: e.tensor_tensor(krT[:, blk], t1[:], t2[:], ALU.add), reads=[t1, t2], writes=[krT])

                        with k.scope() as sgt:
                            wg = [sgt.sb(f"wg{i}", [128, 16, 512], BF16, dma=True) for i in range(2)]
                            gsb = [sgt.sb(f"gsb{i}", [128, 512], BF16) for i in range(2)]
                            gps = [sgt.ps(f"gps{i}", [128, 512]) for i in range(2)]
                            m = 0
                            for n in range(8):
                                wi = wg[n % 2]
                                wload(wi, win[:, 5696 + n * 512:5696 + (n + 1) * 512], 512)
                                for tt in range(16):
                                    i = m % 2
                                    m += 1
                                    for c in range(16):
                                        k.op("pe", lambda e, i=i, c=c, tt=tt, wi=wi: e.matmul(gps[i][:], hT[:, c, tt * 128:(tt + 1) * 128], wi[:, c, :], start=(c == 0), stop=(c == 15)),
                                             reads=[hT, wi], writes=[gps[i]], inc=(c == 15))
                                    k.op("act", lambda e, i=i: e.activation(gsb[i][:], gps[i][:], AF.Sigmoid), reads=[gps[i]], writes=[gsb[i]])
                                    r0 = b * S + tt * 128
                                    k.dma("sp", lambda e, i=i, r0=r0, n=n: e.dma_start(out=gD[r0:r0 + 128, n * 512:(n + 1) * 512], in_=gsb[i][:]), reads=[gsb[i]], dst=r_g)

                    with k.scope() as sa:
                        cosq = sa.sb("cosq", [64, S], F32)
                        sinq = sa.sb("sinq", [64, S], F32)
                        with k.scope() as srt:
                            rope_tables(srt, b, cosq, sinq, 192.0 ** -0.5)
                        wuq = sa.sb("wuq", [128, 4, 1536], BF16, dma=True)
                        wuqs = sa.sb("wuqs", [128, 4, 8, 64], BF16, dma=True)
                        wukv = sa.sb("wukv", [128, 4, 2048], BF16, dma=True)
                        wload(wuq, Wl["w_uq"], 1536)
                        wload(wukv, Wl["w_ukv"], 2048)
                        uqv = Wl["w_uq"].rearrange("(c p) (h x) -> p c h x", p=128, x=192)
                        k.dma("pool", [lambda e, c=c: e.dma_start(out=wuqs[:, c, :, 0:32], in_=uqv[:, c, :, 160:192]) for c in range(4)]
                              + [lambda e, c=c: e.dma_start(out=wuqs[:, c, :, 32:64], in_=uqv[:, c, :, 128:160]) for c in range(4)], dst=wuqs)
                        wukv_h = wukv[:, :, :].rearrange("p c (h x) -> p c h x", x=256)
                        for hh in range(2):
                            with k.scope() as sh2:
                                qnT = sh2.sb("qnT", [128, 4, S], BF16)
                                qrT = sh2.sb("qrT", [64, 4, S], BF16)
                                knT = sh2.sb("knT", [128, 4, S], BF16)
                                Vb = sh2.sb("Vb", [128, 16, 512], BF16)
                                with k.scope() as sp_:
                                    pn = [sp_.ps(f"pn{i}", [128, 512]) for i in range(2)]
                                    pr = sp_.ps("pr", [64, 512])
                                    pz = sp_.ps("pz", [64, 512])
                                    t1 = sp_.sb("t1", [64, 512], F32)
                                    t2 = sp_.sb("t2", [64, 512], F32)
                                    n = 0
                                    for hl in range(4):
                                        h = hh * 4 + hl
                                        for tb_ in range(4):
                                            blk = slice(tb_ * 512, (tb_ + 1) * 512)
                                            ps = pn[n % 2]
                                            n += 1
                                            for c in range(4):
                                                k.op("pe", lambda e, ps=ps, c=c, h=h, blk=blk: e.matmul(ps[:], wuq[:, c, h * 192:h * 192 + 128], cqn[:, c, blk], start=(c == 0), stop=(c == 3)),
                                                     reads=[wuq, cqn], writes=[ps], inc=(c == 3))
                                            evac(qnT[:, hl, blk], ps[:], [ps], [qnT], scale=192.0 ** -0.5)
                                            for c in range(4):
                                                k.op("pe", lambda e, c=c, h=h, blk=blk: e.matmul(pr[:], wuq[:, c, h * 192 + 128:h * 192 + 192], cqn[:, c, blk], start=(c == 0), stop=(c == 3)),
                                                     reads=[wuq, cqn], writes=[pr], inc=(c == 3))
                                            for c in range(4):
                                                k.op("pe", lambda e, c=c, h=h, blk=blk: e.matmul(pz[:], wuqs[:, c, h, :], cqn[:, c, blk], start=(c == 0), stop=(c == 3)),
                                                     reads=[wuqs, cqn], writes=[pz], inc=(c == 3))
                                            k.op("dve", lambda e, blk=blk: e.tensor_tensor(t1[:], pr[:], cosq[:, blk], ALU.mult), reads=[pr, cosq], writes=[t1])
                                            k.op("dve", lambda e, blk=blk: e.tensor_tensor(t2[:], pz[:], sinq[:, blk], ALU.mult), reads=[pz, sinq], writes=[t2])
                                            k.op("dve", lambda e, blk=blk, hl=hl: e.tensor_tensor(qrT[:, hl, blk], t1[:], t2[:], ALU.add), reads=[t1, t2], writes=[qrT])
                                            ps = pn[n % 2]
                                            n += 1
                                            for c in range(4):
                                                k.op("pe", lambda e, ps=ps, c=c, h=h, blk=blk: e.matmul(ps[:], wukv[:, c, h * 256:h * 256 + 128], ckvn[:, c, blk], start=(c == 0), stop=(c == 3)),
                                                     reads=[wukv, ckvn], writes=[ps], inc=(c == 3))
                                            evac(knT[:, hl, blk], ps[:], [ps], [knT])
                                    for tt in range(16):
                                        ps = pn[n % 2]
                                        n += 1
                                        for c in range(4):
                                            k.op("pe", lambda e, ps=ps, c=c, tt=tt: e.matmul(ps[:].rearrange("p (h x) -> p h x", h=4), ckvn[:, c, tt * 128:(tt + 1) * 128],
                                                                                          wukv_h[:, c, hh * 4:(hh + 1) * 4, 128:256], start=(c == 0), stop=(c == 3)),
                                                 reads=[wukv, ckvn], writes=[ps], inc=(c == 3))
                                        evac(Vb[:, tt, :], ps[:], [ps], [Vb])
                                with k.scope() as sat:
                                    Sps = sat.ps("Sps", [128, S])
                                    ptp = [sat.ps(f"ptp{i}", [128, 8, 128], BF16) for i in range(2)]
                                    ops = sat.ps("ops", [128, 512])
                                    P = [sat.sb(f"P{i}", [128, S], BF16) for i in range(2)]
                                    pts = [sat.sb(f"pts{i}", [128, 16, 128], BF16) for i in range(2)]
                                    yb = [sat.sb(f"yb{i}", [128, 512], BF16) for i in range(2)]
                                    sm_ = [[sat.sb(f"sm{i}_{j}", [128, 1], F32) for j in range(4)] for i in range(2)]
                                    steps = [(qt, hl) for qt in range(16) for hl in range(4)]

                                    def stage_a1(i):
                                        qt, hl = steps[i]
                                        ih = i % 2
                                        qs = slice(qt * 128, (qt + 1) * 128)
                                        mx, nmx, ll, rl = sm_[ih]
                                        for nk in range(4):
                                            ks = slice(nk * 512, (nk + 1) * 512)
                                            k.op("pe", lambda e, hl=hl, qs=qs, ks=ks: e.matmul(Sps[:, ks], qnT[:, hl, qs], knT[:, hl, ks], start=True, stop=False),
                                                 reads=[qnT, knT], writes=[Sps], inc=False)
                                            k.op("pe", lambda e, hl=hl, qs=qs, ks=ks: e.matmul(Sps[:, ks], qrT[:, hl, qs], krT[:, ks], start=False, stop=True),
                                                 reads=[qrT, krT], writes=[Sps], inc=(nk == 3))
                                        k.op("dve", lambda e, mx=mx: e.reduce_max(mx[:], Sps[:], AX.X), reads=[Sps], writes=[mx])
                                        k.op("dve", lambda e, mx=mx, nmx=nmx: e.tensor_scalar(nmx[:], mx[:], -1.0, None, ALU.mult), reads=[mx], writes=[nmx])

                                    def stage_a2(i):
                                        ih = i % 2
                                        mx, nmx, ll, rl = sm_[ih]
                                        k.op("act", lambda e, ih=ih, nmx=nmx, ll=ll: e.activation(P[ih][:], Sps[:], AF.Exp, bias=nmx[:, 0:1], scale=1.0, accum_out=ll[:, 0:1]),
                                             reads=[Sps, nmx], writes=[P[ih], ll])

                                    def stage_b1(i):
                                        ih = i % 2
                                        for hf in range(2):
                                            for j in range(8):
                                                c = hf * 8 + j
                                                k.op("pe", lambda e, ih=ih, hf=hf, j=j, c=c: e.transpose(ptp[hf][:, j, :], P[ih][:, c * 128:(c + 1) * 128], ident[:]),
                                                     reads=[P[ih], ident], writes=[ptp[hf]], inc=(j == 7))
                                            evac(pts[ih][:, hf * 8:(hf + 1) * 8, :], ptp[hf][:], [ptp[hf]], [pts[ih]], eng="act")

                                    def stage_b2(i):
                                        qt, hl = steps[i]
                                        ih = i % 2
                                        iy = qt % 2
                                        mx, nmx, ll, rl = sm_[ih]
                                        for c in range(16):
                                            k.op("pe", lambda e, ih=ih, c=c, hl=hl: e.matmul(ops[:, hl * 128:(hl + 1) * 128], pts[ih][:, c, :], Vb[:, c, hl * 128:(hl + 1) * 128], start=(c == 0), stop=(c == 15)),
                                                 reads=[pts[ih], Vb], writes=[ops], inc=(c == 15))
                                        k.op("dve", lambda e, ll=ll, rl=rl: e.reciprocal(rl[:], ll[:]), reads=[ll], writes=[rl])
                                        k.op("dve", lambda e, iy=iy, hl=hl, rl=rl: e.tensor_scalar(yb[iy][:, hl * 128:(hl + 1) * 128], ops[:, hl * 128:(hl + 1) * 128], rl[:, 0:1], None, ALU.mult),
                                             reads=[ops, rl], writes=[yb[iy]])
                                        if hl == 3:
                                            r0 = b * S + qt * 128
                                            k.dma("sp", lambda e, iy=iy, r0=r0: e.dma_start(out=ybD[r0:r0 + 128, hh * 512:(hh + 1) * 512], in_=yb[iy][:]), reads=[yb[iy]], dst=r_yb)

                                    for i in range(len(steps) + 1):
                                        if i < len(steps):
                                            stage_a1(i)
                                        if i >= 1:
                                            stage_b1(i - 1)
                                        if i < len(steps):
                                            stage_a2(i)
                                        if i >= 1:
                                            stage_b2(i - 1)

            def merge_layer(l, xin, r_xin):
                Wl = W[l]
                with k.scope() as sm:
                    wau = sm.sb("wau", [128, 4, D], BF16, dma=True)
                    wbu = sm.sb("wbu", [128, 8, D], BF16, dma=True)
                    wo = sm.sb("wo", [128, 16, D], BF16, dma=True)
                    wload(wau, Wl["w_a_up"], D)
                    wload(wbu, Wl["w_b_up"], D, 2)
                    wload(wo, Wl["w_o"], D, 4)
                    o_t2 = [[sm.sb(f"o{g}_{i}", [128, 512], F32, dma=True) for g in range(3)] for i in range(2)]
                    ls2 = [sm.sb(f"ls{i}", [128, 3, 4], F32, dma=True) for i in range(2)]
                    ybt2 = [sm.sb(f"ybt{i}", [128, 1024], BF16, dma=True) for i in range(2)]
                    gt2 = [sm.sb(f"gt{i}", [128, 4096], BF16, dma=True) for i in range(2)]
                    xt2 = [sm.sb(f"xt{i}", [128, D], F32, dma=True) for i in range(2)]
                    gp = sm.sb("gp", [128, D], F32, dma=True)
                    mm = sm.sb("mm", [128, 4], F32)
                    ee = sm.sb("ee", [128, 3, 4], F32)
                    den = sm.sb("den", [128, 4], F32)
                    yaf = sm.sb("yaf", [128, 512], F32)
                    tmp = sm.sb("tmp", [128, 512], F32)
                    tmp2 = sm.sb("tmp2", [128, 512], F32)
                    ya = sm.sb("ya", [128, 512], BF16)
                    yaT = sm.sb("yaT", [128, 4, 128], BF16)
                    ybT = sm.sb("ybT", [128, 8, 128], BF16)
                    mg = sm.sb("mg", [128, D], BF16)
                    mT = sm.sb("mT", [128, 16, 128], BF16)
                    xn = sm.sb("xn", [128, D], F32)
                    ptp = [sm.ps(f"ptp{i}", [128, 8, 128], BF16) for i in range(2)]
                    ua = sm.ps("ua", [128, 512])
                    ub = sm.ps("ub", [128, 512])
                    ops = [sm.ps(f"ops{i}", [128, 512]) for i in range(2)]
                    def issue_loads(t):
                        i_ = t % 2
                        r0 = t * 128
                        for g in range(3):
                            k.dma("sp", lambda e, g=g, r0=r0, i_=i_: e.dma_start(out=o_t2[i_][g][:], in_=oD[g][r0:r0 + 128, :]), reads=[r_o[g]], dst=o_t2[i_][g])
                        k.dma("sp", [lambda e, g=g, r0=r0, i_=i_: e.dma_start(out=ls2[i_][:, g, :], in_=lseD[g][r0:r0 + 128, :]) for g in range(3)], reads=r_lse, dst=ls2[i_])
                        k.dma("sp", lambda e, r0=r0, i_=i_: e.dma_start(out=ybt2[i_][:], in_=ybD[r0:r0 + 128, :]), reads=[r_yb], dst=ybt2[i_])
                        k.dma("sp", lambda e, r0=r0, i_=i_: e.dma_start(out=gt2[i_][:], in_=gD[r0:r0 + 128, :]), reads=[r_g], dst=gt2[i_])
                        k.dma("sp", lambda e, r0=r0, i_=i_: e.dma_start(out=xt2[i_][:], in_=xin[r0:r0 + 128, :]), reads=[r_xin] if r_xin else [], dst=xt2[i_])

                    issue_loads(0)
                    for t in range(NT):
                        b = t // 16
                        r0 = t * 128
                        o_t, ls, ybt, gt, xt = o_t2[t % 2], ls2[t % 2], ybt2[t % 2], gt2[t % 2], xt2[t % 2]
                        if t % 16 == 0:
                            k.dma("sp", lambda e, b=b: e.dma_start(out=gp[:], in_=mod_row(l, b, 2).broadcast_to([128, D])), reads=[r_mod], dst=gp)
                            k.op("dve", lambda e: e.tensor_scalar(gp[:], gp[:], 1.0, None, ALU.add), reads=[gp], writes=[gp])
                        if t + 1 < NT:
                            issue_loads(t + 1)
                        k.op("dve", lambda e: e.tensor_tensor(mm[:], ls[:, 0, :], ls[:, 1, :], ALU.max), reads=[ls], writes=[mm])
                        k.op("dve", lambda e: e.tensor_tensor(mm[:], mm[:], ls[:, 2, :], ALU.max), reads=[mm, ls], writes=[mm])
                        for g in range(3):
                            k.op("dve", lambda e, g=g: e.tensor_tensor(ee[:, g, :], ls[:, g, :], mm[:], ALU.subtract), reads=[ls, mm], writes=[ee])
                        k.op("act", lambda e: e.activation(ee[:], ee[:], AF.Exp), reads=[ee], writes=[ee])
                        k.op("dve", lambda e: e.tensor_tensor(den[:], ee[:, 0, :], ee[:, 1, :], ALU.add), reads=[ee], writes=[den])
                        k.op("dve", lambda e: e.tensor_tensor(den[:], den[:], ee[:, 2, :], ALU.add), reads=[den, ee], writes=[den])
                        k.op("dve", lambda e: e.reciprocal(den[:], den[:]), reads=[den], writes=[den])
                        for g in range(3):
                            k.op("dve", lambda e, g=g: e.tensor_tensor(ee[:, g, :], ee[:, g, :], den[:], ALU.mult), reads=[ee, den], writes=[ee])
                        for h in range(4):
                            hs = slice(h * 128, (h + 1) * 128)
                            k.op("dve", lambda e, h=h, hs=hs: e.tensor_scalar(yaf[:, hs], o_t[0][:, hs], ee[:, 0, h:h + 1], None, ALU.mult), reads=[o_t[0], ee], writes=[yaf])
                            k.op("dve", lambda e, h=h, hs=hs: e.scalar_tensor_tensor(yaf[:, hs], o_t[1][:, hs], ee[:, 1, h:h + 1], yaf[:, hs], ALU.mult, ALU.add), reads=[o_t[1], ee, yaf], writes=[yaf])
                            k.op("dve", lambda e, h=h, hs=hs: e.scalar_tensor_tensor(ya[:, hs], o_t[2][:, hs], ee[:, 2, h:h + 1], yaf[:, hs], ALU.mult, ALU.add), reads=[o_t[2], ee, yaf], writes=[ya])
                        for j in range(4):
                            k.op("pe", lambda e, j=j: e.transpose(ptp[0][:, j, :], ya[:, j * 128:(j + 1) * 128], ident[:]), reads=[ya, ident], writes=[ptp[0]], inc=(j == 4 - 1))
                        evac(yaT[:], ptp[0][:, 0:4, :], [ptp[0]], [yaT])
                        for j in range(8):
                            k.op("pe", lambda e, j=j: e.transpose(ptp[1][:, j, :], ybt[:, j * 128:(j + 1) * 128], ident[:]), reads=[ybt, ident], writes=[ptp[1]], inc=(j == 8 - 1))
                        evac(ybT[:], ptp[1][:], [ptp[1]], [ybT])
                        for n in range(4):
                            ns = slice(n * 512, (n + 1) * 512)
                            for c in range(4):
                                k.op("pe", lambda e, c=c, ns=ns: e.matmul(ua[:], yaT[:, c, :], wau[:, c, ns], start=(c == 0), stop=(c == 3)), reads=[yaT, wau], writes=[ua], inc=(c == 3))
                            for c in range(8):
                                k.op("pe", lambda e, c=c, ns=ns: e.matmul(ub[:], ybT[:, c, :], wbu[:, c, ns], start=(c == 0), stop=(c == 7)), reads=[ybT, wbu], writes=[ub], inc=(c == 7))
                            k.op("dve", lambda e, ns=ns: e.tensor_tensor(tmp[:], ua[:], gt[:, ns], ALU.mult), reads=[ua, gt], writes=[tmp])
                            k.op("dve", lambda e, n=n: e.tensor_tensor(tmp2[:], ub[:], gt[:, 2048 + n * 512:2048 + (n + 1) * 512], ALU.mult), reads=[ub, gt], writes=[tmp2])
                            k.op("pool", lambda e, ns=ns: e.tensor_tensor(mg[:, ns], tmp[:], tmp2[:], ALU.add), reads=[tmp, tmp2], writes=[mg])
                        for hf in range(2):
                            for j in range(8):
                                c = hf * 8 + j
                                k.op("pe", lambda e, hf=hf, j=j, c=c: e.transpose(ptp[hf][:, j, :], mg[:, c * 128:(c + 1) * 128], ident[:]), reads=[mg, ident], writes=[ptp[hf]], inc=(j == 8 - 1))
                            evac(mT[:, hf * 8:(hf + 1) * 8, :], ptp[hf][:], [ptp[hf]], [mT])
                        for n in range(4):
                            ns = slice(n * 512, (n + 1) * 512)
                            op_ = ops[n % 2]
                            for c in range(16):
                                k.op("pe", lambda e, c=c, ns=ns, op_=op_: e.matmul(op_[:], mT[:, c, :], wo[:, c, ns], start=(c == 0), stop=(c == 15)), reads=[mT, wo], writes=[op_], inc=(c == 15))
                            k.op("dve", lambda e, ns=ns, op_=op_: e.tensor_tensor(xn[:, ns], op_[:], gp[:, ns], ALU.mult), reads=[op_, gp], writes=[xn])
                            k.op("pool", lambda e, ns=ns: e.tensor_tensor(xn[:, ns], xn[:, ns], xt[:, ns], ALU.add), reads=[xn, xt], writes=[xn])
                        k.dma("sp", lambda e, r0=r0: e.dma_start(out=xaD[r0:r0 + 128, :], in_=xn[:]), reads=[xn], dst=r_xa)

            def moe_round(l, rnd, last):
                Wl = W[l]
                t0 = rnd * (TR // 128)
                with k.scope() as smo:
                    sl_i = smo.sb("sl_i", [128, 32, 2], I32)
                    wts = smo.sb("wts", [128, 32, 2], F32)
                    with k.scope() as s1:
                        wr = s1.sb("wr", [128, 16, 72], F32, dma=True)
                        k.dma("sp", [lambda e: e.dma_start(out=wr[:, :, 0:8], in_=Wl["w_grp"].rearrange("(c p) n -> p c n", p=128)),
                                     lambda e: e.dma_start(out=wr[:, :, 8:72], in_=Wl["w_exp"].rearrange("(c p) n -> p c n", p=128))], dst=wr)
                        br = s1.sb("br", [128, 72], F32, dma=True)
                        k.dma("sp", [lambda e: e.dma_start(out=br[:, 0:8], in_=Wl["b_grp"].broadcast_to([128, 8])),
                                     lambda e: e.dma_start(out=br[:, 8:72], in_=Wl["b_exp"].broadcast_to([128, 64]))], dst=br)
                        A2 = s1.sb("A2", [128, D], F32, dma=True)
                        B2 = s1.sb("B2", [128, D], F32, dma=True)
                        G2 = bcast(s1, "G2", Wl["ln2_g"], D)
                        R = s1.sb("R", [128, 64], BF16)
                        k.op("pool", lambda e: e.memset(R[:], 0.0), writes=[R])
                        xt = [s1.sb(f"xt{i}", [128, D], F32, dma=True) for i in range(2)]
                        junk = s1.sb("junk", [128, D], F32)
                        h2f = s1.sb("h2f", [128, D], F32)
                        h2b = [s1.sb(f"h2b{i}", [128, D], BF16) for i in range(2)]
                        h2T = s1.sb("h2T", [128, 16, 128], F32)
                        st = s1.sb("st", [128, 2], F32)
                        lg = s1.sb("lg", [128, 72], F32)
                        m8 = s1.sb("m8", [128, 8], F32)
                        s8 = s1.sb("s8", [128, 8], F32)
                        sc_ = s1.sb("sc_", [128, 16], F32)
                        eg = s1.sb("eg", [128, 8], F32)
                        Gm = s1.sb("Gm", [128, 8], F32)
                        lm = s1.sb("lm", [128, 64], F32)
                        E1 = s1.sb("E1", [128, 64], F32)
                        E2 = s1.sb("E2", [128, 64], F32)
                        Ab = s1.sb("Ab", [128, 64], BF16)
                        cnt = s1.sb("cnt", [128, 64], F32)
                        tq = s1.sb("tq", [128, 64], F32)
                        slf = s1.sb("slf", [128, 2], F32)
                        ptf = [s1.ps(f"ptf{i}", [128, 4, 128], F32) for i in range(2)]
                        lps = s1.ps("lps", [128, 72])
                        cps = s1.ps("cps", [128, 64])
                        for tl in range(TR // 128):
                            t = t0 + tl
                            b = t // 16
                            i = tl % 2
                            r0 = t * 128
                            if t % 16 == 0:
                                k.dma("sp", lambda e, b=b: e.dma_start(out=A2[:], in_=mod_row(l, b, 4).broadcast_to([128, D])), reads=[r_mod], dst=A2)
                                k.dma("sp", lambda e, b=b: e.dma_start(out=B2[:], in_=mod_row(l, b, 3).broadcast_to([128, D])), reads=[r_mod], dst=B2)
                                k.op("dve", lambda e: e.scalar_tensor_tensor(A2[:], A2[:], 1.0, G2[:], ALU.add, ALU.mult), reads=[A2, G2], writes=[A2])
                            k.dma("sp", lambda e, i=i, r0=r0: e.dma_start(out=xt[i][:], in_=xaD[r0:r0 + 128, :]), reads=[r_xa], dst=xt[i])
                            ln_stats(cst, xt[i], junk, st)
                            k.op("dve", lambda e, i=i: e.scalar_tensor_tensor(junk[:], xt[i][:], st[:, 1:2], A2[:], ALU.mult, ALU.mult), reads=[xt[i], st, A2], writes=[junk])
                            k.op("dve", lambda e: e.tensor_tensor(h2f[:], junk[:], B2[:], ALU.add), reads=[junk, B2], writes=[h2f])
                            k.op("act", lambda e, i=i: e.copy(h2b[i][:], h2f[:]), reads=[h2f], writes=[h2b[i]])
                            for c4 in range(4):
                                pf = ptf[c4 % 2]
                                for j in range(4):
                                    c = c4 * 4 + j
                                    k.op("pe", lambda e, pf=pf, j=j, c=c: e.transpose(pf[:, j, :], h2f[:, c * 128:(c + 1) * 128], identf[:]), reads=[h2f, identf], writes=[pf], inc=(j == 4 - 1))
                                evac(h2T[:, c4 * 4:(c4 + 1) * 4, :], pf[:], [pf], [h2T])
                            for c in range(16):
                                k.op("pe", lambda e, c=c: e.matmul(lps[:], h2T[:, c, :], wr[:, c, :], start=(c == 0), stop=(c == 15)), reads=[h2T, wr], writes=[lps], inc=(c == 15))
                            k.op("dve", lambda e: e.tensor_tensor(lg[:], lps[:], br[:], ALU.add), reads=[lps, br], writes=[lg])
                            k.op("dve", lambda e: e.max(m8[:], lg[:, 0:8]), reads=[lg], writes=[m8])
                            k.op("dve", lambda e: e.tensor_scalar(sc_[:, 0:1], m8[:, 0:1], -1.0, None, ALU.mult), reads=[m8], writes=[sc_])
                            k.op("act", lambda e: e.activation(eg[:], lg[:, 0:8], AF.Exp, bias=sc_[:, 0:1], scale=1.0, accum_out=sc_[:, 1:2]), reads=[lg, sc_], writes=[eg, sc_])
                            k.op("dve", lambda e: e.reciprocal(sc_[:, 2:3], sc_[:, 1:2]), reads=[sc_], writes=[sc_])
                            k.op("dve", lambda e: e.tensor_scalar(Gm[:], lg[:, 0:8], m8[:, 0:1], None, ALU.is_equal), reads=[lg, m8], writes=[Gm])
                            k.op("dve", lambda e: e.tensor_scalar(eg[:], Gm[:], 1.0, BIG, ALU.subtract, ALU.mult), reads=[Gm], writes=[eg])
                            for g in range(8):
                                k.op("dve", lambda e, g=g: e.tensor_scalar(lm[:, g * 8:(g + 1) * 8], lg[:, 8 + g * 8:16 + g * 8], eg[:, g:g + 1], None, ALU.add), reads=[lg, eg], writes=[lm])
                            k.op("dve", lambda e: e.max(s8[:], lm[:]), reads=[lm], writes=[s8])
                            k.op("dve", lambda e: e.tensor_scalar(E1[:], lm[:], s8[:, 0:1], None, ALU.is_equal), reads=[lm, s8], writes=[E1])
                            k.op("dve", lambda e: e.tensor_scalar(E2[:], lm[:], s8[:, 1:2], None, ALU.is_equal), reads=[lm, s8], writes=[E2])
                            k.op("dve", lambda e: e.tensor_scalar(sc_[:, 3:4], s8[:, 0:1], -1.0, None, ALU.mult), reads=[s8], writes=[sc_])
                            k.op("act", lambda e: e.activation(sc_[:, 4:5], s8[:, 1:2], AF.Exp, bias=sc_[:, 3:4], scale=1.0), reads=[s8, sc_], writes=[sc_])
                            k.op("dve", lambda e: e.tensor_scalar(sc_[:, 5:6], sc_[:, 4:5], 1.0, None, ALU.add), reads=[sc_], writes=[sc_])
                            k.op("dve", lambda e: e.reciprocal(sc_[:, 5:6], sc_[:, 5:6]), reads=[sc_], writes=[sc_])
                            k.op("dve", lambda e: e.tensor_tensor(sc_[:, 6:7], sc_[:, 2:3], sc_[:, 5:6], ALU.mult), reads=[sc_], writes=[sc_])
                            k.op("dve", lambda e: e.tensor_tensor(sc_[:, 7:8], sc_[:, 6:7], sc_[:, 4:5], ALU.mult), reads=[sc_], writes=[sc_])
                            k.op("dve", lambda e: e.tensor_tensor(Ab[:], E1[:], E2[:], ALU.add), reads=[E1, E2], writes=[Ab])
                            k.op("pe", lambda e: e.matmul(cps[:], LT[:], Ab[:], start=True, stop=False), reads=[LT, Ab], writes=[cps])
                            k.op("pe", lambda e: e.matmul(cps[:], onesb[:], R[:], start=False, stop=True), reads=[onesb, R], writes=[cps])
                            k.op("dve", lambda e: e.tensor_copy(cnt[:], cps[:]), reads=[cps], writes=[cnt])
                            k.op("pool", lambda e: e.tensor_tensor(R[:], R[:], Ab[:], ALU.add), reads=[R, Ab], writes=[R])
                            for kk_, Ek in ((0, E1), (1, E2)):
                                k.op("dve", lambda e, Ek=Ek: e.tensor_tensor(tq[:], Ek[:], cnt[:], ALU.mult), reads=[Ek, cnt], writes=[tq])
                                k.op("dve", lambda e, kk_=kk_: e.reduce_sum(sc_[:, 8 + kk_:9 + kk_], tq[:], AX.X), reads=[tq], writes=[sc_])
                                k.op("dve", lambda e, Ek=Ek: e.tensor_tensor(tq[:], Ek[:], eC[:], ALU.mult), reads=[Ek, eC], writes=[tq])
                                k.op("dve", lambda e, kk_=kk_: e.reduce_sum(sc_[:, 10 + kk_:11 + kk_], tq[:], AX.X), reads=[tq], writes=[sc_])
                                k.op("dve", lambda e, kk_=kk_: e.tensor_scalar(sc_[:, 12 + kk_:13 + kk_], sc_[:, 8 + kk_:9 + kk_], float(C_CAP), None, ALU.is_lt), reads=[sc_], writes=[sc_])
                                k.op("dve", lambda e, kk_=kk_: e.tensor_tensor(sc_[:, 8 + kk_:9 + kk_], sc_[:, 8 + kk_:9 + kk_], sc_[:, 10 + kk_:11 + kk_], ALU.add), reads=[sc_], writes=[sc_])
                                k.op("dve", lambda e, kk_=kk_: e.tensor_scalar(sc_[:, 8 + kk_:9 + kk_], sc_[:, 8 + kk_:9 + kk_], float(-NSLOT), None, ALU.add), reads=[sc_], writes=[sc_])
                                k.op("dve", lambda e, kk_=kk_: e.tensor_tensor(sc_[:, 8 + kk_:9 + kk_], sc_[:, 8 + kk_:9 + kk_], sc_[:, 12 + kk_:13 + kk_], ALU.mult), reads=[sc_], writes=[sc_])
                                k.op("dve", lambda e, kk_=kk_: e.tensor_scalar(slf[:, kk_:kk_ + 1], sc_[:, 8 + kk_:9 + kk_], float(NSLOT), None, ALU.add), reads=[sc_], writes=[slf])
                                k.op("dve", lambda e, kk_=kk_, tl=tl: e.tensor_tensor(wts[:, tl, kk_:kk_ + 1], sc_[:, 6 + kk_:7 + kk_], sc_[:, 12 + kk_:13 + kk_], ALU.mult), reads=[sc_], writes=[wts])
                            k.op("dve", lambda e, tl=tl: e.tensor_copy(sl_i[:, tl, :], slf[:]), reads=[slf], writes=[sl_i])
                            for kk_ in range(2):
                                k.dma("pool", lambda e, i=i, tl=tl, kk_=kk_: e.indirect_dma_start(
                                    out=XsD[:, :], out_offset=bass.IndirectOffsetOnAxis(ap=sl_i[:, tl, kk_:kk_ + 1], axis=0),
                                    in_=h2b[i][:, :], in_offset=None), reads=[h2b[i], sl_i], dst=r_xs)
                        if "slotD" in ext:
                            k.dma("sp", [lambda e: e.dma_start(out=slotD[t0 * 128:t0 * 128 + TR, 0:2].rearrange("(t p) k -> p t k", p=128), in_=wts[:], allow_slow_non_contiguous=True)],
                                  reads=[wts], dst=r_slot)

                    with k.scope() as s2:
                        wgu = [s2.sb(f"wgu{i}", [128, 16, 1024], BF16, dma=True) for i in range(2)]
                        wd = [s2.sb(f"wd{i}", [128, 4, D], BF16, dma=True) for i in range(2)]
                        xsl = [s2.sb(f"xsl{i}", [128, 2, D], BF16, dma=True) for i in range(2)]
                        xT = s2.sb("xT", [128, 16, 256], BF16)
                        sg_ = s2.sb("sg_", [128, 256], F32)
                        aT = s2.sb("aT", [128, 4, 256], BF16)
                        ysb = [s2.sb(f"ysb{i}", [128, D], F32) for i in range(2)]
                        ptp = [s2.ps(f"ptp{i}", [128, 8, 128], BF16) for i in range(2)]
                        gps = s2.ps("gps", [128, 256])
                        ups = s2.ps("ups", [128, 256])
                        yps = [s2.ps(f"yps{i}", [128, 512]) for i in range(2)]
                        for ex in range(NE):
                            i = ex % 2
                            gi, ei = ex // 8, ex % 8
                            wload(wgu[i], Wl["w_gu"][gi][ei], 1024, 4)
                            wload(wd[i], Wl["w_down"][gi][ei], D, 2)
                            s0 = ex * C_CAP
                            k.dma("sp", [lambda e, i=i, s=s, s0=s0: e.dma_start(out=xsl[i][:, s, :], in_=XsD[s0 + s * 128:s0 + (s + 1) * 128, :]) for s in range(2)],
                                  reads=[r_xs], dst=xsl[i])
                            for s in range(2):
                                for hf in range(2):
                                    for j in range(8):
                                        c = hf * 8 + j
                                        k.op("pe", lambda e, i=i, s=s, hf=hf, j=j, c=c: e.transpose(ptp[hf][:, j, :], xsl[i][:, s, c * 128:(c + 1) * 128], ident[:]),
                                             reads=[xsl[i], ident], writes=[ptp[hf]], inc=(j == 8 - 1))
                                    evac(xT[:, hf * 8:(hf + 1) * 8, s * 128:(s + 1) * 128], ptp[hf][:], [ptp[hf]], [xT])
                            for j in range(4):
                                for c in range(16):
                                    k.op("pe", lambda e, i=i, j=j, c=c: e.matmul(gps[:], wgu[i][:, c, j * 128:(j + 1) * 128], xT[:, c, :], start=(c == 0), stop=(c == 15)),
                                         reads=[wgu[i], xT], writes=[gps], inc=(c == 15))
                                for c in range(16):
                                    k.op("pe", lambda e, i=i, j=j, c=c: e.matmul(ups[:], wgu[i][:, c, 512 + j * 128:512 + (j + 1) * 128], xT[:, c, :], start=(c == 0), stop=(c == 15)),
                                         reads=[wgu[i], xT], writes=[ups], inc=(c == 15))
                                k.op("act", lambda e: e.activation(sg_[:], gps[:], AF.Silu), reads=[gps], writes=[sg_])
                                k.op("dve", lambda e, j=j: e.tensor_tensor(aT[:, j, :], sg_[:], ups[:], ALU.mult), reads=[sg_, ups], writes=[aT])
                            for s in range(2):
                                for n in range(4):
                                    yp = yps[n % 2]
                                    for j in range(4):
                                        k.op("pe", lambda e, i=i, s=s, n=n, j=j, yp=yp: e.matmul(yp[:], aT[:, j, s * 128:(s + 1) * 128], wd[i][:, j, n * 512:(n + 1) * 512], start=(j == 0), stop=(j == 3)),
                                             reads=[aT, wd[i]], writes=[yp], inc=(j == 3))
                                    evac(ysb[s][:, n * 512:(n + 1) * 512], yp[:], [yp], [ysb[s]])
                                k.dma("sp", lambda e, s=s, s0=s0: e.dma_start(out=YsD[s0 + s * 128:s0 + (s + 1) * 128, :], in_=ysb[s][:]), reads=[ysb[s]], dst=r_ys)

                    with k.scope() as s3:
                        gp = s3.sb("gp", [128, D], F32, dma=True)
                        y1 = [s3.sb(f"y1_{i}", [128, D], F32, dma=True) for i in range(2)]
                        y2 = [s3.sb(f"y2_{i}", [128, D], F32, dma=True) for i in range(2)]
                        xt = [s3.sb(f"xt{i}", [128, D], F32, dma=True) for i in range(2)]
                        xn = [s3.sb(f"xn{i}", [128, D], F32) for i in range(2)]
                        junk = s3.sb("junk", [128, D], F32)
                        st = s3.sb("st", [128, 2], F32)
                        fg = bcast(s3, "fg", fin_g, D) if last else None
                        for tl in range(TR // 128):
                            t = t0 + tl
                            b = t // 16
                            i = tl % 2
                            r0 = t * 128
                            if t % 16 == 0:
                                k.dma("sp", lambda e, b=b: e.dma_start(out=gp[:], in_=mod_row(l, b, 5).broadcast_to([128, D])), reads=[r_mod], dst=gp)
                                k.op("dve", lambda e: e.tensor_scalar(gp[:], gp[:], 1.0, None, ALU.add), reads=[gp], writes=[gp])
                            for kk_, yy in ((0, y1[i]), (1, y2[i])):
                                k.dma("pool", lambda e, yy=yy, tl=tl, kk_=kk_: e.indirect_dma_start(
                                    out=yy[:, :], out_offset=None, in_=YsD[:, :],
                                    in_offset=bass.IndirectOffsetOnAxis(ap=sl_i[:, tl, kk_:kk_ + 1], axis=0)), reads=[r_ys, sl_i], dst=yy)
                            k.dma("sp", lambda e, i=i, r0=r0: e.dma_start(out=xt[i][:], in_=xaD[r0:r0 + 128, :]), reads=[r_xa], dst=xt[i])
                            k.op("dve", lambda e, i=i, tl=tl: e.tensor_scalar(y1[i][:], y1[i][:], wts[:, tl, 0:1], None, ALU.mult), reads=[y1[i], wts], writes=[y1[i]])
                            k.op("dve", lambda e, i=i, tl=tl: e.scalar_tensor_tensor(y1[i][:], y2[i][:], wts[:, tl, 1:2], y1[i][:], ALU.mult, ALU.add), reads=[y1[i], y2[i], wts], writes=[y1[i]])
                            k.op("pool", lambda e, i=i: e.tensor_tensor(y1[i][:], y1[i][:], gp[:], ALU.mult), reads=[y1[i], gp], writes=[y1[i]])
                            k.op("dve", lambda e, i=i: e.tensor_tensor(xn[i][:], y1[i][:], xt[i][:], ALU.add), reads=[y1[i], xt[i]], writes=[xn[i]])
                            if last:
                                ln_stats(cst, xn[i], junk, st)
                                k.op("dve", lambda e, i=i: e.scalar_tensor_tensor(xn[i][:], xn[i][:], st[:, 1:2], fg[:], ALU.mult, ALU.mult), reads=[xn[i], st, fg], writes=[xn[i]])
                                k.dma("sp", lambda e, i=i, r0=r0: e.dma_start(out=out_d[r0:r0 + 128, :], in_=xn[i][:]), reads=[xn[i]], dst=r_out)
                            else:
                                k.dma("sp", lambda e, i=i, r0=r0: e.dma_start(out=xbD[r0:r0 + 128, :], in_=xn[i][:]), reads=[xn[i]], dst=r_xb)

            xin, r_xin = x_d, None
            for l in range(nl_attn):
                for b in range(NB):
                    attn_seq(l, b, xin, r_xin)
                merge_layer(l, xin, r_xin)
                if l < nl_moe:
                    last = (stop is None and l == NL - 1)
                    for rnd in range(NR):
                        moe_round(l, rnd, last)
                    xin, r_xin = xbD, r_xb
            if stop is not None:
                with k.scope() as sd:
                    t_ = sd.sb("dbgt", [128, D], F32, dma=True)
                    src, rs_ = (modD, r_mod) if stop == "mod" else ((xaD, r_xa) if stop == "attn" else (xbD, r_xb))
                    if stop == "mod":
                        k.dma("sp", lambda e: e.dma_start(out=t_[0:2 * NB, :], in_=modD.rearrange("l b (j n) -> (l b) j n", n=D)[:, 0, :]), reads=[r_mod], dst=t_)
                        k.dma("sp", lambda e: e.dma_start(out=out_d[0:2 * NB, :], in_=t_[0:2 * NB, :]), reads=[t_], dst=r_out)
                    else:
                        for t in range(NT):
                            k.dma("sp", lambda e, t=t: e.dma_start(out=t_[:], in_=src[t * 128:(t + 1) * 128, :]), reads=[rs_], dst=t_)
                            k.dma("sp", lambda e, t=t: e.dma_start(out=out_d[t * 128:(t + 1) * 128, :], in_=t_[:]), reads=[t_], dst=r_out)
        k.barrier()
    return nc


def make_in_maps(inputs, n_cores, NB, NL=2, NG=8, stop=None):
    f = lambda a: np.ascontiguousarray(a)
    shared = {}
    run_attn = stop != "mod"
    run_moe = stop not in ("mod", "attn")
    for l in range(NL if stop is None else 1):
        shared[f"w_ada_{l}"] = f(inputs["w_ada"][l])
        shared[f"b_ada_{l}"] = f(inputs["b_ada"][l][None])
        if run_attn:
            shared[f"ln1_g_{l}"] = f(inputs["ln1_g"][l][None])
            for n in ("w_in", "q_norm_g", "w_uq", "kv_norm_g", "w_ukv", "w_a_up", "w_b_up", "w_o"):
                shared[f"{n}_{l}"] = f(inputs[n][l])
        if run_moe:
            shared[f"ln2_g_{l}"] = f(inputs["ln2_g"][l][None])
            shared[f"w_grp_{l}"] = f(inputs["w_grp"][l])
            shared[f"b_grp_{l}"] = f(inputs["b_grp"][l][None])
            shared[f"w_exp_{l}"] = f(inputs["w_exp"][l])
            shared[f"b_exp_{l}"] = f(inputs["b_exp"][l][None])
            for g in range(NG):
                shared[f"w_gu_{l}_{g}"] = f(inputs["w_gu"][l][g * 8:(g + 1) * 8])
                shared[f"w_down_{l}_{g}"] = f(inputs["w_down"][l][g * 8:(g + 1) * 8])
    if stop is None:
        shared["final_g"] = f(inputs["final_g"][None])
    maps = []
    for c in range(n_cores):
        m = dict(shared)
        sl = slice(c * NB, (c + 1) * NB)
        m["x"] = f(inputs["x"][sl]).reshape(NB * S, D)
        m["c"] = f(inputs["c"][sl])
        m["positions"] = f(inputs["positions"][sl]).astype(np.int32)
        maps.append(m)
    return maps


def kernel(**inputs):
    inputs = {k_: np.asarray(v) for k_, v in inputs.items()}
    n = N_CORES
    NB = 16 // n
    nc = build(NB)
    maps = make_in_maps(inputs, n, NB)
    res = run_bass_kernel_spmd(nc, maps, core_ids=list(range(n)))
    out = np.concatenate([r["out"].reshape(NB, S, D) for r in res.results], axis=0)
    return out.astype(np.float32)
```

```python
import math
from contextlib import ExitStack, contextmanager

import numpy as np
import concourse.bass as bass
import concourse.mybir as mybir
from concourse.bass_utils import run_bass_kernel_spmd

F32 = mybir.dt.float32
BF16 = mybir.dt.bfloat16
I32 = mybir.dt.int32
ALU = mybir.AluOpType
AF = mybir.ActivationFunctionType
AX = mybir.AxisListType

S = 2048
D = 2048
NE_FULL = 64
C_CAP = 256
TR = 4096
EPS = 1e-6
BIG = 1.0e9
IN_COLS = 9792
N_CORES = 8
SAME_ENG_WAIT = True


class Res:
    def __init__(self, k, name, dma=False, multi=False):
        self.name = name
        self.w = None
        self.r = {}
        self.multi = multi
        self.sem = None
        self.cnt = 0
        if dma:
            self.sem, self.cnt = k.take_sem()
            k.live.append(self)


class Tl:
    def __init__(self, t, res):
        self.t = t
        self.res = res

    def __getitem__(self, i):
        return self.t[i]


class Eng:
    def __init__(self, k, name, h):
        self.name = name
        self.h = h
        self.sem = k.new_sem("e_" + name)
        self.cnt = 0
        self.waited = {}
        self.pend_r = []
        self.pend_w = []

    def wait(self, tok):
        if tok is None:
            return
        sem, val = tok
        if sem is self.sem and (self.name == "pe" or (self.name in ("act", "dve") and not SAME_ENG_WAIT)):
            return
        if self.waited.get(id(sem), 0) >= val:
            return
        self.waited[id(sem)] = val
        self.h.wait_ge(sem, val)


class Scope:
    def __init__(self, k):
        self.k = k
        self.stack = ExitStack()
        self.res = []

    def sb(self, name, shape, dt, dma=False):
        self.k.uid += 1
        t = self.stack.enter_context(self.k.nc.sbuf_tensor(f"{name}_{self.k.uid}", shape, dt))
        r = Res(self.k, name, dma)
        self.res.append(r)
        return Tl(t, r)

    def ps(self, name, shape, dt=F32):
        self.k.uid += 1
        t = self.stack.enter_context(self.k.nc.psum_tensor(f"{name}_{self.k.uid}", shape, dt))
        r = Res(self.k, name, False)
        self.res.append(r)
        return Tl(t, r)


class K:
    def __init__(self, nc, stack):
        self.nc = nc
        self.stack = stack
        self.free = []
        self.live = []
        self.uid = 0
        self.eng = {
            "pe": Eng(self, "pe", nc.tensor), "act": Eng(self, "act", nc.scalar),
            "dve": Eng(self, "dve", nc.vector), "pool": Eng(self, "pool", nc.gpsimd),
            "sp": Eng(self, "sp", nc.sync),
        }

    def new_sem(self, name):
        self.uid += 1
        self.nsem = getattr(self, "nsem", 0) + 1
        return self.stack.enter_context(self.nc.semaphore(f"{name}_{self.uid}"))

    def take_sem(self):
        while self.free:
            sem, cnt = self.free.pop()
            if cnt < 24000:
                return (sem, cnt)
        return (self.new_sem("d"), 0)

    def res(self, name, dma=True, multi=True):
        return Res(self, name, dma, multi)

    def barrier(self):
        toks = [(e.sem, e.cnt) for e in self.eng.values() if e.cnt]
        toks += [(r.sem, r.cnt) for r in self.live if r.cnt]
        for e in self.eng.values():
            for t in toks:
                e.wait(t)
        for e in self.eng.values():
            if e.cnt > 24000:
                e.sem = self.new_sem("e_" + e.name)
                e.cnt = 0

    @contextmanager
    def scope(self):
        sc = Scope(self)
        try:
            yield sc
        except BaseException:
            import traceback
            if not getattr(self, "_tb_done", False):
                traceback.print_exc()
                self._tb_done = True
            raise
        else:
            self.barrier()
            for r in sc.res:
                if r.sem is not None:
                    self.live.remove(r)
                    self.free.append((r.sem, r.cnt))
            sc.stack.close()

    @staticmethod
    def _r(x):
        return x if isinstance(x, Res) else x.res

    def _deps(self, e, reads, writes):
        for x in reads:
            e.wait(self._r(x).w)
        for x in writes:
            x = self._r(x)
            if not x.multi:
                e.wait(x.w)
            for tok in list(x.r.values()):
                e.wait(tok)

    def op(self, en, fn, reads=(), writes=(), inc=True):
        e = self.eng[en]
        self._deps(e, reads, writes)
        if not inc:
            fn(e.h)
            e.pend_r.extend(reads)
            e.pend_w.extend(writes)
            return None
        e.cnt += 1
        tok = (e.sem, e.cnt)
        fn(e.h).then_inc(e.sem, 1)
        for x in list(reads) + e.pend_r:
            self._r(x).r[id(e.sem)] = tok
        for x in list(writes) + e.pend_w:
            x = self._r(x)
            x.w = tok
            x.r = {}
        e.pend_r, e.pend_w = [], []
        return tok

    def dma(self, en, fns, reads=(), dst=None, inc=16):
        e = self.eng[en]
        d = self._r(dst)
        self._deps(e, reads, [d])
        if not isinstance(fns, (list, tuple)):
            fns = [fns]
        for fn in fns:
            d.cnt += inc
            fn(e.h).then_inc(d.sem, inc)
        tok = (d.sem, d.cnt)
        for x in reads:
            self._r(x).r[id(d.sem)] = tok
        d.w = tok
        if not d.multi:
            d.r = {}
        return tok


def build(NB, NL=2, NG=8, stop=None, ext=()):
    T = NB * S
    NT = T // 128
    NE = NG * 8
    NR = T // TR
    nc = bass.Bass("TRN2", target_bir_lowering=False)

    def din(name, shape, dt=F32):
        return nc.dram_tensor(name, shape, dt, kind="ExternalInput").ap()

    def dscr(name, shape, dt=F32):
        kind = "ExternalOutput" if name in ext else "Internal"
        return nc.dram_tensor(name, shape, dt, kind=kind).ap()

    run_attn = stop != "mod"
    run_moe = stop not in ("mod", "attn")
    nl_attn = NL if stop is None else (1 if run_attn else 0)
    nl_moe = NL if stop is None else (1 if run_moe else 0)

    x_d = din("x", [T, D])
    c_d = din("c", [NB, D])
    pos_d = din("positions", [NB, S], I32)
    W = {}
    for l in range(NL if stop is None else 1):
        W[l] = dict(
            w_ada=din(f"w_ada_{l}", [D, 6 * D]), b_ada=din(f"b_ada_{l}", [1, 6 * D]))
        if run_attn:
            W[l].update(
                ln1_g=din(f"ln1_g_{l}", [1, D]),
                w_in=din(f"w_in_{l}", [D, IN_COLS]),
                q_norm_g=din(f"q_norm_g_{l}", [512]), w_uq=din(f"w_uq_{l}", [512, 1536]),
                kv_norm_g=din(f"kv_norm_g_{l}", [512]), w_ukv=din(f"w_ukv_{l}", [512, 2048]),
                w_a_up=din(f"w_a_up_{l}", [512, D]), w_b_up=din(f"w_b_up_{l}", [1024, D]),
                w_o=din(f"w_o_{l}", [D, D]))
        if run_moe:
            W[l].update(
                ln2_g=din(f"ln2_g_{l}", [1, D]),
                w_grp=din(f"w_grp_{l}", [D, 8]), b_grp=din(f"b_grp_{l}", [1, 8]),
                w_exp=din(f"w_exp_{l}", [D, 64]), b_exp=din(f"b_exp_{l}", [1, 64]),
                w_gu=[din(f"w_gu_{l}_{g}", [8, D, 1024]) for g in range(NG)],
                w_down=[din(f"w_down_{l}_{g}", [8, 512, D]) for g in range(NG)])
    fin_g = din("final_g", [1, D]) if stop is None else None
    out_d = nc.dram_tensor("out", [T, D], F32, kind="ExternalOutput").ap()

    modD = dscr("modD", [2, NB, 6 * D])
    xaD = dscr("xaD", [T, D])
    xbD = dscr("xbD", [T, D])
    oD = [dscr(f"oD{g}", [T, 512]) for g in range(3)]
    lseD = [dscr(f"lseD{g}", [T, 4]) for g in range(3)]
    ybD = dscr("ybD", [T, 1024], BF16)
    gD = dscr("gD", [T, 4096], BF16)
    NSLOT = NE * C_CAP
    XsD = dscr("XsD", [NSLOT + 128, D], BF16)
    YsD = dscr("YsD", [NSLOT + 128, D])
    slotD = dscr("slotD", [T, 4])

    slopes = [2.0 ** (-8.0 * (n + 1) / 12.0) for n in range(12)]

    with ExitStack() as stack:
        k = K(nc, stack)
        r_mod = k.res("modD")
        r_xa = k.res("xaD")
        r_xb = k.res("xbD")
        r_o = [k.res(f"oD{g}") for g in range(3)]
        r_lse = [k.res(f"lseD{g}") for g in range(3)]
        r_yb = k.res("ybD")
        r_g = k.res("gD")
        r_xs = k.res("XsD")
        r_ys = k.res("YsD")
        r_out = k.res("out")
        r_slot = k.res("slotD")

        with k.scope() as g0:
            identf = g0.sb("identf", [128, 128], F32)
            ident = g0.sb("ident", [128, 128], BF16)
            onesf = g0.sb("onesf", [128, 128], F32)
            onesb = g0.sb("onesb", [128, 128], BF16)
            LTf = g0.sb("LTf", [128, 128], F32)
            LT = g0.sb("LT", [128, 128], BF16)
            mband = g0.sb("mband", [128, 384], F32)
            eCi = g0.sb("eCi", [128, 64], I32)
            eC = g0.sb("eC", [128, 64], F32)
            invi = g0.sb("invi", [64, 1], I32)
            inv = g0.sb("inv", [64, 1], F32)
            sgn = g0.sb("sgn", [64, 1], F32)

            k.op("pool", lambda e: e.memset(onesf[:], 1.0), writes=[onesf])
            k.op("pool", lambda e: e.memset(identf[:], 1.0), writes=[identf])
            k.op("pool", lambda e: e.affine_select(identf[:], identf[:], [[-1, 128]], ALU.is_equal, 0.0,
                                                    base=0, channel_multiplier=1), reads=[identf], writes=[identf])
            k.op("dve", lambda e: e.tensor_copy(ident[:], identf[:]), reads=[identf], writes=[ident])
            k.op("dve", lambda e: e.tensor_copy(onesb[:], onesf[:]), reads=[onesf], writes=[onesb])
            k.op("pool", lambda e: e.affine_select(LTf[:], onesf[:], [[1, 128]], ALU.is_ge, 0.0,
                                                    base=-1, channel_multiplier=-1), reads=[onesf], writes=[LTf])
            k.op("dve", lambda e: e.tensor_copy(LT[:], LTf[:]), reads=[LTf], writes=[LT])
            k.op("pool", lambda e: e.memset(mband[:], 0.0), writes=[mband])
            k.op("pool", lambda e: e.affine_select(mband[:], mband[:], [[1, 384]], ALU.is_ge, BIG,
                                                    base=-64, channel_multiplier=-1), reads=[mband], writes=[mband])
            k.op("pool", lambda e: e.affine_select(mband[:], mband[:], [[-1, 384]], ALU.is_ge, BIG,
                                                    base=192, channel_multiplier=1), reads=[mband], writes=[mband])
            k.op("pool", lambda e: e.iota(eCi[:], [[C_CAP, 64]], base=0, channel_multiplier=0), writes=[eCi])
            k.op("dve", lambda e: e.tensor_copy(eC[:], eCi[:]), reads=[eCi], writes=[eC])
            k.op("pool", lambda e: e.iota(invi[0:32, :], [[0, 1]], base=0, channel_multiplier=1), writes=[invi])
            k.op("pool", lambda e: e.iota(invi[32:64, :], [[0, 1]], base=0, channel_multiplier=1), writes=[invi])
            k.op("dve", lambda e: e.tensor_copy(inv[:], invi[:]), reads=[invi], writes=[inv])
            k.op("act", lambda e: e.activation(inv[:], inv[:], AF.Exp, scale=-math.log(10000.0) / 32.0),
                 reads=[inv], writes=[inv])
            k.op("pool", lambda e: e.memset(sgn[0:32, :], -1.0), writes=[sgn])
            k.op("pool", lambda e: e.memset(sgn[32:64, :], 1.0), writes=[sgn])
            epsD = g0.sb("epsD", [128, 1], F32)
            k.op("pool", lambda e: e.memset(epsD[:], EPS), writes=[epsD])
            cst = {"eps": epsD}
            if run_moe:
                with k.scope() as sz:
                    zeros = sz.sb("zeros", [128, D], F32)
                    k.op("pool", lambda e: e.memset(zeros[:], 0.0), writes=[zeros])
                    k.dma("sp", lambda e: e.dma_start(out=YsD[NSLOT:NSLOT + 128, :], in_=zeros[:]), reads=[zeros], dst=r_ys)

            def bcast(sc, name, row_ap, n, reads=()):
                t = sc.sb(name, [128, n], F32, dma=True)
                k.dma("sp", lambda e: e.dma_start(out=t[:], in_=row_ap.broadcast_to([128, n])), reads=reads, dst=t)
                return t

            rr = [0]

            def evac(out_ap, in_ap, reads, writes, scale=None, eng=None):
                rr[0] += 1
                if (eng == "act") or (eng is None and rr[0] % 2):
                    if scale is None:
                        k.op("act", lambda e: e.copy(out_ap, in_ap), reads=reads, writes=writes)
                    else:
                        k.op("act", lambda e: e.mul(out_ap, in_ap, scale), reads=reads, writes=writes)
                else:
                    if scale is None:
                        k.op("dve", lambda e: e.tensor_copy(out_ap, in_ap), reads=reads, writes=writes)
                    else:
                        k.op("dve", lambda e: e.tensor_scalar(out_ap, in_ap, scale, None, ALU.mult),
                             reads=reads, writes=writes)

            def wload(t, src_ap, ncol, nsplit=1):
                v = src_ap.rearrange("(c p) n -> p c n", p=128)
                step = ncol // nsplit
                k.dma("pool", [lambda e, i=i: e.dma_start(out=t[:, :, i * step:(i + 1) * step],
                                                          in_=v[:, :, i * step:(i + 1) * step])
                               for i in range(nsplit)], dst=t)

            with k.scope() as sm:
                cT = sm.sb("cT", [128, 16, NB], F32, dma=True)
                csT = sm.sb("csT", [128, 16, NB], F32)
                k.dma("sp", [lambda e, b=b: e.dma_start(out=cT[:, :, b], in_=c_d[b].rearrange("(c p) -> p c", p=128),
                                                        allow_slow_non_contiguous=True) for b in range(NB)], dst=cT)
                k.op("act", lambda e: e.activation(csT[:], cT[:], AF.Silu), reads=[cT], writes=[csT])
                csb = sm.sb("csb", [128, 16, NB], BF16)
                k.op("dve", lambda e: e.tensor_copy(csb[:], csT[:]), reads=[csT], writes=[csb])
                wb = [sm.sb(f"wada{i}", [128, 16, 512], BF16, dma=True) for i in range(3)]
                mps = [sm.ps(f"mps{i}", [NB, 512]) for i in range(2)]
                msb = [sm.sb(f"msb{i}", [NB, 512], F32) for i in range(2)]
                for l in W:
                    bsb = sm.sb(f"bsb{l}", [NB, 6 * D], F32, dma=True)
                    k.dma("sp", lambda e, l=l: e.dma_start(out=bsb[:], in_=W[l]["b_ada"].broadcast_to([NB, 6 * D])), dst=bsb)
                    wv = W[l]["w_ada"].rearrange("(c p) n -> p c n", p=128)
                    for n in range(24):
                        i = n % 2
                        wi_ = n % 3
                        k.dma("pool", [lambda e, n=n, i=wi_, h=h: e.dma_start(out=wb[i][:, h * 8:(h + 1) * 8, :],
                                                                          in_=wv[:, h * 8:(h + 1) * 8, n * 512:(n + 1) * 512])
                                     for h in range(2)], dst=wb[wi_])
                        for c in range(16):
                            k.op("pe", lambda e, c=c, i=i, wi_=wi_: e.matmul(mps[i][:], csb[:, c, :], wb[wi_][:, c, :],
                                                                    start=(c == 0), stop=(c == 15)),
                                 reads=[csb, wb[wi_]], writes=[mps[i]], inc=(c == 15))
                        k.op("dve", lambda e, n=n, i=i: e.tensor_tensor(msb[i][:], mps[i][:], bsb[:, n * 512:(n + 1) * 512], ALU.add),
                             reads=[mps[i], bsb], writes=[msb[i]])
                        k.dma("sp", lambda e, n=n, i=i, l=l: e.dma_start(out=modD[l, :, n * 512:(n + 1) * 512], in_=msb[i][:]),
                              reads=[msb[i]], dst=r_mod)

            def mod_row(l, b, j):
                return modD[l, b:b + 1, j * D:(j + 1) * D]

            def ln_stats(sc_tiles, xt, junk, st):
                k.op("act", lambda e: e.activation(junk[:], xt[:], AF.Square, accum_out=st[:, 0:1]),
                     reads=[xt], writes=[junk, st])
                k.op("act", lambda e: e.activation(st[:, 1:2], st[:, 0:1], AF.Sqrt, bias=sc_tiles["eps"][:, 0:1], scale=1.0 / D),
                     reads=[st, sc_tiles["eps"]], writes=[st])
                k.op("dve", lambda e: e.reciprocal(st[:, 1:2], st[:, 1:2]), reads=[st], writes=[st])

            def rope_tables(sc, b, cos_t, sin_t, scale):
                pki = sc.sb("pki", [64, S], I32, dma=True)
                ang = sc.sb("ang", [64, S], F32)
                kk = sc.sb("kk", [64, S], F32)
                kki = sc.sb("kki", [64, S], I32)
                k.dma("sp", lambda e: e.dma_start(out=pki[:], in_=pos_d[b:b + 1, :].broadcast_to([64, S])), dst=pki)
                k.op("dve", lambda e: e.tensor_copy(ang[:], pki[:]), reads=[pki], writes=[ang])
                k.op("dve", lambda e: e.tensor_scalar(ang[:], ang[:], inv[:, 0:1], None, ALU.mult), reads=[ang, inv], writes=[ang])
                TWO_PI = 2.0 * math.pi

                def reduce_sin(dst, shift):
                    k.op("dve", lambda e: e.tensor_scalar(kk[:], ang[:], shift, 1.0 / TWO_PI, ALU.add, ALU.mult), reads=[ang], writes=[kk])
                    k.op("dve", lambda e: e.tensor_copy(kki[:], kk[:]), reads=[kk], writes=[kki])
                    k.op("dve", lambda e: e.tensor_copy(kk[:], kki[:]), reads=[kki], writes=[kk])
                    k.op("dve", lambda e: e.scalar_tensor_tensor(kk[:], kk[:], -TWO_PI, ang[:], ALU.mult, ALU.add), reads=[kk, ang], writes=[kk])
                    if shift:
                        k.op("dve", lambda e: e.tensor_scalar(kk[:], kk[:], shift, None, ALU.add), reads=[kk], writes=[kk])
                    k.op("dve", lambda e: e.tensor_scalar(dst[:], kk[:], math.pi, -TWO_PI, ALU.is_gt, ALU.mult), reads=[kk], writes=[dst])
                    k.op("dve", lambda e: e.tensor_tensor(kk[:], kk[:], dst[:], ALU.add), reads=[kk, dst], writes=[kk])
                    k.op("dve", lambda e: e.tensor_scalar(dst[:], kk[:], -math.pi, TWO_PI, ALU.is_lt, ALU.mult), reads=[kk], writes=[dst])
                    k.op("dve", lambda e: e.tensor_tensor(kk[:], kk[:], dst[:], ALU.add), reads=[kk, dst], writes=[kk])
                    k.op("dve", lambda e: e.tensor_scalar(kk[:], kk[:], math.pi, -math.pi, ALU.min, ALU.max), reads=[kk], writes=[kk])
                    k.op("act", lambda e: e.activation(dst[:], kk[:], AF.Sin), reads=[kk], writes=[dst])

                reduce_sin(sin_t, 0.0)
                reduce_sin(cos_t, math.pi / 2.0)
                k.op("dve", lambda e: e.tensor_scalar(sin_t[:], sin_t[:], sgn[:, 0:1], scale, ALU.mult, ALU.mult), reads=[sin_t, sgn], writes=[sin_t])
                if scale != 1.0:
                    k.op("dve", lambda e: e.tensor_scalar(cos_t[:], cos_t[:], scale, None, ALU.mult), reads=[cos_t], writes=[cos_t])

            def attn_seq(l, b, xin, r_xin):
                Wl = W[l]
                win = Wl["w_in"]
                with k.scope() as so:
                    cqn = so.sb("cqn", [128, 4, S], BF16)
                    ckvn = so.sb("ckvn", [128, 4, S], BF16)
                    krT = so.sb("krT", [64, S], BF16)
                    with k.scope() as sh:
                        hT = sh.sb("hT", [128, 16, S], BF16)
                        with k.scope() as s1:
                            A1 = bcast(s1, "A1", mod_row(l, b, 1), D, reads=[r_mod])
                            B1 = bcast(s1, "B1", mod_row(l, b, 0), D, reads=[r_mod])
                            G1 = bcast(s1, "G1", Wl["ln1_g"], D)
                            k.op("dve", lambda e: e.scalar_tensor_tensor(A1[:], A1[:], 1.0, G1[:], ALU.add, ALU.mult),
                                 reads=[A1, G1], writes=[A1])
                            xt = [s1.sb(f"xt{i}", [128, D], F32, dma=True) for i in range(2)]
                            junk = s1.sb("junk", [128, D], F32)
                            hb = [s1.sb(f"hb{i}", [128, D], BF16) for i in range(2)]
                            st = [s1.sb(f"st{i}", [128, 2], F32) for i in range(2)]
                            pT = [s1.ps(f"pT{i}", [128, 8, 128], BF16) for i in range(2)]
                            for tt in range(16):
                                i = tt % 2
                                r0 = b * S + tt * 128
                                k.dma("sp", lambda e, i=i, r0=r0: e.dma_start(out=xt[i][:], in_=xin[r0:r0 + 128, :]),
                                      reads=[r_xin] if r_xin else [], dst=xt[i])
                                ln_stats(cst, xt[i], junk, st[i])
                                k.op("dve", lambda e, i=i: e.scalar_tensor_tensor(junk[:], xt[i][:], st[i][:, 1:2], A1[:], ALU.mult, ALU.mult),
                                     reads=[xt[i], st[i], A1], writes=[junk])
                                k.op("dve", lambda e, i=i: e.tensor_tensor(hb[i][:], junk[:], B1[:], ALU.add),
                                     reads=[junk, B1], writes=[hb[i]])
                                for hf in range(2):
                                    for j in range(8):
                                        c = hf * 8 + j
                                        k.op("pe", lambda e, i=i, hf=hf, j=j, c=c: e.transpose(pT[hf][:, j, :], hb[i][:, c * 128:(c + 1) * 128], ident[:]),
                                             reads=[hb[i], ident], writes=[pT[hf]], inc=(j == 8 - 1))
                                    evac(hT[:, hf * 8:(hf + 1) * 8, tt * 128:(tt + 1) * 128], pT[hf][:], [pT[hf]], [hT])

                        for g, dil in enumerate((1, 4, 16)):
                            Lc = S // dil
                            tpc = Lc // 128

                            def hblk(c, tb_):
                                if dil == 1:
                                    return hT[:, c, tb_ * 512:(tb_ + 1) * 512]
                                v = hT[:, c, :].rearrange("p (i d) -> p d i", d=dil)
                                if dil == 4:
                                    return v[:, tb_, :]
                                return v[:, 4 * tb_:4 * tb_ + 4, :]

                            def htile(c, tt):
                                if dil == 1:
                                    return hT[:, c, tt * 128:(tt + 1) * 128]
                                v = hT[:, c, :].rearrange("p (i d) -> p d i", d=dil)
                                if dil == 4:
                                    return v[:, tt // 4, (tt % 4) * 128:(tt % 4 + 1) * 128]
                                return v[:, tt, :]

                            def pso(ps):
                                return ps[:].rearrange("p (r i) -> p r i", r=4) if dil == 16 else ps[:]

                            with k.scope() as sg:
                                qT = sg.sb("qT", [128, 4, S], BF16)
                                kT = sg.sb("kT", [128, 4, S], BF16)
                                V = sg.sb("V", [128, 16, 512], BF16)
                                with k.scope() as sw:
                                    wq = sw.sb("wq", [128, 16, 512], BF16, dma=True)
                                    wk = sw.sb("wk", [128, 16, 512], BF16, dma=True)
                                    wv = sw.sb("wv", [128, 16, 512], BF16, dma=True)
                                    wload(wq, win[:, g * 512:(g + 1) * 512], 512)
                                    wload(wk, win[:, 1536 + g * 512:1536 + (g + 1) * 512], 512)
                                    wload(wv, win[:, 3072 + g * 512:3072 + (g + 1) * 512], 512)
                                    pp = [sw.ps(f"pp{i}", [128, 512]) for i in range(2)]
                                    n = 0
                                    for (w_, dst, scl) in ((wq, qT, 128.0 ** -0.5), (wk, kT, None)):
                                        for h in range(4):
                                            for tb_ in range(4):
                                                ps = pp[n % 2]
                                                n += 1
                                                for c in range(16):
                                                    k.op("pe", lambda e, ps=ps, w_=w_, h=h, c=c, tb_=tb_: e.matmul(
                                                        pso(ps), w_[:, c, h * 128:(h + 1) * 128], hblk(c, tb_), start=(c == 0), stop=(c == 15)),
                                                        reads=[w_, hT], writes=[ps], inc=(c == 15))
                                                evac(dst[:, h, tb_ * 512:(tb_ + 1) * 512], ps[:], [ps], [dst], scale=scl)
                                    for tt in range(16):
                                        ps = pp[n % 2]
                                        n += 1
                                        for c in range(16):
                                            k.op("pe", lambda e, ps=ps, c=c, tt=tt: e.matmul(ps[:], htile(c, tt), wv[:, c, :], start=(c == 0), stop=(c == 15)),
                                                 reads=[wv, hT], writes=[ps], inc=(c == 15))
                                        evac(V[:, tt, :], ps[:], [ps], [V])
                                pqi = sg.sb("pqi", [128, 16], I32, dma=True)
                                pq = sg.sb("pq", [128, 16], F32)
                                pki = sg.sb("pki", [128, S], I32, dma=True)
                                pk = sg.sb("pk", [128, S], F32)
                                pb = pos_d[b]
                                if dil == 1:
                                    k.dma("sp", lambda e: e.dma_start(out=pqi[:], in_=pb.rearrange("(t a) -> a t", a=128), allow_slow_non_contiguous=True), dst=pqi)
                                elif dil == 4:
                                    k.dma("sp", lambda e: e.dma_start(out=pqi[:].rearrange("a (r q) -> a r q", r=4),
                                                                      in_=pb.rearrange("(q a r) -> a r q", q=4, a=128, r=4), allow_slow_non_contiguous=True), dst=pqi)
                                else:
                                    k.dma("sp", lambda e: e.dma_start(out=pqi[:], in_=pb.rearrange("(a r) -> a r", r=16)), dst=pqi)
                                k.dma("sp", lambda e: e.dma_start(out=pki[:], in_=pos_d[b:b + 1, :].broadcast_to([128, S])), dst=pki)
                                k.op("dve", lambda e: e.tensor_copy(pq[:], pqi[:]), reads=[pqi], writes=[pq])
                                k.op("dve", lambda e: e.tensor_scalar(pq[:], pq[:], -1.0, None, ALU.mult), reads=[pq], writes=[pq])
                                k.op("dve", lambda e: e.tensor_copy(pk[:], pki[:]), reads=[pki], writes=[pk])
                                dist = [sg.sb(f"dist{i}", [128, 384], F32) for i in range(2)]
                                ssb = [sg.sb(f"ssb{i}", [128, 384], F32) for i in range(2)]
                                pb16 = [sg.sb(f"pb{i}", [128, 384], BF16) for i in range(2)]
                                pts = [sg.sb(f"pts{i}", [128, 3, 128], BF16) for i in range(2)]
                                og = [sg.sb(f"og{i}", [128, 512], F32) for i in range(2)]
                                lse = [sg.sb(f"lse{i}", [128, 4], F32) for i in range(2)]
                                sm_ = [[sg.sb(f"sm{i}_{j}", [128, 1], F32) for j in range(5)] for i in range(2)]
                                sps = [sg.ps(f"sps{i}", [128, 512]) for i in range(2)]
                                ptp = [sg.ps(f"ptp{i}", [128, 4, 128], BF16) for i in range(2)]
                                ops = [sg.ps(f"ops{i}", [128, 512]) for i in range(2)]
                                def geom(tt):
                                    cls, ti = tt // tpc, tt % tpc
                                    i0 = ti * 128
                                    pbase = cls * Lc
                                    lo, hi = max(0, i0 - 128), min(Lc, i0 + 256)
                                    return cls, i0, pbase, lo, hi - lo, 128 - (i0 - lo), (hi - lo) // 128

                                dsteps = [(tt, h) for tt in range(16) for h in range(4)]

                                def dstage_a1(i):
                                    tt, h = dsteps[i]
                                    it, ih = tt % 2, i % 2
                                    cls, i0, pbase, lo, w, c0, nch = geom(tt)
                                    if h == 0:
                                        if dil == 1:
                                            pkv = pk[:, lo:lo + w]
                                        else:
                                            pkv = pk[:, :].rearrange("p (i d) -> p d i", d=dil)[:, cls, lo:lo + w]
                                        k.op("act", lambda e: e.activation(dist[it][:, :w], pkv, AF.Abs, bias=pq[:, tt:tt + 1], scale=1.0),
                                             reads=[pk, pq], writes=[dist[it]])
                                        k.op("dve", lambda e: e.tensor_tensor(dist[it][:, :w], dist[it][:, :w], mband[:, c0:c0 + w], ALU.add),
                                             reads=[dist[it], mband], writes=[dist[it]])
                                    mx, nmx, ll, rl, lnl = sm_[ih]
                                    slope = slopes[g * 4 + h]
                                    q0 = pbase + i0
                                    k.op("pe", lambda e: e.matmul(sps[ih][:, :w], qT[:, h, q0:q0 + 128], kT[:, h, pbase + lo:pbase + lo + w], start=True, stop=True),
                                         reads=[qT, kT], writes=[sps[ih]])
                                    k.op("dve", lambda e: e.scalar_tensor_tensor(ssb[ih][:, :w], dist[it][:, :w], -slope, sps[ih][:, :w], ALU.mult, ALU.add),
                                         reads=[dist[it], sps[ih]], writes=[ssb[ih]])
                                    k.op("dve", lambda e: e.reduce_max(mx[:], ssb[ih][:, :w], AX.X), reads=[ssb[ih]], writes=[mx])
                                    k.op("dve", lambda e: e.tensor_scalar(nmx[:], mx[:], -1.0, None, ALU.mult), reads=[mx], writes=[nmx])

                                def dstage_a2(i):
                                    tt, h = dsteps[i]
                                    ih = i % 2
                                    cls, i0, pbase, lo, w, c0, nch = geom(tt)
                                    mx, nmx, ll, rl, lnl = sm_[ih]
                                    k.op("act", lambda e: e.activation(pb16[ih][:, :w], ssb[ih][:, :w], AF.Exp, bias=nmx[:, 0:1], scale=1.0, accum_out=ll[:, 0:1]),
                                         reads=[ssb[ih], nmx], writes=[pb16[ih], ll])

                                def dstage_b1(i):
                                    tt, h = dsteps[i]
                                    ih = i % 2
                                    cls, i0, pbase, lo, w, c0, nch = geom(tt)
                                    for j in range(nch):
                                        k.op("pe", lambda e, j=j: e.transpose(ptp[ih][:, j, :], pb16[ih][:, j * 128:(j + 1) * 128], ident[:]),
                                             reads=[pb16[ih], ident], writes=[ptp[ih]], inc=(j == nch - 1))
                                    evac(pts[ih][:, :nch, :], ptp[ih][:, :nch, :], [ptp[ih]], [pts[ih]], eng="act")

                                def dstage_b2(i):
                                    tt, h = dsteps[i]
                                    it, ih = tt % 2, i % 2
                                    cls, i0, pbase, lo, w, c0, nch = geom(tt)
                                    mx, nmx, ll, rl, lnl = sm_[ih]
                                    vt0 = (pbase + lo) // 128
                                    for j in range(nch):
                                        k.op("pe", lambda e, j=j: e.matmul(ops[it][:, h * 128:(h + 1) * 128], pts[ih][:, j, :], V[:, vt0 + j, h * 128:(h + 1) * 128],
                                                                          start=(j == 0), stop=(j == nch - 1)), reads=[pts[ih], V], writes=[ops[it]], inc=(j == nch - 1))
                                    k.op("dve", lambda e: e.reciprocal(rl[:], ll[:]), reads=[ll], writes=[rl])
                                    k.op("dve", lambda e: e.tensor_scalar(og[it][:, h * 128:(h + 1) * 128], ops[it][:, h * 128:(h + 1) * 128], rl[:, 0:1], None, ALU.mult),
                                         reads=[ops[it], rl], writes=[og[it]])
                                    k.op("act", lambda e: e.activation(lnl[:], ll[:], AF.Ln), reads=[ll], writes=[lnl])
                                    k.op("dve", lambda e: e.tensor_tensor(lse[it][:, h:h + 1], mx[:], lnl[:], ALU.add), reads=[mx, lnl], writes=[lse[it]])
                                    if h == 3:
                                        ob = oD[g][b * S:(b + 1) * S, :]
                                        lb = lseD[g][b * S:(b + 1) * S, :]
                                        if dil == 1:
                                            orow, lrow = ob[tt * 128:(tt + 1) * 128, :], lb[tt * 128:(tt + 1) * 128, :]
                                        else:
                                            orow = ob.rearrange("(i d) f -> d i f", d=dil)[cls, i0:i0 + 128, :]
                                            lrow = lb.rearrange("(i d) f -> d i f", d=dil)[cls, i0:i0 + 128, :]
                                        k.dma("sp", lambda e: e.dma_start(out=orow, in_=og[it][:]), reads=[og[it]], dst=r_o[g])
                                        k.dma("sp", lambda e: e.dma_start(out=lrow, in_=lse[it][:]), reads=[lse[it]], dst=r_lse[g])

                                for i in range(len(dsteps) + 1):
                                    if i < len(dsteps):
                                        dstage_a1(i)
                                    if i >= 1:
                                        dstage_b1(i - 1)
                                    if i < len(dsteps):
                                        dstage_a2(i)
                                    if i >= 1:
                                        dstage_b2(i - 1)

                        with k.scope() as sl:
                            cosk = sl.sb("cosk", [64, S], F32)
                            sink = sl.sb("sink", [64, S], F32)
                            with k.scope() as srt:
                                rope_tables(srt, b, cosk, sink, 1.0)
                            wcq = sl.sb("wcq", [128, 16, 512], BF16, dma=True)
                            wckv = sl.sb("wckv", [128, 16, 512], BF16, dma=True)
                            wkr = sl.sb("wkr", [128, 16, 128], BF16, dma=True)
                            wload(wcq, win[:, 4608:5120], 512)
                            wload(wckv, win[:, 5120:5632], 512)
                            wv_ = win.rearrange("(c p) n -> p c n", p=128)
                            k.dma("pool", [lambda e: e.dma_start(out=wkr[:, :, 0:64], in_=wv_[:, :, 5632:5696]),
                                           lambda e: e.dma_start(out=wkr[:, :, 64:96], in_=wv_[:, :, 5664:5696]),
                                           lambda e: e.dma_start(out=wkr[:, :, 96:128], in_=wv_[:, :, 5632:5664])], dst=wkr)
                            gq = sl.sb("gq", [128, 4], F32, dma=True)
                            gkv = sl.sb("gkv", [128, 4], F32, dma=True)
                            k.dma("sp", lambda e: e.dma_start(out=gq[:], in_=Wl["q_norm_g"].rearrange("(c p) -> p c", p=128), allow_slow_non_contiguous=True), dst=gq)
                            k.dma("sp", lambda e: e.dma_start(out=gkv[:], in_=Wl["kv_norm_g"].rearrange("(c p) -> p c", p=128), allow_slow_non_contiguous=True), dst=gkv)
                            latf = sl.sb("latf", [128, 4, 512], F32)
                            sq = sl.sb("sq", [128, 4, 512], F32)
                            rs = sl.sb("rs", [128, 512], F32)
                            t1 = sl.sb("t1", [64, 512], F32)
                            t2 = sl.sb("t2", [64, 512], F32)
                            lps = [sl.ps(f"lps{i}", [128, 512]) for i in range(4)]
                            sps_ = sl.ps("ssq", [128, 512])
                            psr = sl.ps("psr", [64, 512])
                            pss = sl.ps("pss", [64, 512])
                            for (w_, gv, dst) in ((wcq, gq, cqn), (wckv, gkv, ckvn)):
                                for tb_ in range(4):
                                    blk = slice(tb_ * 512, (tb_ + 1) * 512)
                                    for c4 in range(4):
                                        for c in range(16):
                                            k.op("pe", lambda e, c4=c4, c=c, w_=w_, blk=blk: e.matmul(lps[c4][:], w_[:, c, c4 * 128:(c4 + 1) * 128], hT[:, c, blk], start=(c == 0), stop=(c == 15)),
                                                 reads=[w_, hT], writes=[lps[c4]], inc=(c == 15))
                                        k.op("act", lambda e, c4=c4: e.copy(latf[:, c4, :], lps[c4][:]), reads=[lps[c4]], writes=[latf])
                                        k.op("act", lambda e, c4=c4: e.activation(sq[:, c4, :], lps[c4][:], AF.Square), reads=[lps[c4]], writes=[sq])
                                    for c4 in range(4):
                                        k.op("pe", lambda e, c4=c4: e.matmul(sps_[:], onesf[:], sq[:, c4, :], start=(c4 == 0), stop=(c4 == 3)),
                                             reads=[onesf, sq], writes=[sps_], inc=(c4 == 3))
                                    k.op("act", lambda e: e.activation(rs[:], sps_[:], AF.Sqrt, bias=epsD[:, 0:1], scale=1.0 / 512.0), reads=[sps_, epsD], writes=[rs])
                                    k.op("dve", lambda e: e.reciprocal(rs[:], rs[:]), reads=[rs], writes=[rs])
                                    for c4 in range(4):
                                        k.op("dve", lambda e, c4=c4, gv=gv, dst=dst, blk=blk: e.scalar_tensor_tensor(dst[:, c4, blk], latf[:, c4, :], gv[:, c4:c4 + 1], rs[:], ALU.mult, ALU.mult),
                                             reads=[latf, gv, rs], writes=[dst])
                            for tb_ in range(4):
                                blk = slice(tb_ * 512, (tb_ + 1) * 512)
                                for c in range(16):
                                    k.op("pe", lambda e, c=c, blk=blk: e.matmul(psr[:], wkr[:, c, 0:64], hT[:, c, blk], start=(c == 0), stop=(c == 15)), reads=[wkr, hT], writes=[psr], inc=(c == 15))
                                for c in range(16):
                                    k.op("pe", lambda e, c=c, blk=blk: e.matmul(pss[:], wkr[:, c, 64:128], hT[:, c, blk], start=(c == 0), stop=(c == 15)), reads=[wkr, hT], writes=[pss], inc=(c == 15))
                                k.op("dve", lambda e, blk=blk: e.tensor_tensor(t1[:], psr[:], cosk[:, blk], ALU.mult), reads=[psr, cosk], writes=[t1])
                                k.op("dve", lambda e, blk=blk: e.tensor_tensor(t2[:], pss[:], sink[:, blk], ALU.mult), reads=[pss, sink], writes=[t2])
                                k.op("dve", lambda e, blk=blk: e.tensor_tensor(krT[:, blk], t1[:], t2[:], ALU.add), reads=[t1, t2], writes=[krT])

                        with k.scope() as sgt:
                            wg = [sgt.sb(f"wg{i}", [128, 16, 512], BF16, dma=True) for i in range(2)]
                            gsb = [sgt.sb(f"gsb{i}", [128, 512], BF16) for i in range(2)]
                            gps = [sgt.ps(f"gps{i}", [128, 512]) for i in range(2)]
                            m = 0
                            for n in range(8):
                                wi = wg[n % 2]
                                wload(wi, win[:, 5696 + n * 512:5696 + (n + 1) * 512], 512)
                                for tt in range(16):
                                    i = m % 2
                                    m += 1
                                    for c in range(16):
                                        k.op("pe", lambda e, i=i, c=c, tt=tt, wi=wi: e.matmul(gps[i][:], hT[:, c, tt * 128:(tt + 1) * 128], wi[:, c, :], start=(c == 0), stop=(c == 15)),
                                             reads=[hT, wi], writes=[gps[i]], inc=(c == 15))
                                    k.op("act", lambda e, i=i: e.activation(gsb[i][:], gps[i][:], AF.Sigmoid), reads=[gps[i]], writes=[gsb[i]])
                                    r0 = b * S + tt * 128
                                    k.dma("sp", lambda e, i=i, r0=r0, n=n: e.dma_start(out=gD[r0:r0 + 128, n * 512:(n + 1) * 512], in_=gsb[i][:]), reads=[gsb[i]], dst=r_g)

                    with k.scope() as sa:
                        cosq = sa.sb("cosq", [64, S], F32)
                        sinq = sa.sb("sinq", [64, S], F32)
                        with k.scope() as srt:
                            rope_tables(srt, b, cosq, sinq, 192.0 ** -0.5)
                        wuq = sa.sb("wuq", [128, 4, 1536], BF16, dma=True)
                        wuqs = sa.sb("wuqs", [128, 4, 8, 64], BF16, dma=True)
                        wukv = sa.sb("wukv", [128, 4, 2048], BF16, dma=True)
                        wload(wuq, Wl["w_uq"], 1536)
                        wload(wukv, Wl["w_ukv"], 2048)
                        uqv = Wl["w_uq"].rearrange("(c p) (h x) -> p c h x", p=128, x=192)
                        k.dma("pool", [lambda e, c=c: e.dma_start(out=wuqs[:, c, :, 0:32], in_=uqv[:, c, :, 160:192]) for c in range(4)]
                              + [lambda e, c=c: e.dma_start(out=wuqs[:, c, :, 32:64], in_=uqv[:, c, :, 128:160]) for c in range(4)], dst=wuqs)
                        wukv_h = wukv[:, :, :].rearrange("p c (h x) -> p c h x", x=256)
                        for hh in range(2):
                            with k.scope() as sh2:
                                qnT = sh2.sb("qnT", [128, 4, S], BF16)
                                qrT = sh2.sb("qrT", [64, 4, S], BF16)
                                knT = sh2.sb("knT", [128, 4, S], BF16)
                                Vb = sh2.sb("Vb", [128, 16, 512], BF16)
                                with k.scope() as sp_:
                                    pn = [sp_.ps(f"pn{i}", [128, 512]) for i in range(2)]
                                    pr = sp_.ps("pr", [64, 512])
                                    pz = sp_.ps("pz", [64, 512])
                                    t1 = sp_.sb("t1", [64, 512], F32)
                                    t2 = sp_.sb("t2", [64, 512], F32)
                                    n = 0
                                    for hl in range(4):
                                        h = hh * 4 + hl
                                        for tb_ in range(4):
                                            blk = slice(tb_ * 512, (tb_ + 1) * 512)
                                            ps = pn[n % 2]
                                            n += 1
                                            for c in range(4):
                                                k.op("pe", lambda e, ps=ps, c=c, h=h, blk=blk: e.matmul(ps[:], wuq[:, c, h * 192:h * 192 + 128], cqn[:, c, blk], start=(c == 0), stop=(c == 3)),
                                                     reads=[wuq, cqn], writes=[ps], inc=(c == 3))
                                            evac(qnT[:, hl, blk], ps[:], [ps], [qnT], scale=192.0 ** -0.5)
                                            for c in range(4):
                                                k.op("pe", lambda e, c=c, h=h, blk=blk: e.matmul(pr[:], wuq[:, c, h * 192 + 128:h * 192 + 192], cqn[:, c, blk], start=(c == 0), stop=(c == 3)),
                                                     reads=[wuq, cqn], writes=[pr], inc=(c == 3))
                                            for c in range(4):
                                                k.op("pe", lambda e, c=c, h=h, blk=blk: e.matmul(pz[:], wuqs[:, c, h, :], cqn[:, c, blk], start=(c == 0), stop=(c == 3)),
                                                     reads=[wuqs, cqn], writes=[pz], inc=(c == 3))
                                            k.op("dve", lambda e, blk=blk: e.tensor_tensor(t1[:], pr[:], cosq[:, blk], ALU.mult), reads=[pr, cosq], writes=[t1])
                                            k.op("dve", lambda e, blk=blk: e.tensor_tensor(t2[:], pz[:], sinq[:, blk], ALU.mult), reads=[pz, sinq], writes=[t2])
                                            k.op("dve", lambda e, blk=blk, hl=hl: e.tensor_tensor(qrT[:, hl, blk], t1[:], t2[:], ALU.add), reads=[t1, t2], writes=[qrT])
                                            ps = pn[n % 2]
                                            n += 1
                                            for c in range(4):
                                                k.op("pe", lambda e, ps=ps, c=c, h=h, blk=blk: e.matmul(ps[:], wukv[:, c, h * 256:h * 256 + 128], ckvn[:, c, blk], start=(c == 0), stop=(c == 3)),
                                                     reads=[wukv, ckvn], writes=[ps], inc=(c == 3))
                                            evac(knT[:, hl, blk], ps[:], [ps], [knT])
                                    for tt in range(16):
                                        ps = pn[n % 2]
                                        n += 1
                                        for c in range(4):
                                            k.op("pe", lambda e, ps=ps, c=c, tt=tt: e.matmul(ps[:].rearrange("p (h x) -> p h x", h=4), ckvn[:, c, tt * 128:(tt + 1) * 128],
                                                                                          wukv_h[:, c, hh * 4:(hh + 1) * 4, 128:256], start=(c == 0), stop=(c == 3)),
                                                 reads=[wukv, ckvn], writes=[ps], inc=(c == 3))
                                        evac(Vb[:, tt, :], ps[:], [ps], [Vb])
                                with k.scope() as sat:
                                    Sps = sat.ps("Sps", [128, S])
                                    ptp = [sat.ps(f"ptp{i}", [128, 8, 128], BF16) for i in range(2)]
                                    ops = sat.ps("ops", [128, 512])
                                    P = [sat.sb(f"P{i}", [128, S], BF16) for i in range(2)]
                                    pts = [sat.sb(f"pts{i}", [128, 16, 128], BF16) for i in range(2)]
                                    yb = [sat.sb(f"yb{i}", [128, 512], BF16) for i in range(2)]
                                    sm_ = [[sat.sb(f"sm{i}_{j}", [128, 1], F32) for j in range(4)] for i in range(2)]
                                    steps = [(qt, hl) for qt in range(16) for hl in range(4)]

                                    def stage_a1(i):
                                        qt, hl = steps[i]
                                        ih = i % 2
                                        qs = slice(qt * 128, (qt + 1) * 128)
                                        mx, nmx, ll, rl = sm_[ih]
                                        for nk in range(4):
                                            ks = slice(nk * 512, (nk + 1) * 512)
                                            k.op("pe", lambda e, hl=hl, qs=qs, ks=ks: e.matmul(Sps[:, ks], qnT[:, hl, qs], knT[:, hl, ks], start=True, stop=False),
                                                 reads=[qnT, knT], writes=[Sps], inc=False)
                                            k.op("pe", lambda e, hl=hl, qs=qs, ks=ks: e.matmul(Sps[:, ks], qrT[:, hl, qs], krT[:, ks], start=False, stop=True),
                                                 reads=[qrT, krT], writes=[Sps], inc=(nk == 3))
                                        k.op("dve", lambda e, mx=mx: e.reduce_max(mx[:], Sps[:], AX.X), reads=[Sps], writes=[mx])
                                        k.op("dve", lambda e, mx=mx, nmx=nmx: e.tensor_scalar(nmx[:], mx[:], -1.0, None, ALU.mult), reads=[mx], writes=[nmx])

                                    def stage_a2(i):
                                        ih = i % 2
                                        mx, nmx, ll, rl = sm_[ih]
                                        k.op("act", lambda e, ih=ih, nmx=nmx, ll=ll: e.activation(P[ih][:], Sps[:], AF.Exp, bias=nmx[:, 0:1], scale=1.0, accum_out=ll[:, 0:1]),
                                             reads=[Sps, nmx], writes=[P[ih], ll])

                                    def stage_b1(i):
                                        ih = i % 2
                                        for hf in range(2):
                                            for j in range(8):
                                                c = hf * 8 + j
                                                k.op("pe", lambda e, ih=ih, hf=hf, j=j, c=c: e.transpose(ptp[hf][:, j, :], P[ih][:, c * 128:(c + 1) * 128], ident[:]),
                                                     reads=[P[ih], ident], writes=[ptp[hf]], inc=(j == 7))
                                            evac(pts[ih][:, hf * 8:(hf + 1) * 8, :], ptp[hf][:], [ptp[hf]], [pts[ih]], eng="act")

                                    def stage_b2(i):
                                        qt, hl = steps[i]
                                        ih = i % 2
                                        iy = qt % 2
                                        mx, nmx, ll, rl = sm_[ih]
                                        for c in range(16):
                                            k.op("pe", lambda e, ih=ih, c=c, hl=hl: e.matmul(ops[:, hl * 128:(hl + 1) * 128], pts[ih][:, c, :], Vb[:, c, hl * 128:(hl + 1) * 128], start=(c == 0), stop=(c == 15)),
                                                 reads=[pts[ih], Vb], writes=[ops], inc=(c == 15))
                                        k.op("dve", lambda e, ll=ll, rl=rl: e.reciprocal(rl[:], ll[:]), reads=[ll], writes=[rl])
                                        k.op("dve", lambda e, iy=iy, hl=hl, rl=rl: e.tensor_scalar(yb[iy][:, hl * 128:(hl + 1) * 128], ops[:, hl * 128:(hl + 1) * 128], rl[:, 0:1], None, ALU.mult),
                                             reads=[ops, rl], writes=[yb[iy]])
                                        if hl == 3:
                                            r0 = b * S + qt * 128
                                            k.dma("sp", lambda e, iy=iy, r0=r0: e.dma_start(out=ybD[r0:r0 + 128, hh * 512:(hh + 1) * 512], in_=yb[iy][:]), reads=[yb[iy]], dst=r_yb)

                                    for i in range(len(steps) + 1):
                                        if i < len(steps):
                                            stage_a1(i)
                                        if i >= 1:
                                            stage_b1(i - 1)
                                        if i < len(steps):
                                            stage_a2(i)
                                        if i >= 1:
                                            stage_b2(i - 1)

            def merge_layer(l, xin, r_xin):
                Wl = W[l]
                with k.scope() as sm:
                    wau = sm.sb("wau", [128, 4, D], BF16, dma=True)
                    wbu = sm.sb("wbu", [128, 8, D], BF16, dma=True)
                    wo = sm.sb("wo", [128, 16, D], BF16, dma=True)
                    wload(wau, Wl["w_a_up"], D)
                    wload(wbu, Wl["w_b_up"], D, 2)
                    wload(wo, Wl["w_o"], D, 4)
                    o_t2 = [[sm.sb(f"o{g}_{i}", [128, 512], F32, dma=True) for g in range(3)] for i in range(2)]
                    ls2 = [sm.sb(f"ls{i}", [128, 3, 4], F32, dma=True) for i in range(2)]
                    ybt2 = [sm.sb(f"ybt{i}", [128, 1024], BF16, dma=True) for i in range(2)]
                    gt2 = [sm.sb(f"gt{i}", [128, 4096], BF16, dma=True) for i in range(2)]
                    xt2 = [sm.sb(f"xt{i}", [128, D], F32, dma=True) for i in range(2)]
                    gp = sm.sb("gp", [128, D], F32, dma=True)
                    mm = sm.sb("mm", [128, 4], F32)
                    ee = sm.sb("ee", [128, 3, 4], F32)
                    den = sm.sb("den", [128, 4], F32)
                    yaf = sm.sb("yaf", [128, 512], F32)
                    tmp = sm.sb("tmp", [128, 512], F32)
                    tmp2 = sm.sb("tmp2", [128, 512], F32)
                    ya = sm.sb("ya", [128, 512], BF16)
                    yaT = sm.sb("yaT", [128, 4, 128], BF16)
                    ybT = sm.sb("ybT", [128, 8, 128], BF16)
                    mg = sm.sb("mg", [128, D], BF16)
                    mT = sm.sb("mT", [128, 16, 128], BF16)
                    xn = sm.sb("xn", [128, D], F32)
                    ptp = [sm.ps(f"ptp{i}", [128, 8, 128], BF16) for i in range(2)]
                    ua = sm.ps("ua", [128, 512])
                    ub = sm.ps("ub", [128, 512])
                    ops = [sm.ps(f"ops{i}", [128, 512]) for i in range(2)]
                    def issue_loads(t):
                        i_ = t % 2
                        r0 = t * 128
                        for g in range(3):
                            k.dma("sp", lambda e, g=g, r0=r0, i_=i_: e.dma_start(out=o_t2[i_][g][:], in_=oD[g][r0:r0 + 128, :]), reads=[r_o[g]], dst=o_t2[i_][g])
                        k.dma("sp", [lambda e, g=g, r0=r0, i_=i_: e.dma_start(out=ls2[i_][:, g, :], in_=lseD[g][r0:r0 + 128, :]) for g in range(3)], reads=r_lse, dst=ls2[i_])
                        k.dma("sp", lambda e, r0=r0, i_=i_: e.dma_start(out=ybt2[i_][:], in_=ybD[r0:r0 + 128, :]), reads=[r_yb], dst=ybt2[i_])
                        k.dma("sp", lambda e, r0=r0, i_=i_: e.dma_start(out=gt2[i_][:], in_=gD[r0:r0 + 128, :]), reads=[r_g], dst=gt2[i_])
                        k.dma("sp", lambda e, r0=r0, i_=i_: e.dma_start(out=xt2[i_][:], in_=xin[r0:r0 + 128, :]), reads=[r_xin] if r_xin else [], dst=xt2[i_])

                    issue_loads(0)
                    for t in range(NT):
                        b = t // 16
                        r0 = t * 128
                        o_t, ls, ybt, gt, xt = o_t2[t % 2], ls2[t % 2], ybt2[t % 2], gt2[t % 2], xt2[t % 2]
                        if t % 16 == 0:
                            k.dma("sp", lambda e, b=b: e.dma_start(out=gp[:], in_=mod_row(l, b, 2).broadcast_to([128, D])), reads=[r_mod], dst=gp)
                            k.op("dve", lambda e: e.tensor_scalar(gp[:], gp[:], 1.0, None, ALU.add), reads=[gp], writes=[gp])
                        if t + 1 < NT:
                            issue_loads(t + 1)
                        k.op("dve", lambda e: e.tensor_tensor(mm[:], ls[:, 0, :], ls[:, 1, :], ALU.max), reads=[ls], writes=[mm])
                        k.op("dve", lambda e: e.tensor_tensor(mm[:], mm[:], ls[:, 2, :], ALU.max), reads=[mm, ls], writes=[mm])
                        for g in range(3):
                            k.op("dve", lambda e, g=g: e.tensor_tensor(ee[:, g, :], ls[:, g, :], mm[:], ALU.subtract), reads=[ls, mm], writes=[ee])
                        k.op("act", lambda e: e.activation(ee[:], ee[:], AF.Exp), reads=[ee], writes=[ee])
                        k.op("dve", lambda e: e.tensor_tensor(den[:], ee[:, 0, :], ee[:, 1, :], ALU.add), reads=[ee], writes=[den])
                        k.op("dve", lambda e: e.tensor_tensor(den[:], den[:], ee[:, 2, :], ALU.add), reads=[den, ee], writes=[den])
                        k.op("dve", lambda e: e.reciprocal(den[:], den[:]), reads=[den], writes=[den])
                        for g in range(3):
                            k.op("dve", lambda e, g=g: e.tensor_tensor(ee[:, g, :], ee[:, g, :], den[:], ALU.mult), reads=[ee, den], writes=[ee])
                        for h in range(4):
                            hs = slice(h * 128, (h + 1) * 128)
                            k.op("dve", lambda e, h=h, hs=hs: e.tensor_scalar(yaf[:, hs], o_t[0][:, hs], ee[:, 0, h:h + 1], None, ALU.mult), reads=[o_t[0], ee], writes=[yaf])
                            k.op("dve", lambda e, h=h, hs=hs: e.scalar_tensor_tensor(yaf[:, hs], o_t[1][:, hs], ee[:, 1, h:h + 1], yaf[:, hs], ALU.mult, ALU.add), reads=[o_t[1], ee, yaf], writes=[yaf])
                            k.op("dve", lambda e, h=h, hs=hs: e.scalar_tensor_tensor(ya[:, hs], o_t[2][:, hs], ee[:, 2, h:h + 1], yaf[:, hs], ALU.mult, ALU.add), reads=[o_t[2], ee, yaf], writes=[ya])
                        for j in range(4):
                            k.op("pe", lambda e, j=j: e.transpose(ptp[0][:, j, :], ya[:, j * 128:(j + 1) * 128], ident[:]), reads=[ya, ident], writes=[ptp[0]], inc=(j == 4 - 1))
                        evac(yaT[:], ptp[0][:, 0:4, :], [ptp[0]], [yaT])
                        for j in range(8):
                            k.op("pe", lambda e, j=j: e.transpose(ptp[1][:, j, :], ybt[:, j * 128:(j + 1) * 128], ident[:]), reads=[ybt, ident], writes=[ptp[1]], inc=(j == 8 - 1))
                        evac(ybT[:], ptp[1][:], [ptp[1]], [ybT])
                        for n in range(4):
                            ns = slice(n * 512, (n + 1) * 512)
                            for c in range(4):
                                k.op("pe", lambda e, c=c, ns=ns: e.matmul(ua[:], yaT[:, c, :], wau[:, c, ns], start=(c == 0), stop=(c == 3)), reads=[yaT, wau], writes=[ua], inc=(c == 3))
                            for c in range(8):
                                k.op("pe", lambda e, c=c, ns=ns: e.matmul(ub[:], ybT[:, c, :], wbu[:, c, ns], start=(c == 0), stop=(c == 7)), reads=[ybT, wbu], writes=[ub], inc=(c == 7))
                            k.op("dve", lambda e, ns=ns: e.tensor_tensor(tmp[:], ua[:], gt[:, ns], ALU.mult), reads=[ua, gt], writes=[tmp])
                            k.op("dve", lambda e, n=n: e.tensor_tensor(tmp2[:], ub[:], gt[:, 2048 + n * 512:2048 + (n + 1) * 512], ALU.mult), reads=[ub, gt], writes=[tmp2])
                            k.op("pool", lambda e, ns=ns: e.tensor_tensor(mg[:, ns], tmp[:], tmp2[:], ALU.add), reads=[tmp, tmp2], writes=[mg])
                        for hf in range(2):
                            for j in range(8):
                                c = hf * 8 + j
                                k.op("pe", lambda e, hf=hf, j=j, c=c: e.transpose(ptp[hf][:, j, :], mg[:, c * 128:(c + 1) * 128], ident[:]), reads=[mg, ident], writes=[ptp[hf]], inc=(j == 8 - 1))
                            evac(mT[:, hf * 8:(hf + 1) * 8, :], ptp[hf][:], [ptp[hf]], [mT])
                        for n in range(4):
                            ns = slice(n * 512, (n + 1) * 512)
                            op_ = ops[n % 2]
                            for c in range(16):
                                k.op("pe", lambda e, c=c, ns=ns, op_=op_: e.matmul(op_[:], mT[:, c, :], wo[:, c, ns], start=(c == 0), stop=(c == 15)), reads=[mT, wo], writes=[op_], inc=(c == 15))
                            k.op("dve", lambda e, ns=ns, op_=op_: e.tensor_tensor(xn[:, ns], op_[:], gp[:, ns], ALU.mult), reads=[op_, gp], writes=[xn])
                            k.op("pool", lambda e, ns=ns: e.tensor_tensor(xn[:, ns], xn[:, ns], xt[:, ns], ALU.add), reads=[xn, xt], writes=[xn])
                        k.dma("sp", lambda e, r0=r0: e.dma_start(out=xaD[r0:r0 + 128, :], in_=xn[:]), reads=[xn], dst=r_xa)

            def moe_round(l, rnd, last):
                Wl = W[l]
                t0 = rnd * (TR // 128)
                with k.scope() as smo:
                    sl_i = smo.sb("sl_i", [128, 32, 2], I32)
                    wts = smo.sb("wts", [128, 32, 2], F32)
                    with k.scope() as s1:
                        wr = s1.sb("wr", [128, 16, 72], F32, dma=True)
                        k.dma("sp", [lambda e: e.dma_start(out=wr[:, :, 0:8], in_=Wl["w_grp"].rearrange("(c p) n -> p c n", p=128)),
                                     lambda e: e.dma_start(out=wr[:, :, 8:72], in_=Wl["w_exp"].rearrange("(c p) n -> p c n", p=128))], dst=wr)
                        br = s1.sb("br", [128, 72], F32, dma=True)
                        k.dma("sp", [lambda e: e.dma_start(out=br[:, 0:8], in_=Wl["b_grp"].broadcast_to([128, 8])),
                                     lambda e: e.dma_start(out=br[:, 8:72], in_=Wl["b_exp"].broadcast_to([128, 64]))], dst=br)
                        A2 = s1.sb("A2", [128, D], F32, dma=True)
                        B2 = s1.sb("B2", [128, D], F32, dma=True)
                        G2 = bcast(s1, "G2", Wl["ln2_g"], D)
                        R = s1.sb("R", [128, 64], BF16)
                        k.op("pool", lambda e: e.memset(R[:], 0.0), writes=[R])
                        xt = [s1.sb(f"xt{i}", [128, D], F32, dma=True) for i in range(2)]
                        junk = s1.sb("junk", [128, D], F32)
                        h2f = s1.sb("h2f", [128, D], F32)
                        h2b = [s1.sb(f"h2b{i}", [128, D], BF16) for i in range(2)]
                        h2T = s1.sb("h2T", [128, 16, 128], F32)
                        st = s1.sb("st", [128, 2], F32)
                        lg = s1.sb("lg", [128, 72], F32)
                        m8 = s1.sb("m8", [128, 8], F32)
                        s8 = s1.sb("s8", [128, 8], F32)
                        sc_ = s1.sb("sc_", [128, 16], F32)
                        eg = s1.sb("eg", [128, 8], F32)
                        Gm = s1.sb("Gm", [128, 8], F32)
                        lm = s1.sb("lm", [128, 64], F32)
                        E1 = s1.sb("E1", [128, 64], F32)
                        E2 = s1.sb("E2", [128, 64], F32)
                        Ab = s1.sb("Ab", [128, 64], BF16)
                        cnt = s1.sb("cnt", [128, 64], F32)
                        tq = s1.sb("tq", [128, 64], F32)
                        slf = s1.sb("slf", [128, 2], F32)
                        ptf = [s1.ps(f"ptf{i}", [128, 4, 128], F32) for i in range(2)]
                        lps = s1.ps("lps", [128, 72])
                        cps = s1.ps("cps", [128, 64])
                        for tl in range(TR // 128):
                            t = t0 + tl
                            b = t // 16
                            i = tl % 2
                            r0 = t * 128
                            if t % 16 == 0:
                                k.dma("sp", lambda e, b=b: e.dma_start(out=A2[:], in_=mod_row(l, b, 4).broadcast_to([128, D])), reads=[r_mod], dst=A2)
                                k.dma("sp", lambda e, b=b: e.dma_start(out=B2[:], in_=mod_row(l, b, 3).broadcast_to([128, D])), reads=[r_mod], dst=B2)
                                k.op("dve", lambda e: e.scalar_tensor_tensor(A2[:], A2[:], 1.0, G2[:], ALU.add, ALU.mult), reads=[A2, G2], writes=[A2])
                            k.dma("sp", lambda e, i=i, r0=r0: e.dma_start(out=xt[i][:], in_=xaD[r0:r0 + 128, :]), reads=[r_xa], dst=xt[i])
                            ln_stats(cst, xt[i], junk, st)
                            k.op("dve", lambda e, i=i: e.scalar_tensor_tensor(junk[:], xt[i][:], st[:, 1:2], A2[:], ALU.mult, ALU.mult), reads=[xt[i], st, A2], writes=[junk])
                            k.op("dve", lambda e: e.tensor_tensor(h2f[:], junk[:], B2[:], ALU.add), reads=[junk, B2], writes=[h2f])
                            k.op("act", lambda e, i=i: e.copy(h2b[i][:], h2f[:]), reads=[h2f], writes=[h2b[i]])
                            for c4 in range(4):
                                pf = ptf[c4 % 2]
                                for j in range(4):
                                    c = c4 * 4 + j
                                    k.op("pe", lambda e, pf=pf, j=j, c=c: e.transpose(pf[:, j, :], h2f[:, c * 128:(c + 1) * 128], identf[:]), reads=[h2f, identf], writes=[pf], inc=(j == 4 - 1))
                                evac(h2T[:, c4 * 4:(c4 + 1) * 4, :], pf[:], [pf], [h2T])
                            for c in range(16):
                                k.op("pe", lambda e, c=c: e.matmul(lps[:], h2T[:, c, :], wr[:, c, :], start=(c == 0), stop=(c == 15)), reads=[h2T, wr], writes=[lps], inc=(c == 15))
                            k.op("dve", lambda e: e.tensor_tensor(lg[:], lps[:], br[:], ALU.add), reads=[lps, br], writes=[lg])
                            k.op("dve", lambda e: e.max(m8[:], lg[:, 0:8]), reads=[lg], writes=[m8])
                            k.op("dve", lambda e: e.tensor_scalar(sc_[:, 0:1], m8[:, 0:1], -1.0, None, ALU.mult), reads=[m8], writes=[sc_])
                            k.op("act", lambda e: e.activation(eg[:], lg[:, 0:8], AF.Exp, bias=sc_[:, 0:1], scale=1.0, accum_out=sc_[:, 1:2]), reads=[lg, sc_], writes=[eg, sc_])
                            k.op("dve", lambda e: e.reciprocal(sc_[:, 2:3], sc_[:, 1:2]), reads=[sc_], writes=[sc_])
                            k.op("dve", lambda e: e.tensor_scalar(Gm[:], lg[:, 0:8], m8[:, 0:1], None, ALU.is_equal), reads=[lg, m8], writes=[Gm])
                            k.op("dve", lambda e: e.tensor_scalar(eg[:], Gm[:], 1.0, BIG, ALU.subtract, ALU.mult), reads=[Gm], writes=[eg])
                            for g in range(8):
                                k.op("dve", lambda e, g=g: e.tensor_scalar(lm[:, g * 8:(g + 1) * 8], lg[:, 8 + g * 8:16 + g * 8], eg[:, g:g + 1], None, ALU.add), reads=[lg, eg], writes=[lm])
                            k.op("dve", lambda e: e.max(s8[:], lm[:]), reads=[lm], writes=[s8])
                            k.op("dve", lambda e: e.tensor_scalar(E1[:], lm[:], s8[:, 0:1], None, ALU.is_equal), reads=[lm, s8], writes=[E1])
                            k.op("dve", lambda e: e.tensor_scalar(E2[:], lm[:], s8[:, 1:2], None, ALU.is_equal), reads=[lm, s8], writes=[E2])
                            k.op("dve", lambda e: e.tensor_scalar(sc_[:, 3:4], s8[:, 0:1], -1.0, None, ALU.mult), reads=[s8], writes=[sc_])
                            k.op("act", lambda e: e.activation(sc_[:, 4:5], s8[:, 1:2], AF.Exp, bias=sc_[:, 3:4], scale=1.0), reads=[s8, sc_], writes=[sc_])
                            k.op("dve", lambda e: e.tensor_scalar(sc_[:, 5:6], sc_[:, 4:5], 1.0, None, ALU.add), reads=[sc_], writes=[sc_])
                            k.op("dve", lambda e: e.reciprocal(sc_[:, 5:6], sc_[:, 5:6]), reads=[sc_], writes=[sc_])
                            k.op("dve", lambda e: e.tensor_tensor(sc_[:, 6:7], sc_[:, 2:3], sc_[:, 5:6], ALU.mult), reads=[sc_], writes=[sc_])
                            k.op("dve", lambda e: e.tensor_tensor(sc_[:, 7:8], sc_[:, 6:7], sc_[:, 4:5], ALU.mult), reads=[sc_], writes=[sc_])
                            k.op("dve", lambda e: e.tensor_tensor(Ab[:], E1[:], E2[:], ALU.add), reads=[E1, E2], writes=[Ab])
                            k.op("pe", lambda e: e.matmul(cps[:], LT[:], Ab[:], start=True, stop=False), reads=[LT, Ab], writes=[cps])
                            k.op("pe", lambda e: e.matmul(cps[:], onesb[:], R[:], start=False, stop=True), reads=[onesb, R], writes=[cps])
                            k.op("dve", lambda e: e.tensor_copy(cnt[:], cps[:]), reads=[cps], writes=[cnt])
                            k.op("pool", lambda e: e.tensor_tensor(R[:], R[:], Ab[:], ALU.add), reads=[R, Ab], writes=[R])
                            for kk_, Ek in ((0, E1), (1, E2)):
                                k.op("dve", lambda e, Ek=Ek: e.tensor_tensor(tq[:], Ek[:], cnt[:], ALU.mult), reads=[Ek, cnt], writes=[tq])
                                k.op("dve", lambda e, kk_=kk_: e.reduce_sum(sc_[:, 8 + kk_:9 + kk_], tq[:], AX.X), reads=[tq], writes=[sc_])
                                k.op("dve", lambda e, Ek=Ek: e.tensor_tensor(tq[:], Ek[:], eC[:], ALU.mult), reads=[Ek, eC], writes=[tq])
                                k.op("dve", lambda e, kk_=kk_: e.reduce_sum(sc_[:, 10 + kk_:11 + kk_], tq[:], AX.X), reads=[tq], writes=[sc_])
                                k.op("dve", lambda e, kk_=kk_: e.tensor_scalar(sc_[:, 12 + kk_:13 + kk_], sc_[:, 8 + kk_:9 + kk_], float(C_CAP), None, ALU.is_lt), reads=[sc_], writes=[sc_])
                                k.op("dve", lambda e, kk_=kk_: e.tensor_tensor(sc_[:, 8 + kk_:9 + kk_], sc_[:, 8 + kk_:9 + kk_], sc_[:, 10 + kk_:11 + kk_], ALU.add), reads=[sc_], writes=[sc_])
                                k.op("dve", lambda e, kk_=kk_: e.tensor_scalar(sc_[:, 8 + kk_:9 + kk_], sc_[:, 8 + kk_:9 + kk_], float(-NSLOT), None, ALU.add), reads=[sc_], writes=[sc_])
                                k.op("dve", lambda e, kk_=kk_: e.tensor_tensor(sc_[:, 8 + kk_:9 + kk_], sc_[:, 8 + kk_:9 + kk_], sc_[:, 12 + kk_:13 + kk_], ALU.mult), reads=[sc_], writes=[sc_])
                                k.op("dve", lambda e, kk_=kk_: e.tensor_scalar(slf[:, kk_:kk_ + 1], sc_[:, 8 + kk_:9 + kk_], float(NSLOT), None, ALU.add), reads=[sc_], writes=[slf])
                                k.op("dve", lambda e, kk_=kk_, tl=tl: e.tensor_tensor(wts[:, tl, kk_:kk_ + 1], sc_[:, 6 + kk_:7 + kk_], sc_[:, 12 + kk_:13 + kk_], ALU.mult), reads=[sc_], writes=[wts])
                            k.op("dve", lambda e, tl=tl: e.tensor_copy(sl_i[:, tl, :], slf[:]), reads=[slf], writes=[sl_i])
                            for kk_ in range(2):
                                k.dma("pool", lambda e, i=i, tl=tl, kk_=kk_: e.indirect_dma_start(
                                    out=XsD[:, :], out_offset=bass.IndirectOffsetOnAxis(ap=sl_i[:, tl, kk_:kk_ + 1], axis=0),
                                    in_=h2b[i][:, :], in_offset=None), reads=[h2b[i], sl_i], dst=r_xs)
                        if "slotD" in ext:
                            k.dma("sp", [lambda e: e.dma_start(out=slotD[t0 * 128:t0 * 128 + TR, 0:2].rearrange("(t p) k -> p t k", p=128), in_=wts[:], allow_slow_non_contiguous=True)],
                                  reads=[wts], dst=r_slot)

                    with k.scope() as s2:
                        wgu = [s2.sb(f"wgu{i}", [128, 16, 1024], BF16, dma=True) for i in range(2)]
                        wd = [s2.sb(f"wd{i}", [128, 4, D], BF16, dma=True) for i in range(2)]
                        xsl = [s2.sb(f"xsl{i}", [128, 2, D], BF16, dma=True) for i in range(2)]
                        xT = s2.sb("xT", [128, 16, 256], BF16)
                        sg_ = s2.sb("sg_", [128, 256], F32)
                        aT = s2.sb("aT", [128, 4, 256], BF16)
                        ysb = [s2.sb(f"ysb{i}", [128, D], F32) for i in range(2)]
                        ptp = [s2.ps(f"ptp{i}", [128, 8, 128], BF16) for i in range(2)]
                        gps = s2.ps("gps", [128, 256])
                        ups = s2.ps("ups", [128, 256])
                        yps = [s2.ps(f"yps{i}", [128, 512]) for i in range(2)]
                        for ex in range(NE):
                            i = ex % 2
                            gi, ei = ex // 8, ex % 8
                            wload(wgu[i], Wl["w_gu"][gi][ei], 1024, 4)
                            wload(wd[i], Wl["w_down"][gi][ei], D, 2)
                            s0 = ex * C_CAP
                            k.dma("sp", [lambda e, i=i, s=s, s0=s0: e.dma_start(out=xsl[i][:, s, :], in_=XsD[s0 + s * 128:s0 + (s + 1) * 128, :]) for s in range(2)],
                                  reads=[r_xs], dst=xsl[i])
                            for s in range(2):
                                for hf in range(2):
                                    for j in range(8):
                                        c = hf * 8 + j
                                        k.op("pe", lambda e, i=i, s=s, hf=hf, j=j, c=c: e.transpose(ptp[hf][:, j, :], xsl[i][:, s, c * 128:(c + 1) * 128], ident[:]),
                                             reads=[xsl[i], ident], writes=[ptp[hf]], inc=(j == 8 - 1))
                                    evac(xT[:, hf * 8:(hf + 1) * 8, s * 128:(s + 1) * 128], ptp[hf][:], [ptp[hf]], [xT])
                            for j in range(4):
                                for c in range(16):
                                    k.op("pe", lambda e, i=i, j=j, c=c: e.matmul(gps[:], wgu[i][:, c, j * 128:(j + 1) * 128], xT[:, c, :], start=(c == 0), stop=(c == 15)),
                                         reads=[wgu[i], xT], writes=[gps], inc=(c == 15))
                                for c in range(16):
                                    k.op("pe", lambda e, i=i, j=j, c=c: e.matmul(ups[:], wgu[i][:, c, 512 + j * 128:512 + (j + 1) * 128], xT[:, c, :], start=(c == 0), stop=(c == 15)),
                                         reads=[wgu[i], xT], writes=[ups], inc=(c == 15))
                                k.op("act", lambda e: e.activation(sg_[:], gps[:], AF.Silu), reads=[gps], writes=[sg_])
                                k.op("dve", lambda e, j=j: e.tensor_tensor(aT[:, j, :], sg_[:], ups[:], ALU.mult), reads=[sg_, ups], writes=[aT])
                            for s in range(2):
                                for n in range(4):
                                    yp = yps[n % 2]
                                    for j in range(4):
                                        k.op("pe", lambda e, i=i, s=s, n=n, j=j, yp=yp: e.matmul(yp[:], aT[:, j, s * 128:(s + 1) * 128], wd[i][:, j, n * 512:(n + 1) * 512], start=(j == 0), stop=(j == 3)),
                                             reads=[aT, wd[i]], writes=[yp], inc=(j == 3))
                                    evac(ysb[s][:, n * 512:(n + 1) * 512], yp[:], [yp], [ysb[s]])
                                k.dma("sp", lambda e, s=s, s0=s0: e.dma_start(out=YsD[s0 + s * 128:s0 + (s + 1) * 128, :], in_=ysb[s][:]), reads=[ysb[s]], dst=r_ys)

                    with k.scope() as s3:
                        gp = s3.sb("gp", [128, D], F32, dma=True)
                        y1 = [s3.sb(f"y1_{i}", [128, D], F32, dma=True) for i in range(2)]
                        y2 = [s3.sb(f"y2_{i}", [128, D], F32, dma=True) for i in range(2)]
                        xt = [s3.sb(f"xt{i}", [128, D], F32, dma=True) for i in range(2)]
                        xn = [s3.sb(f"xn{i}", [128, D], F32) for i in range(2)]
                        junk = s3.sb("junk", [128, D], F32)
                        st = s3.sb("st", [128, 2], F32)
                        fg = bcast(s3, "fg", fin_g, D) if last else None
                        for tl in range(TR // 128):
                            t = t0 + tl
                            b = t // 16
                            i = tl % 2
                            r0 = t * 128
                            if t % 16 == 0:
                                k.dma("sp", lambda e, b=b: e.dma_start(out=gp[:], in_=mod_row(l, b, 5).broadcast_to([128, D])), reads=[r_mod], dst=gp)
                                k.op("dve", lambda e: e.tensor_scalar(gp[:], gp[:], 1.0, None, ALU.add), reads=[gp], writes=[gp])
                            for kk_, yy in ((0, y1[i]), (1, y2[i])):
                                k.dma("pool", lambda e, yy=yy, tl=tl, kk_=kk_: e.indirect_dma_start(
                                    out=yy[:, :], out_offset=None, in_=YsD[:, :],
                                    in_offset=bass.IndirectOffsetOnAxis(ap=sl_i[:, tl, kk_:kk_ + 1], axis=0)), reads=[r_ys, sl_i], dst=yy)
                            k.dma("sp", lambda e, i=i, r0=r0: e.dma_start(out=xt[i][:], in_=xaD[r0:r0 + 128, :]), reads=[r_xa], dst=xt[i])
                            k.op("dve", lambda e, i=i, tl=tl: e.tensor_scalar(y1[i][:], y1[i][:], wts[:, tl, 0:1], None, ALU.mult), reads=[y1[i], wts], writes=[y1[i]])
                            k.op("dve", lambda e, i=i, tl=tl: e.scalar_tensor_tensor(y1[i][:], y2[i][:], wts[:, tl, 1:2], y1[i][:], ALU.mult, ALU.add), reads=[y1[i], y2[i], wts], writes=[y1[i]])
                            k.op("pool", lambda e, i=i: e.tensor_tensor(y1[i][:], y1[i][:], gp[:], ALU.mult), reads=[y1[i], gp], writes=[y1[i]])
                            k.op("dve", lambda e, i=i: e.tensor_tensor(xn[i][:], y1[i][:], xt[i][:], ALU.add), reads=[y1[i], xt[i]], writes=[xn[i]])
                            if last:
                                ln_stats(cst, xn[i], junk, st)
                                k.op("dve", lambda e, i=i: e.scalar_tensor_tensor(xn[i][:], xn[i][:], st[:, 1:2], fg[:], ALU.mult, ALU.mult), reads=[xn[i], st, fg], writes=[xn[i]])
                                k.dma("sp", lambda e, i=i, r0=r0: e.dma_start(out=out_d[r0:r0 + 128, :], in_=xn[i][:]), reads=[xn[i]], dst=r_out)
                            else:
                                k.dma("sp", lambda e, i=i, r0=r0: e.dma_start(out=xbD[r0:r0 + 128, :], in_=xn[i][:]), reads=[xn[i]], dst=r_xb)

            xin, r_xin = x_d, None
            for l in range(nl_attn):
                for b in range(NB):
                    attn_seq(l, b, xin, r_xin)
                merge_layer(l, xin, r_xin)
                if l < nl_moe:
                    last = (stop is None and l == NL - 1)
                    for rnd in range(NR):
                        moe_round(l, rnd, last)
                    xin, r_xin = xbD, r_xb
            if stop is not None:
                with k.scope() as sd:
                    t_ = sd.sb("dbgt", [128, D], F32, dma=True)
                    src, rs_ = (modD, r_mod) if stop == "mod" else ((xaD, r_xa) if stop == "attn" else (xbD, r_xb))
                    if stop == "mod":
                        k.dma("sp", lambda e: e.dma_start(out=t_[0:2 * NB, :], in_=modD.rearrange("l b (j n) -> (l b) j n", n=D)[:, 0, :]), reads=[r_mod], dst=t_)
                        k.dma("sp", lambda e: e.dma_start(out=out_d[0:2 * NB, :], in_=t_[0:2 * NB, :]), reads=[t_], dst=r_out)
                    else:
                        for t in range(NT):
                            k.dma("sp", lambda e, t=t: e.dma_start(out=t_[:], in_=src[t * 128:(t + 1) * 128, :]), reads=[rs_], dst=t_)
                            k.dma("sp", lambda e, t=t: e.dma_start(out=out_d[t * 128:(t + 1) * 128, :], in_=t_[:]), reads=[t_], dst=r_out)
        k.barrier()
    return nc


def make_in_maps(inputs, n_cores, NB, NL=2, NG=8, stop=None):
    f = lambda a: np.ascontiguousarray(a)
    shared = {}
    run_attn = stop != "mod"
    run_moe = stop not in ("mod", "attn")
    for l in range(NL if stop is None else 1):
        shared[f"w_ada_{l}"] = f(inputs["w_ada"][l])
        shared[f"b_ada_{l}"] = f(inputs["b_ada"][l][None])
        if run_attn:
            shared[f"ln1_g_{l}"] = f(inputs["ln1_g"][l][None])
            for n in ("w_in", "q_norm_g", "w_uq", "kv_norm_g", "w_ukv", "w_a_up", "w_b_up", "w_o"):
                shared[f"{n}_{l}"] = f(inputs[n][l])
        if run_moe:
            shared[f"ln2_g_{l}"] = f(inputs["ln2_g"][l][None])
            shared[f"w_grp_{l}"] = f(inputs["w_grp"][l])
            shared[f"b_grp_{l}"] = f(inputs["b_grp"][l][None])
            shared[f"w_exp_{l}"] = f(inputs["w_exp"][l])
            shared[f"b_exp_{l}"] = f(inputs["b_exp"][l][None])
            for g in range(NG):
                shared[f"w_gu_{l}_{g}"] = f(inputs["w_gu"][l][g * 8:(g + 1) * 8])
                shared[f"w_down_{l}_{g}"] = f(inputs["w_down"][l][g * 8:(g + 1) * 8])
    if stop is None:
        shared["final_g"] = f(inputs["final_g"][None])
    maps = []
    for c in range(n_cores):
        m = dict(shared)
        sl = slice(c * NB, (c + 1) * NB)
        m["x"] = f(inputs["x"][sl]).reshape(NB * S, D)
        m["c"] = f(inputs["c"][sl])
        m["positions"] = f(inputs["positions"][sl]).astype(np.int32)
        maps.append(m)
    return maps


def kernel(**inputs):
    inputs = {k_: np.asarray(v) for k_, v in inputs.items()}
    n = N_CORES
    NB = 16 // n
    nc = build(NB)
    maps = make_in_maps(inputs, n, NB)
    res = run_bass_kernel_spmd(nc, maps, core_ids=list(range(n)))
    out = np.concatenate([r["out"].reshape(NB, S, D) for r in res.results], axis=0)
    return out.astype(np.float32)
```
